# Optimizing a Trainium2 kernel written in Bass

```python
import jax, jax.numpy as jnp
from jax import lax
import numpy as np

D_MODEL = 1024
BATCH = 4
SEQ = 8192
DEPTH = 1

N_META = 16
MIX_WIDTH = D_MODEL
POOL_WIDTH = MIX_WIDTH // 2
POOL_WINDOWS = (2, 4, 8, 16)
N_POOL_GROUPS = 4
POOL_GROUP = POOL_WIDTH // N_POOL_GROUPS
GLA_HEADS = 4
GLA_VDIM = MIX_WIDTH - POOL_WIDTH
GLA_HEAD_V = GLA_VDIM // GLA_HEADS
GLA_KDIM = GLA_VDIM // 2
GLA_HEAD_K = GLA_KDIM // GLA_HEADS
GATE_RANK = 16
GATE_NORMALIZER = 16.0
CHUNK = 64
N_GROUPS = 4
EXPERTS_PER_GROUP = 4
N_EXPERTS = N_GROUPS * EXPERTS_PER_GROUP
TOP_K_IN_GROUP = 2
EXPERT_HIDDEN = D_MODEL // 4
EPS = 1e-6
IN_COLS = POOL_WIDTH + 2 * GLA_KDIM + 2 * GLA_VDIM + GATE_RANK
SPLIT_POINTS = (POOL_WIDTH,
                POOL_WIDTH + GLA_KDIM,
                POOL_WIDTH + 2 * GLA_KDIM,
                POOL_WIDTH + 2 * GLA_KDIM + GLA_VDIM,
                POOL_WIDTH + 2 * GLA_KDIM + 2 * GLA_VDIM)

kernel_name = "hymba_pool_gla_hmoe_block"


def rmsnorm(x, w):
    xf = x.astype(jnp.float32)
    y = xf * lax.rsqrt(jnp.mean(xf * xf, axis=-1, keepdims=True) + EPS)
    return (y * w.astype(jnp.float32)).astype(x.dtype)


def pool_mixer(u, pool_w, pool_scale):
    B, L, _ = u.shape
    uf = u.astype(jnp.float32).reshape(B, L, N_POOL_GROUPS, POOL_GROUP)
    cs = jnp.concatenate([jnp.zeros((B, 1, N_POOL_GROUPS, POOL_GROUP), jnp.float32),
                          jnp.cumsum(uf, axis=1)], axis=1)
    t = jnp.arange(L)
    outs = []
    for g, w in enumerate(POOL_WINDOWS):
        lo = jnp.maximum(t + 1 - w, 0)
        cs_g = cs[:, :, g]
        win_sum = cs_g[:, 1:] - cs_g[:, lo]
        cnt = (t + 1 - lo).astype(jnp.float32)[None, :, None]
        outs.append(win_sum / cnt - uf[:, :, g])
    pooled = jnp.stack(outs, axis=2)
    mixed = jnp.einsum('blgc,gcd->blgd', pooled, pool_w.astype(jnp.float32))
    y = mixed.reshape(B, L, POOL_WIDTH) * pool_scale.astype(jnp.float32)
    return y.astype(u.dtype)


def gla_mixer(q, k, v, r, g_lr, w_gate_up, b_gate, gla_norm_w):
    B, L, _ = q.shape
    f32 = jnp.float32
    log_a = jax.nn.log_sigmoid(g_lr.astype(f32) @ w_gate_up.astype(f32)
                               + b_gate.astype(f32)) / GATE_NORMALIZER
    pad = CHUNK - N_META

    def heads(a, dh):
        a = jnp.pad(a.astype(f32), ((0, 0), (pad, 0), (0, 0)))
        return a.reshape(B, -1, CHUNK, GLA_HEADS, dh).transpose(0, 3, 1, 2, 4)

    qh = heads(q, GLA_HEAD_K) * (GLA_HEAD_K ** -0.5)
    kh = heads(k, GLA_HEAD_K)
    vh = heads(v, GLA_HEAD_V)
    gh = heads(log_a, GLA_HEAD_K)
    b = jnp.cumsum(gh, axis=3)
    b_last = b[:, :, :, -1:, :]
    q_t = qh * jnp.exp(b)
    k_t = kh * jnp.exp(-b)
    k_end = kh * jnp.exp(b_last - b)
    causal = jnp.tril(jnp.ones((CHUNK, CHUNK), dtype=bool))
    scores = jnp.where(causal, jnp.einsum('bhncd,bhnsd->bhncs', q_t, k_t), 0.0)
    o_intra = jnp.einsum('bhncs,bhnsv->bhncv', scores, vh)
    dS = jnp.einsum('bhncd,bhncv->bhndv', k_end, vh)
    decay = jnp.exp(b_last[:, :, :, 0, :])

    def step(S, inp):
        dS_n, dec_n = inp
        return dec_n[..., None] * S + dS_n, S

    S0 = jnp.zeros((B, GLA_HEADS, GLA_HEAD_K, GLA_HEAD_V), f32)
    _, S_start = lax.scan(step, S0, (jnp.moveaxis(dS, 2, 0), jnp.moveaxis(decay, 2, 0)))
    S_start = jnp.moveaxis(S_start, 0, 2)
    o_inter = jnp.einsum('bhncd,bhndv->bhncv', q_t, S_start)
    o = (o_intra + o_inter).transpose(0, 2, 3, 1, 4).reshape(B, -1, GLA_HEADS, GLA_HEAD_V)[:, pad:]
    o = o * lax.rsqrt(jnp.mean(o * o, axis=-1, keepdims=True) + EPS) * gla_norm_w.astype(f32)
    o = o.reshape(B, L, GLA_VDIM) * jax.nn.silu(r.astype(f32))
    return o.astype(q.dtype)


def hier_moe(x, w_rg, b_rg, w_re, b_re, w_gate, w_up, w_down):
    B, L, D = x.shape
    f32 = jnp.float32
    xt = x.reshape(-1, D)
    xf = xt.astype(f32)
    g_logits = xf @ w_rg.astype(f32) + b_rg.astype(f32)
    g_prob = jax.nn.softmax(g_logits, axis=-1)
    g_sel = jnp.argmax(g_logits, axis=-1)
    p_g = jnp.take_along_axis(g_prob, g_sel[:, None], axis=-1)
    e_logits = (xf @ w_re.astype(f32) + b_re.astype(f32)).reshape(-1, N_GROUPS, EXPERTS_PER_GROUP)
    e_logits = jnp.take_along_axis(e_logits, g_sel[:, None, None], axis=1)[:, 0]
    e_prob = jax.nn.softmax(e_logits, axis=-1)
    top_p, top_i = lax.top_k(e_prob, TOP_K_IN_GROUP)
    top_p = top_p / jnp.sum(top_p, axis=-1, keepdims=True)
    weights = p_g * top_p
    expert_id = g_sel[:, None] * EXPERTS_PER_GROUP + top_i
    combine = jnp.sum(jax.nn.one_hot(expert_id, N_EXPERTS, dtype=f32) * weights[..., None], axis=1)
    y = jnp.zeros(xf.shape, f32)
    for e in range(N_EXPERTS):
        h = jax.nn.silu(xt @ w_gate[e]) * (xt @ w_up[e])
        y = y + combine[:, e:e + 1] * (h @ w_down[e]).astype(f32)
    return y.reshape(B, L, D).astype(x.dtype)


def setup_inputs(seed: int = 0) -> dict:
    key = jax.random.key(seed)
    ks = jax.random.split(key, 20)
    n = jax.random.normal
    D = D_MODEL
    return {
        "x": n(ks[0], (BATCH, SEQ, D), jnp.float32),
        "meta_tokens": n(ks[1], (N_META, D), jnp.float32),
        "norm_mix_w": 1.0 + 0.05 * n(ks[2], (DEPTH, D), jnp.float32),
        "w_in": n(ks[3], (DEPTH, D, IN_COLS), jnp.float32) * D ** -0.5,
        "w_gate_up": n(ks[4], (DEPTH, GATE_RANK, GLA_KDIM), jnp.float32) * GATE_RANK ** -0.5,
        "b_gate": 0.1 * n(ks[5], (DEPTH, GLA_KDIM), jnp.float32),
        "gla_norm_w": 1.0 + 0.05 * n(ks[6], (DEPTH, GLA_HEAD_V), jnp.float32),
        "pool_w": n(ks[7], (DEPTH, N_POOL_GROUPS, POOL_GROUP, POOL_GROUP), jnp.float32) * POOL_GROUP ** -0.5,
        "pool_scale": 1.0 + 0.05 * n(ks[8], (DEPTH, POOL_WIDTH), jnp.float32),
        "w_out": n(ks[9], (DEPTH, MIX_WIDTH, D), jnp.float32) * MIX_WIDTH ** -0.5,
        "norm_ffn_w": 1.0 + 0.05 * n(ks[10], (DEPTH, D), jnp.float32),
        "w_router_group": n(ks[11], (DEPTH, D, N_GROUPS), jnp.float32) * D ** -0.5,
        "b_router_group": 0.01 * n(ks[12], (DEPTH, N_GROUPS), jnp.float32),
        "w_router_expert": n(ks[13], (DEPTH, D, N_EXPERTS), jnp.float32) * D ** -0.5,
        "b_router_expert": 0.01 * n(ks[14], (DEPTH, N_EXPERTS), jnp.float32),
        "w_expert_gate": n(ks[15], (DEPTH, N_EXPERTS, D, EXPERT_HIDDEN), jnp.float32) * D ** -0.5,
        "w_expert_up": n(ks[16], (DEPTH, N_EXPERTS, D, EXPERT_HIDDEN), jnp.float32) * D ** -0.5,
        "w_expert_down": n(ks[17], (DEPTH, N_EXPERTS, EXPERT_HIDDEN, D), jnp.float32) * EXPERT_HIDDEN ** -0.5,
        "final_norm_w": 1.0 + 0.05 * n(ks[18], (D,), jnp.float32),
    }


def reference(x, meta_tokens, norm_mix_w, w_in, w_gate_up, b_gate, gla_norm_w, pool_w,
              pool_scale, w_out, norm_ffn_w, w_router_group, b_router_group,
              w_router_expert, b_router_expert, w_expert_gate, w_expert_up,
              w_expert_down, final_norm_w):
    B = x.shape[0]
    meta = jnp.broadcast_to(meta_tokens[None].astype(x.dtype), (B, N_META, D_MODEL))
    h = jnp.concatenate([meta, x], axis=1)
    for l in range(DEPTH):
        u = rmsnorm(h, norm_mix_w[l])
        proj = u @ w_in[l]
        pool_v, q, k, v, r, g_lr = jnp.split(proj, SPLIT_POINTS, axis=-1)
        y_pool = pool_mixer(pool_v, pool_w[l], pool_scale[l])
        y_gla = gla_mixer(q, k, v, r, g_lr, w_gate_up[l], b_gate[l], gla_norm_w[l])
        h = h + jnp.concatenate([y_pool, y_gla], axis=-1) @ w_out[l]
        h = h + hier_moe(rmsnorm(h, norm_ffn_w[l]), w_router_group[l], b_router_group[l],
                         w_router_expert[l], b_router_expert[l], w_expert_gate[l],
                         w_expert_up[l], w_expert_down[l])
    return rmsnorm(h, final_norm_w)[:, N_META:]
```

```python
import numpy as np
from contextlib import ExitStack
import concourse.bass as bass
import concourse.mybir as mybir
from concourse.bass_utils import run_bass_kernel_spmd

F32 = mybir.dt.float32
BF16 = mybir.dt.bfloat16
AF = mybir.ActivationFunctionType
ALU = mybir.AluOpType
AX = mybir.AxisListType

D = 1024
NIN = 2064
NE = 16
EH = 256
EPS = 1e-6
BIG = 1.0e4
ENGS = ("pe", "act", "dve", "pool", "sp")
OVERLAP = False
MIX_WIDTH = 3
MIX_STAGGER = 2


class _Rec:
    def __init__(self):
        self.call = None

    def __getattr__(self, name):
        def f(*a, **k):
            self.call = (name, a, k)
            return self
        return f


class Prog:
    def __init__(self, nc, dma_sems=()):
        self.nc = nc
        self.ops = {e: [] for e in ENGS}
        self.cnt = {e: 0 for e in ENGS}
        for s in dma_sems:
            self.cnt[s] = 0
        self.seen = {e: {} for e in ENGS}
        self.lastw = {}
        self.readers = {}
        self.stopped = False
        import os
        self.debug = bool(os.environ.get("TRKDEBUG"))

    def op(self, eng, fn, reads=(), writes=(), sig=True, dma_sem=None):
        if self.stopped:
            return None
        waits = {}

        def need(tok):
            if tok is None:
                return
            s, v = tok
            if s == eng and eng == "pe":
                return
            if self.seen[eng].get(s, 0) >= v:
                return
            if waits.get(s, 0) < v:
                waits[s] = v

        for b in reads:
            need(self.lastw.get(b))
        for b in writes:
            need(self.lastw.get(b))
            for t in self.readers.get(b, ()):
                need(t)
        for s, v in waits.items():
            self.seen[eng][s] = v
        if dma_sem is not None:
            self.cnt[dma_sem] += 16
            tok = (dma_sem, self.cnt[dma_sem])
            inc = (dma_sem, 16)
        elif sig:
            self.cnt[eng] += 1
            tok = (eng, self.cnt[eng])
            inc = (eng, 1)
        else:
            tok = (eng, self.cnt[eng] + 1)
            inc = None
        for b in reads:
            self.readers.setdefault(b, []).append(tok)
        for b in writes:
            self.lastw[b] = tok
            self.readers[b] = []
        rec = _Rec()
        fn(rec)
        call = rec.call
        self.ops[eng].append((sorted(waits.items()), call, inc))
        if self.debug:
            import sys
            ln = sys._getframe(1).f_lineno
            print(f"OP {eng:4s} L{ln} waits={sorted(waits.items())} tok={tok} r={list(reads)} w={list(writes)}")
        return tok

    def finish(self, eng, toks):
        w = {}
        for tk in toks:
            if tk is None:
                continue
            s, v = tk
            w[s] = max(w.get(s, 0), v)
        for s in self.cnt:
            if self.cnt[s] > 0:
                w[s] = max(w.get(s, 0), self.cnt[s])
        self.ops[eng].append((sorted(w.items()), None, None))

    def emit(self, sems):
        nc = self.nc
        engobj = {"pe": "tensor", "act": "scalar", "dve": "vector", "pool": "gpsimd", "sp": "sync"}
        with nc.Block() as block:
            for e in ENGS:
                ops = self.ops[e]

                def body(eng, ops=ops):
                    for waits, fn, inc in ops:
                        for s, v in waits:
                            eng.wait_ge(sems[s], v)
                        if fn is None:
                            continue
                        ins = getattr(eng, fn[0])(*fn[1], **fn[2])
                        if inc is not None:
                            ins.then_inc(sems[inc[0]], inc[1])

                getattr(block, engobj[e])(body)


def build(NPRE, NBLK, STAGE=99):
    nc = bass.Bass("TRN2", target_bir_lowering=False)
    TP, TM = NPRE * 128, NBLK * 512
    dt_in = lambda name, shape: nc.dram_tensor(name, list(shape), F32, kind="ExternalInput").ap()
    xp_d = dt_in("xp", (TP, D))
    xm_d = dt_in("xm", (TM, D))
    pmask_d = dt_in("pmask", (128, NPRE))
    w_in_d = dt_in("w_in", (D, NIN))
    w_gu_d = dt_in("w_gu_b", (17, 256))
    gnw_d = dt_in("gnw", (128, 1))
    pool_w_d = dt_in("pool_w", (4, 128, 128))
    pscale_d = dt_in("pscale", (128, 4))
    w_out_d = dt_in("w_out", (D, D))
    nmw_d = dt_in("nmw_pk", (128, 8))
    nfw_d = dt_in("nfw_pk", (128, 8))
    fnw_d = dt_in("fnw", (D,))
    w_r_d = dt_in("w_r", (D, 20))
    b_r_d = dt_in("b_r", (20,))
    w_eg_d = dt_in("w_eg", (NE, D, EH))
    w_eu_d = dt_in("w_eu", (NE, D, EH))
    w_ed_d = dt_in("w_ed", (NE, EH, D))
    consts_d = dt_in("consts", (128, 8, 128))
    out_d = nc.dram_tensor("out", [TM, D], F32, kind="ExternalOutput").ap()
    wgu_s = nc.dram_tensor("wgu_scratch", [NE, 128, 8, 512], BF16, kind="Internal").ap()
    wd_s = nc.dram_tensor("wd_scratch", [NE, 128, 2, D], BF16, kind="Internal").ap()

    with ExitStack() as st:
        sb = lambda name, shape, dt: st.enter_context(nc.sbuf_tensor(name, list(shape), dt))
        w_in = sb("w_in_sb", (128, 8, NIN), BF16)
        w_out = sb("w_out_sb", (128, 8, D), BF16)
        pool_w = sb("pool_w_sb", (128, 4, 128), BF16)
        w_r = sb("w_r_sb", (128, 8, 20), F32)
        b_r = sb("b_r_sb", (128, 20), F32)
        w_gu = sb("w_gu_sb", (17, 256), F32)
        gnw = sb("gnw_sb", (128, 1), F32)
        pscale = sb("pscale_sb", (128, 4), F32)
        pmask = sb("pmask_sb", (128, NPRE), F32)
        fnw = sb("fnw_b", (128, D), F32)
        consts = sb("consts_sb", (128, 8, 128), F32)
        idb = sb("idb", (128, 128), BF16)
        NSLOT = 2
        wgu = [sb(f"wgu{i}", (128, 8, 512), BF16) for i in range(NSLOT)]
        wd = [sb(f"wd{i}", (128, 2, D), BF16) for i in range(NSLOT)]
        xb = [sb(f"x{i}", (128, 4, D), F32) for i in range(2)]
        x = xb[0]
        xn2T = sb("xn2T", (128, 8, 512), BF16)
        nmw_pk = sb("nmw_pk_sb", (128, 8), F32)
        nfw_pk = sb("nfw_pk_sb", (128, 8), F32)
        junk = sb("junk", (128, D), BF16)
        xn = [sb(f"xn{i}", (128, D), BF16) for i in range(2)]
        xnT = sb("xnT", (128, 8, 512), BF16)
        xn2f = sb("xn2f", (128, D), F32)
        xn2Tf = sb("xn2Tf", (128, 8, 128), F32)
        pvT = sb("pvT", (128, 4, 528), F32)
        pa = sb("pa", (128, 528), F32)
        pb = sb("pb", (128, 528), F32)
        pooledT = sb("pooledT", (128, 4, 512), BF16)
        ymT = sb("ymT", (128, 8, 512), BF16)
        qT = sb("qT", (128, 2, 512), BF16)
        kT = sb("kT", (128, 2, 512), BF16)
        srT = sb("srT", (128, 4, 512), BF16)
        g1 = sb("g1", (32, 512), F32)
        vtok = [sb(f"vtok{i}", (128, 512), BF16) for i in range(2)]
        sp = [sb(f"sp{i}", (128, 256), F32) for i in range(2)]
        eb = [sb(f"eb{i}", (128, 2, 128), F32) for i in range(2)]
        enb = [sb(f"enb{i}", (128, 2, 128), F32) for i in range(2)]
        eend = [sb(f"eend{i}", (128, 256), F32) for i in range(2)]
        qtX = [[sb(f"qt{c}{i}", (128, 2, 128), BF16) for c in "AB"] for i in range(2)]
        ktX = [[sb(f"kt{c}{i}", (128, 2, 128), BF16) for c in "AB"] for i in range(2)]
        kend = [sb(f"kend{i}", (128, 256), BF16) for i in range(2)]
        scT = sb("scT", (128, 4, 128), BF16)
        on = sb("on", (128, 512), BF16)
        S = sb("S", (128, 2, 128), F32)
        Sb = [sb(f"Sb{i}", (128, 2, 128), BF16) for i in range(2)]
        stat = sb("stat", (128, 4, 16), F32)
        stat4b = [sb(f"stat4{i}", (128, 16), F32) for i in range(2)]
        sg = sb("sg", (128, 2, 512), BF16)
        hT = [sb(f"hT{i}", (128, 2, 512), BF16) for i in range(2)]
        comb = sb("comb", (128, 4, 16), F32)
        rtb = [sb(f"rt{i}", (128, 128), F32) for i in range(2)]
        ps = st.enter_context(nc.psum_tensor("ps", [128, 7, 512], F32))

        dma_sems = ["ldc", "ldp"] + [f"ldx{p}{i}" for p in range(2) for i in range(4)] + [f"st{p}{i}" for p in range(2) for i in range(4)]
        for i in range(2):
            dma_sems += [f"wlg{i}", f"wld{i}", f"cvg{i}", f"cvu{i}", f"cvd{i}", f"csg{i}", f"csd{i}"]
        sems = {}
        for s in list(ENGS) + dma_sems:
            sems[s] = st.enter_context(nc.semaphore(s))
        P = Prog(nc, dma_sems=dma_sems)
        bankctr = [0]

        hard = [False]

        def stage(n):
            P.stopped = (STAGE < n) or hard[0]

        def nb():
            b = bankctr[0] % 7
            bankctr[0] += 1
            return b

        def pe_fence(b):
            P.op("pe", lambda e: e.matmul(ps[:, b, 508:512], lhsT=idb[0:1, :], rhs=idb[0:1, 0:4], start=True, stop=True),
                 reads=["idb"], writes=[f"ps{b}"])

        IDF = consts[:, 0, :]
        TRI_INC = consts[:, 1, :]
        TRI_STR = consts[:, 2, :]
        NEG16 = consts[:, 3, 0:1]
        CAUS4 = consts[:, 4:8, :]

        def ld(eng, out, in_, name, sem="ldc"):
            P.op(eng, lambda e: e.dma_start(out=out, in_=in_), writes=[name], dma_sem=sem)

        ld("sp", consts[:], consts_d, "consts")
        ld("sp", pmask[:], pmask_d, "pmask")
        ld("sp", w_gu[:], w_gu_d, "w_gu")
        ld("sp", gnw[:], gnw_d, "gnw")
        ld("sp", pscale[:], pscale_d, "pscale")
        ld("sp", nmw_pk[:], nmw_d, "nmw")
        ld("sp", nfw_pk[:], nfw_d, "nfw")
        ld("sp", fnw[:], fnw_d.partition_broadcast(128), "fnw")
        ld("sp", b_r[:], b_r_d.partition_broadcast(128), "b_r")
        ld("sp", w_r[:], w_r_d.rearrange("(kc p) n -> p kc n", p=128), "w_r")
        for kc in range(8):
            ld("pool", w_in[:, kc, :], w_in_d[kc * 128:(kc + 1) * 128, :], "w_in", "ldp")
        ld("pool", pool_w[:], pool_w_d.rearrange("g c d -> c g d"), "pool_w", "ldp")
        for kc in range(8):
            ld("pool", w_out[:, kc, :], w_out_d[kc * 128:(kc + 1) * 128, :], "w_out", "ldp")
        for name in ("consts", "pmask", "w_gu", "gnw", "pscale", "nmw", "nfw", "fnw", "b_r", "w_r"):
            P.lastw[name] = ("ldc", P.cnt["ldc"])
        for name in ("w_in", "pool_w", "w_out"):
            P.lastw[name] = ("ldp", P.cnt["ldp"])
        for kc in range(8):
            P.op("dve", lambda e: e.tensor_scalar(out=w_in[:, kc, :], in0=w_in[:, kc, :], scalar1=nmw_pk[:, kc:kc + 1],
                                                   scalar2=None, op0=ALU.mult), reads=["w_in", "nmw"], writes=["w_in"])
            P.op("dve", lambda e: e.tensor_scalar(out=w_r[:, kc, :], in0=w_r[:, kc, :], scalar1=nfw_pk[:, kc:kc + 1],
                                                  scalar2=None, op0=ALU.mult), reads=["w_r", "nfw"], writes=["w_r"])
        P.op("dve", lambda e: e.tensor_copy(out=idb[:], in_=IDF), reads=["consts"], writes=["idb"])
        P.op("dve", lambda e: e.memset(S[:], 0.0), writes=["S"])
        P.op("dve", lambda e: e.memset(g1[:], 1.0), writes=[f"g1_{i}" for i in range(4)])
        for p_ in range(2):
            for i_ in range(2):
                P.op("pool", lambda e: e.memset(qtX[p_][i_][:], 0.0), writes=[f"qt{p_}"])
                P.op("pool", lambda e: e.memset(ktX[p_][i_][:], 0.0), writes=[f"kt{p_}"])
        P.op("dve", lambda e: e.memset(pvT[:], 0.0), writes=["pvT"])

        cvt_tok = {}

        def convert_load(e_):
            s_ = e_ % NSLOT
            P.op("pool", lambda e: e.dma_start(out=wgu[s_][:, :, 0:256],
                                               in_=w_eg_d[e_].rearrange("(kc p) n -> p kc n", p=128)),
                 writes=[f"wgu{s_}a"], dma_sem=f"cvg{s_}")
            P.op("pool", lambda e: e.dma_start(out=wgu[s_][:, :, 256:512],
                                               in_=w_eu_d[e_].rearrange("(kc p) n -> p kc n", p=128)),
                 writes=[f"wgu{s_}b"], dma_sem=f"cvu{s_}")
            P.op("pool", lambda e: e.dma_start(out=wd[s_][:], in_=w_ed_d[e_].rearrange("(hc p) n -> p hc n", p=128)),
                 writes=[f"wd{s_}"], dma_sem=f"cvd{s_}")

        def convert_store(e_):
            s_ = e_ % NSLOT
            for kc in range(8):
                P.op("dve", lambda e: e.tensor_scalar(out=wgu[s_][:, kc, :], in0=wgu[s_][:, kc, :],
                                                      scalar1=nfw_pk[:, kc:kc + 1], scalar2=None, op0=ALU.mult),
                     reads=[f"wgu{s_}a", f"wgu{s_}b", "nfw"], writes=[f"wgu{s_}a", f"wgu{s_}b"])
            P.op("pool", lambda e: e.dma_start(out=wgu_s[e_], in_=wgu[s_][:]), reads=[f"wgu{s_}a", f"wgu{s_}b"],
                 writes=[f"wgus{e_}"], dma_sem=f"csg{s_}")
            P.op("pool", lambda e: e.dma_start(out=wd_s[e_], in_=wd[s_][:]), reads=[f"wd{s_}"],
                 writes=[f"wds{e_}"], dma_sem=f"csd{s_}")

        def run_streams(gens, width, stagger=1):
            it = iter(gens)
            active = []
            since = stagger
            done = False
            while True:
                if not done and len(active) < width and since >= stagger:
                    g = next(it, None)
                    if g is None:
                        done = True
                    else:
                        active.append(g)
                        since = 0
                if not active:
                    if done:
                        break
                    since = stagger
                    continue
                since += 1
                for g in list(reversed(active)):
                    try:
                        next(g)
                    except StopIteration:
                        active.remove(g)

        def rmsnorm(x_ap, xname, wb, wname, out_ap, oname, sidx):
            stn = f"stat{sidx}"
            P.op("dve", lambda e: e.memset(stat[:, sidx, 0:1], 0.0), writes=[stn])
            P.op("act", lambda e: e.activation(out=junk[:], in_=x_ap, func=AF.Square, accum_out=stat[:, sidx, 0:1]),
                 reads=[xname], writes=["junk", stn])
            P.op("act", lambda e: e.activation(out=stat[:, sidx, 1:2], in_=stat[:, sidx, 0:1], func=AF.Ln,
                                               scale=1.0 / D, bias=EPS), reads=[stn], writes=[stn])
            P.op("act", lambda e: e.activation(out=stat[:, sidx, 2:3], in_=stat[:, sidx, 1:2], func=AF.Exp, scale=-0.5),
                 reads=[stn], writes=[stn])
            if wb is None:
                P.op("dve", lambda e: e.tensor_scalar(out=out_ap, in0=x_ap, scalar1=stat[:, sidx, 2:3], scalar2=None,
                                                      op0=ALU.mult), reads=[xname, stn], writes=[oname])
            else:
                P.op("dve", lambda e: e.scalar_tensor_tensor(out=out_ap, in0=x_ap, scalar=stat[:, sidx, 2:3], in1=wb[:],
                                                             op0=ALU.mult, op1=ALU.mult),
                     reads=[xname, stn, wname], writes=[oname])

        def transpose_bf(src, sname, dst3, dname):
            for half in range(2):
                b = nb()
                for c in range(4):
                    kc = half * 4 + c
                    P.op("pe", lambda e: e.matmul(ps[:, b, c * 128:(c + 1) * 128],
                                                  lhsT=src[:, kc * 128:(kc + 1) * 128], rhs=idb[:], start=True, stop=True),
                         reads=[sname, "idb"], writes=[f"ps{b}"], sig=(c == 3))
                P.op("dve", lambda e: e.tensor_copy(
                    out=dst3[:, half * 4:(half + 1) * 4, :], in_=ps[:, b, :].rearrange("p (c n) -> p c n", c=4)),
                    reads=[f"ps{b}"], writes=[dname])

        def kv_tokmajor(c0, xtn):
            bk = nb()
            for kc in range(8):
                P.op("pe", lambda e: e.matmul(ps[:, bk, 0:256], lhsT=xnT[:, kc, c0:c0 + 128],
                                              rhs=w_in[:, kc, 768:1024], start=(kc == 0), stop=(kc == 7)),
                     reads=[xtn, "w_in"], writes=[f"ps{bk}"], sig=(kc == 7))
            bv = nb()
            for kc in range(8):
                P.op("pe", lambda e: e.matmul(ps[:, bv, 0:512], lhsT=xnT[:, kc, c0:c0 + 128],
                                              rhs=w_in[:, kc, 1024:1536], start=(kc == 0), stop=(kc == 7)),
                     reads=[xtn, "w_in"], writes=[f"ps{bv}"], sig=(kc == 7))
            return bk, bv

        def softplus_neg(gc0, g1name, par, mask_col=None):
            bl = nb()
            P.op("pe", lambda e: e.matmul(ps[:, bl, 0:256], lhsT=g1[0:17, gc0:gc0 + 128], rhs=w_gu[:, :],
                                          start=True, stop=True), reads=[g1name, "w_gu"], writes=[f"ps{bl}"], sig=False)
            pe_fence(bl)
            P.op("act", lambda e: e.activation(out=sp[par][:], in_=ps[:, bl, 0:256], func=AF.Exp, scale=-1.0),
                 reads=[f"ps{bl}"], writes=[f"sp{par}"])
            P.op("act", lambda e: e.activation(out=sp[par][:], in_=sp[par][:], func=AF.Ln, bias=1.0),
                 reads=[f"sp{par}"], writes=[f"sp{par}"])
            if mask_col is not None:
                P.op("dve", lambda e: e.tensor_scalar(out=sp[par][:], in0=sp[par][:], scalar1=pmask[:, mask_col:mask_col + 1],
                                                      scalar2=None, op0=ALU.mult),
                     reads=[f"sp{par}", "pmask"], writes=[f"sp{par}"])

        def kend_and_v(bk, bv, par):
            be = nb()
            P.op("pe", lambda e: e.matmul(ps[:, be, 0:256], lhsT=TRI_STR, rhs=sp[par][:], start=True, stop=True),
                 reads=["consts", f"sp{par}"], writes=[f"ps{be}"], sig=False)
            pe_fence(be)
            P.op("act", lambda e: e.activation(out=eend[par][:], in_=ps[:, be, 0:256], func=AF.Exp),
                 reads=[f"ps{be}"], writes=[f"eend{par}"])
            P.op("dve", lambda e: e.tensor_tensor(out=kend[par][:], in0=ps[:, bk, 0:256], in1=eend[par][:], op=ALU.mult),
                 reads=[f"ps{bk}", f"eend{par}"], writes=[f"kend{par}"])
            P.op("dve", lambda e: e.tensor_copy(out=vtok[par][:], in_=ps[:, bv, 0:512]),
                 reads=[f"ps{bv}"], writes=[f"vtok{par}"])

        def cum_decay(par, want_neg):
            bb = nb()
            for j in range(2):
                P.op("pe", lambda e: e.matmul(ps[:, bb, j * 128:(j + 1) * 128], lhsT=sp[par][:, j * 128:(j + 1) * 128],
                                              rhs=TRI_INC, start=True, stop=True),
                     reads=[f"sp{par}", "consts"], writes=[f"ps{bb}"], sig=False)
            pe_fence(bb)
            P.op("act", lambda e: e.activation(out=eb[par][:], in_=ps[:, bb, 0:256].rearrange("p (j n) -> p j n", j=2),
                                               func=AF.Exp), reads=[f"ps{bb}"], writes=[f"eb{par}"])
            if want_neg:
                P.op("act", lambda e: e.activation(out=enb[par][:], in_=ps[:, bb, 0:256].rearrange("p (j n) -> p j n", j=2),
                                                   func=AF.Exp, scale=-1.0), reads=[f"ps{bb}"], writes=[f"enb{par}"])

        def state_update(par):
            bs = nb()
            for j in range(2):
                P.op("pe", lambda e: e.matmul(ps[:, bs, j * 256:(j + 1) * 256], lhsT=kend[par][:, j * 128:(j + 1) * 128],
                                              rhs=vtok[par][:, j * 256:(j + 1) * 256], start=True, stop=True),
                     reads=[f"kend{par}", f"vtok{par}"], writes=[f"ps{bs}"], sig=(j == 1))
            for j in range(2):
                for hh in range(2):
                    r0 = hh * 64
                    P.op("dve", lambda e: e.scalar_tensor_tensor(
                        out=S[r0:r0 + 64, j, :], in0=S[r0:r0 + 64, j, :], scalar=eb[par][r0:r0 + 64, j, 127:128],
                        in1=ps[r0:r0 + 64, bs, j * 256 + hh * 128:j * 256 + hh * 128 + 128],
                        op0=ALU.mult, op1=ALU.add), reads=["S", f"eb{par}", f"ps{bs}"], writes=["S"])

        def prefix_tile(i):
            if i % 2 == 0 and i // 2 < NE:
                convert_load(i // 2)
            if i % 2 == 1 and i // 2 < NE:
                convert_store(i // 2)
            slot = i % 4
            par = i % 2
            c0 = slot * 128
            P.op("sp", lambda e: e.dma_start(out=x[:, slot, :], in_=xp_d[i * 128:(i + 1) * 128, :]),
                 writes=[f"x0{slot}"], dma_sem=f"ldx0{slot}")
            rmsnorm(x[:, slot, :], f"x0{slot}", None, None, xn[par][:], f"xn{par}", slot)
            yield
            transpose_bf(xn[par], f"xn{par}", xnT[:, :, c0:c0 + 128], f"xnT{slot}")
            yield
            bk, bv = kv_tokmajor(c0, f"xnT{slot}")
            bg = nb()
            for kc in range(8):
                P.op("pe", lambda e: e.matmul(ps[0:16, bg, 0:128], lhsT=w_in[:, kc, 2048:2064],
                                              rhs=xnT[:, kc, c0:c0 + 128], start=(kc == 0), stop=(kc == 7)),
                     reads=[f"xnT{slot}", "w_in"], writes=[f"ps{bg}"], sig=(kc == 7))
            P.op("act", lambda e: e.activation(out=g1[0:16, c0:c0 + 128], in_=ps[0:16, bg, 0:128], func=AF.Copy),
                 reads=[f"ps{bg}"], writes=[f"g1_{slot}"])
            softplus_neg(c0, f"g1_{slot}", par, mask_col=i)
            kend_and_v(bk, bv, par)
            cum_decay(par, False)
            state_update(par)
            if i == NPRE - 1:
                bh = nb()
                for g in range(4):
                    for kc in range(8):
                        P.op("pe", lambda e: e.matmul(ps[:, bh, g * 16:(g + 1) * 16],
                                                      lhsT=w_in[:, kc, g * 128:(g + 1) * 128],
                                                      rhs=xnT[:, kc, c0 + 112:c0 + 128],
                                                      start=(kc == 0), stop=(kc == 7)),
                             reads=[f"xnT{slot}", "w_in"], writes=[f"ps{bh}"], sig=(g == 3 and kc == 7))
                P.op("act", lambda e: e.activation(out=pvT[:, :, 0:16],
                                                   in_=ps[:, bh, 0:64].rearrange("p (g n) -> p g n", g=4), func=AF.Copy),
                     reads=[f"ps{bh}"], writes=["pvT"])

        run_streams([prefix_tile(i) for i in range(NPRE)], 3, 1)
        for e_ in range(NE):
            if e_ == NPRE // 2 and NPRE % 2 == 1:
                convert_store(e_)
            elif e_ >= (NPRE + 1) // 2:
                convert_load(e_)
                convert_store(e_)
        P.op("act", lambda e: e.activation(out=Sb[0][:], in_=S[:], func=AF.Copy), reads=["S"], writes=["Sb0"])

        XT = [f"xnT{t}" for t in range(4)]
        G1ALL = [f"g1_{t}" for t in range(4)]
        out_toks = []

        def load_expert(ge):
            e_ = ge % NE
            s_ = ge % NSLOT
            P.op("sp", lambda e: e.dma_start(out=wgu[s_][:], in_=wgu_s[e_]), reads=[f"wgus{e_}"],
                 writes=[f"wgu{s_}a", f"wgu{s_}b"], dma_sem=f"wlg{s_}")
            P.op("sp", lambda e: e.dma_start(out=wd[s_][:], in_=wd_s[e_]), reads=[f"wds{e_}"],
                 writes=[f"wd{s_}"], dma_sem=f"wld{s_}")

        NGE = NBLK * NE
        load_expert(0)

        def front_tile(blk, t):
            r0 = blk * 512 + t * 128
            par = t % 2
            pb_ = blk % 2
            x = xb[pb_]
            P.op("sp", lambda e: e.dma_start(out=x[:, t, :], in_=xm_d[r0:r0 + 128, :]),
                 writes=[f"x{pb_}{t}"], dma_sem=f"ldx{pb_}{t}")
            rmsnorm(x[:, t, :], f"x{pb_}{t}", None, None, xn[par][:], f"xn{par}", t)
            yield
            transpose_bf(xn[par], f"xn{par}", xnT[:, :, t * 128:(t + 1) * 128], f"xnT{t}")

        def proj_chunk(col0, m):
            b = nb()
            for kc in range(8):
                P.op("pe", lambda e: e.matmul(ps[0:m, b, :], lhsT=w_in[:, kc, col0:col0 + m], rhs=xnT[:, kc, :],
                                              start=(kc == 0), stop=(kc == 7)),
                     reads=XT + ["w_in"], writes=[f"ps{b}"], sig=(kc == 7))
            return b

        def block_front(blk):
            fts = [front_tile(blk, t) for t in range(4)]
            next(fts[0])
            yield
            for t in range(4):
                if t + 1 < 4:
                    next(fts[t + 1])
                    yield
                for _ in fts[t]:
                    pass
                yield
            for g in range(4):
                b = proj_chunk(g * 128, 128)
                P.op("act", lambda e: e.activation(out=pvT[:, g, 16:528], in_=ps[:, b, :], func=AF.Copy),
                     reads=[f"ps{b}"], writes=["pvT"])
                yield
            for j in range(2):
                b = proj_chunk(512 + j * 128, 128)
                P.op("act", lambda e: e.activation(out=qT[:, j, :], in_=ps[:, b, :], func=AF.Copy),
                     reads=[f"ps{b}"], writes=["qT"])
                yield
            for j in range(2):
                b = proj_chunk(768 + j * 128, 128)
                P.op("act", lambda e: e.activation(out=kT[:, j, :], in_=ps[:, b, :], func=AF.Copy),
                     reads=[f"ps{b}"], writes=["kT"])
                yield
            b = proj_chunk(2048, 16)
            P.op("act", lambda e: e.activation(out=g1[0:16, :], in_=ps[0:16, b, :], func=AF.Copy),
                 reads=[f"ps{b}"], writes=G1ALL)
            for h in range(4):
                b = proj_chunk(1536 + h * 128, 128)
                P.op("act", lambda e: e.activation(out=srT[:, h, :], in_=ps[:, b, :], func=AF.Silu),
                     reads=[f"ps{b}"], writes=["srT"])
                yield
            for g in range(4):
                p_ = pvT[:, g, :]
                w_ = 2 << g
                P.op("pool", lambda e: e.tensor_tensor(out=pa[:, 1:528], in0=p_[:, 1:528], in1=p_[:, 0:527], op=ALU.add),
                     reads=["pvT"], writes=["pa"])
                cur, curname = pa, "pa"
                if g >= 1:
                    P.op("pool", lambda e: e.tensor_tensor(out=pb[:, 3:528], in0=pa[:, 3:528], in1=pa[:, 1:526], op=ALU.add),
                         reads=["pa"], writes=["pb"])
                    cur, curname = pb, "pb"
                if g >= 2:
                    P.op("pool", lambda e: e.tensor_tensor(out=pa[:, 7:528], in0=pb[:, 7:528], in1=pb[:, 3:524], op=ALU.add),
                         reads=["pb"], writes=["pa"])
                    cur, curname = pa, "pa"
                if g >= 3:
                    P.op("pool", lambda e: e.tensor_tensor(out=pb[:, 15:528], in0=pa[:, 15:528], in1=pa[:, 7:520], op=ALU.add),
                         reads=["pa"], writes=["pb"])
                    cur, curname = pb, "pb"
                P.op("dve", lambda e: e.scalar_tensor_tensor(
                    out=pooledT[:, g, :], in0=cur[:, 16:528], scalar=1.0 / w_, in1=p_[:, 16:528],
                    op0=ALU.mult, op1=ALU.subtract), reads=[curname, "pvT"], writes=["pooledT"])
                yield
            P.op("pool", lambda e: e.tensor_copy(out=pvT[:, :, 0:16], in_=pvT[:, :, 512:528]), reads=["pvT"], writes=["pvT"])
            for g in range(4):
                b = nb()
                P.op("pe", lambda e: e.matmul(ps[:, b, :], lhsT=pool_w[:, g, :], rhs=pooledT[:, g, :], start=True, stop=True),
                     reads=["pool_w", "pooledT"], writes=[f"ps{b}"])
                P.op("act", lambda e: e.activation(out=ymT[:, g, :], in_=ps[:, b, :], func=AF.Copy, scale=pscale[:, g:g + 1]),
                     reads=[f"ps{b}", "pscale"], writes=["ymTp"])
            yield

        tile_ctr = [0]

        def main_tile(blk, t):
            c0 = t * 128
            par = tile_ctr[0] % 2
            tile_ctr[0] += 1
            stat4 = stat4b[par]
            st4n = f"stat4{par}"
            pb_ = blk % 2
            x = xb[pb_]
            xn_ = f"x{pb_}{t}"
            bk, bv = kv_tokmajor(c0, f"xnT{t}")
            softplus_neg(c0, f"g1_{t}", par)
            kend_and_v(bk, bv, par)
            cum_decay(par, True)
            for i_ in range(2):
                r0 = i_ * 64
                P.op("dve", lambda e: e.scalar_tensor_tensor(out=qtX[par][i_][r0:r0 + 64], in0=qT[r0:r0 + 64, :, c0:c0 + 128],
                                                             scalar=0.125, in1=eb[par][r0:r0 + 64], op0=ALU.mult, op1=ALU.mult),
                     reads=["qT", f"eb{par}"], writes=[f"qt{par}"])
                P.op("dve", lambda e: e.tensor_tensor(out=ktX[par][i_][r0:r0 + 64], in0=kT[r0:r0 + 64, :, c0:c0 + 128],
                                                      in1=enb[par][r0:r0 + 64], op=ALU.mult),
                     reads=["kT", f"enb{par}"], writes=[f"kt{par}"])
            yield
            bsc = nb()
            for h in range(4):
                j = h // 2
                P.op("pe", lambda e: e.matmul(ps[:, bsc, h * 128:(h + 1) * 128], lhsT=ktX[par][h % 2][:, j, :],
                                              rhs=qtX[par][h % 2][:, j, :], start=True, stop=True),
                     reads=[f"kt{par}", f"qt{par}"], writes=[f"ps{bsc}"], sig=(h == 3))
            P.op("dve", lambda e: e.tensor_tensor(out=scT[:], in0=ps[:, bsc, :].rearrange("p (h n) -> p h n", h=4),
                                                  in1=CAUS4, op=ALU.mult),
                 reads=[f"ps{bsc}", "consts"], writes=["scT"])
            yield
            bo = nb()
            for h in range(4):
                j = h // 2
                P.op("pe", lambda e: e.matmul(ps[:, bo, h * 128:(h + 1) * 128], lhsT=scT[:, h, :],
                                              rhs=vtok[par][:, h * 128:(h + 1) * 128], start=True, stop=False),
                     reads=["scT", f"vtok{par}"], writes=[f"ps{bo}"], sig=False)
                P.op("pe", lambda e: e.matmul(ps[:, bo, h * 128:(h + 1) * 128], lhsT=qtX[par][h % 2][:, j, :],
                                              rhs=Sb[par][:, j, :], start=False, stop=True),
                     reads=[f"qt{par}", f"Sb{par}"], writes=[f"ps{bo}"], sig=(h == 3))
            state_update(par)
            P.op("act", lambda e: e.activation(out=Sb[1 - par][:], in_=S[:], func=AF.Copy),
                 reads=["S"], writes=[f"Sb{1 - par}"])
            P.op("dve", lambda e: e.memset(stat4[:, 0:4], 0.0), writes=[st4n])
            for h in range(4):
                P.op("act", lambda e: e.activation(out=junk[:, 0:128], in_=ps[:, bo, h * 128:(h + 1) * 128],
                                                   func=AF.Square, accum_out=stat4[:, h:h + 1]),
                     reads=[f"ps{bo}"], writes=["junk", st4n])
            P.op("act", lambda e: e.activation(out=stat4[:, 4:8], in_=stat4[:, 0:4], func=AF.Ln, scale=1.0 / 128,
                                               bias=EPS), reads=[st4n], writes=[st4n])
            P.op("act", lambda e: e.activation(out=stat4[:, 8:12], in_=stat4[:, 4:8], func=AF.Exp, scale=-0.5),
                 reads=[st4n], writes=[st4n])
            for h in range(4):
                P.op("act", lambda e: e.activation(out=on[:, h * 128:(h + 1) * 128], in_=ps[:, bo, h * 128:(h + 1) * 128],
                                                   func=AF.Copy, scale=stat4[:, 8 + h:9 + h]),
                     reads=[f"ps{bo}", st4n], writes=["on"])
            yield
            bt = nb()
            for h in range(4):
                P.op("pe", lambda e: e.matmul(ps[:, bt, h * 128:(h + 1) * 128], lhsT=on[:, h * 128:(h + 1) * 128],
                                              rhs=idb[:], start=True, stop=True),
                     reads=["on", "idb"], writes=[f"ps{bt}"], sig=(h == 3))
            P.op("dve", lambda e: e.scalar_tensor_tensor(out=ymT[:, 4:8, c0:c0 + 128],
                                                         in0=ps[:, bt, :].rearrange("p (h n) -> p h n", h=4),
                                                         scalar=gnw[:, 0:1], in1=srT[:, :, c0:c0 + 128],
                                                         op0=ALU.mult, op1=ALU.mult),
                 reads=[f"ps{bt}", "gnw", "srT"], writes=[f"ymTg{t}"])
            yield
            for half in range(2):
                b = nb()
                for kc in range(8):
                    P.op("pe", lambda e: e.matmul(
                        ps[:, b, :], lhsT=ymT[:, kc, c0:c0 + 128], rhs=w_out[:, kc, half * 512:(half + 1) * 512],
                        start=(kc == 0), stop=(kc == 7)), reads=["ymTp", f"ymTg{t}", "w_out"], writes=[f"ps{b}"],
                        sig=(kc == 7))
                P.op("dve", lambda e: e.tensor_tensor(
                    out=x[:, t, half * 512:(half + 1) * 512], in0=ps[:, b, :], in1=x[:, t, half * 512:(half + 1) * 512],
                    op=ALU.add), reads=[f"ps{b}", xn_], writes=[xn_])
            if not OVERLAP:
                yield
                for _ in main_tile_b(blk, t):
                    yield

        def main_tile_b(blk, t):
            c0 = t * 128
            par = t % 2
            rt = rtb[par]
            rtn = f"rt{par}"
            pb_ = blk % 2
            x = xb[pb_]
            rmsnorm(x[:, t, :], f"x{pb_}{t}", None, None, xn2f[:], "xn2f", t)
            yield
            for half in range(2):
                b = nb()
                for c in range(4):
                    kc = half * 4 + c
                    P.op("pe", lambda e: e.matmul(ps[:, b, c * 128:(c + 1) * 128],
                                                  lhsT=xn2f[:, kc * 128:(kc + 1) * 128], rhs=IDF, start=True, stop=True),
                         reads=["xn2f", "consts"], writes=[f"ps{b}"], sig=(c == 3))
                P.op("dve", lambda e: e.tensor_copy(
                    out=xn2Tf[:, half * 4:(half + 1) * 4, :], in_=ps[:, b, :].rearrange("p (c n) -> p c n", c=4)),
                    reads=[f"ps{b}"], writes=["xn2Tf"])
                P.op("act", lambda e: e.activation(
                    out=xn2T[:, half * 4:(half + 1) * 4, c0:c0 + 128], in_=xn2Tf[:, half * 4:(half + 1) * 4, :],
                    func=AF.Copy), reads=["xn2Tf"], writes=[f"xn2T{t}"])
            yield
            br = nb()
            for kc in range(8):
                P.op("pe", lambda e: e.matmul(ps[:, br, 0:20], lhsT=xn2Tf[:, kc, :], rhs=w_r[:, kc, :],
                                              start=(kc == 0), stop=(kc == 7)),
                     reads=["xn2Tf", "w_r"], writes=[f"ps{br}"], sig=False)
            pe_fence(br)
            V = lambda fn: P.op("dve", fn, reads=[rtn], writes=[rtn])
            P.op("dve", lambda e: e.tensor_tensor(out=rt[:, 0:20], in0=ps[:, br, 0:20], in1=b_r[:], op=ALU.add),
                 reads=[f"ps{br}", "b_r"], writes=[rtn])
            V(lambda e: e.tensor_reduce(out=rt[:, 20:21], in_=rt[:, 0:4], axis=AX.X, op=ALU.max))
            V(lambda e: e.tensor_scalar(out=rt[:, 24:28], in0=rt[:, 0:4], scalar1=rt[:, 20:21], scalar2=None, op0=ALU.is_equal))
            V(lambda e: e.tensor_scalar(out=rt[:, 21:22], in0=rt[:, 20:21], scalar1=-1.0, scalar2=None, op0=ALU.mult))
            P.op("dve", lambda e: e.memset(rt[:, 22:23], 0.0), reads=[rtn], writes=[rtn])
            P.op("act", lambda e: e.activation(out=rt[:, 28:32], in_=rt[:, 0:4], func=AF.Exp, bias=rt[:, 21:22],
                                               accum_out=rt[:, 22:23]), reads=[rtn], writes=[rtn])
            V(lambda e: e.reciprocal(out=rt[:, 23:24], in_=rt[:, 22:23]))
            V(lambda e: e.tensor_scalar(out=rt[:, 32:36], in0=rt[:, 24:28], scalar1=BIG, scalar2=-BIG, op0=ALU.mult, op1=ALU.add))
            for g in range(4):
                V(lambda e: e.tensor_scalar(out=rt[:, 40 + 4 * g:44 + 4 * g], in0=rt[:, 4 + 4 * g:8 + 4 * g],
                                            scalar1=rt[:, 32 + g:33 + g], scalar2=None, op0=ALU.add))
            V(lambda e: e.tensor_reduce(out=rt[:, 36:37], in_=rt[:, 40:56], axis=AX.X, op=ALU.max))
            V(lambda e: e.tensor_scalar(out=rt[:, 56:72], in0=rt[:, 40:56], scalar1=rt[:, 36:37], scalar2=None, op0=ALU.is_equal))
            V(lambda e: e.scalar_tensor_tensor(out=rt[:, 72:88], in0=rt[:, 56:72], scalar=-BIG, in1=rt[:, 40:56],
                                               op0=ALU.mult, op1=ALU.add))
            V(lambda e: e.tensor_reduce(out=rt[:, 37:38], in_=rt[:, 72:88], axis=AX.X, op=ALU.max))
            V(lambda e: e.tensor_scalar(out=rt[:, 88:104], in0=rt[:, 72:88], scalar1=rt[:, 37:38], scalar2=None, op0=ALU.is_equal))
            V(lambda e: e.tensor_tensor(out=rt[:, 38:39], in0=rt[:, 37:38], in1=rt[:, 36:37], op=ALU.subtract))
            P.op("act", lambda e: e.activation(out=rt[:, 39:40], in_=rt[:, 38:39], func=AF.Exp), reads=[rtn], writes=[rtn])
            V(lambda e: e.tensor_scalar(out=rt[:, 104:105], in0=rt[:, 39:40], scalar1=1.0, scalar2=None, op0=ALU.add))
            V(lambda e: e.reciprocal(out=rt[:, 105:106], in_=rt[:, 104:105]))
            V(lambda e: e.tensor_tensor(out=rt[:, 106:107], in0=rt[:, 105:106], in1=rt[:, 23:24], op=ALU.mult))
            V(lambda e: e.tensor_tensor(out=rt[:, 107:108], in0=rt[:, 23:24], in1=rt[:, 106:107], op=ALU.subtract))
            V(lambda e: e.tensor_scalar(out=rt[:, 108:124], in0=rt[:, 56:72], scalar1=rt[:, 106:107], scalar2=None, op0=ALU.mult))
            P.op("dve", lambda e: e.scalar_tensor_tensor(out=comb[:, t, :], in0=rt[:, 88:104], scalar=rt[:, 107:108],
                                                         in1=rt[:, 108:124], op0=ALU.mult, op1=ALU.add),
                 reads=[rtn], writes=[f"comb{t}"])

        X2T = [f"xn2T{t}" for t in range(4)]

        def experts(blk):
            pb_ = blk % 2
            x = xb[pb_]
            for e_ in range(NE):
                ge = blk * NE + e_
                s_ = ge % NSLOT
                if ge + 1 < NGE:
                    load_expert(ge + 1)
                hp = ge % 2
                for hc in range(2):
                    bb2 = []
                    for gu in range(2):
                        b = nb()
                        bb2.append(b)
                        col = gu * 256 + hc * 128
                        for kc in range(8):
                            P.op("pe", lambda e: e.matmul(
                                ps[:, b, :], lhsT=wgu[s_][:, kc, col:col + 128], rhs=xn2T[:, kc, :],
                                start=(kc == 0), stop=(kc == 7)), reads=X2T + [f"wgu{s_}" + "ab"[gu]], writes=[f"ps{b}"],
                                sig=(kc == 7))
                    bg_, bu_ = bb2
                    P.op("act", lambda e: e.activation(out=sg[:, hc, :], in_=ps[:, bg_, :], func=AF.Silu),
                         reads=[f"ps{bg_}"], writes=[f"sg{hc}"])
                    P.op("dve", lambda e: e.tensor_tensor(out=hT[hp][:, hc, :], in0=ps[:, bu_, :], in1=sg[:, hc, :], op=ALU.mult),
                         reads=[f"ps{bu_}", f"sg{hc}"], writes=[f"hT{hp}"])
                for t in range(4):
                    for half in range(2):
                        b = nb()
                        for hc in range(2):
                            P.op("pe", lambda e: e.matmul(
                                ps[:, b, :], lhsT=hT[hp][:, hc, t * 128:(t + 1) * 128],
                                rhs=wd[s_][:, hc, half * 512:(half + 1) * 512], start=(hc == 0), stop=(hc == 1)),
                                reads=[f"hT{hp}", f"wd{s_}"], writes=[f"ps{b}"], sig=(hc == 1))
                        P.op("dve", lambda e: e.scalar_tensor_tensor(
                            out=x[:, t, half * 512:(half + 1) * 512], in0=ps[:, b, :], scalar=comb[:, t, e_:e_ + 1],
                            in1=x[:, t, half * 512:(half + 1) * 512], op0=ALU.mult, op1=ALU.add),
                            reads=[f"ps{b}", f"comb{t}", f"x{pb_}{t}"], writes=[f"x{pb_}{t}"])
                yield

        def final_tile(blk, t):
            pb_ = blk % 2
            x = xb[pb_]
            r0 = blk * 512 + t * 128
            rmsnorm(x[:, t, :], f"x{pb_}{t}", fnw, "fnw", x[:, t, :], f"x{pb_}{t}", t)
            out_toks.append(P.op("sp", lambda e: e.dma_start(out=out_d[r0:r0 + 128, :], in_=x[:, t, :]),
                                 reads=[f"x{pb_}{t}"], dma_sem=f"st{pb_}{t}"))

        def final(blk):
            for t in range(4):
                final_tile(blk, t)

        def mixer1(blk):
            for _ in block_front(blk):
                yield
            gens = [main_tile(blk, t) for t in range(4)]
            active, nxt, since = [], 0, MIX_STAGGER
            while active or nxt < 4:
                if nxt < 4 and len(active) < MIX_WIDTH and since >= MIX_STAGGER:
                    active.append(gens[nxt])
                    nxt += 1
                    since = 0
                since += 1
                for g in list(reversed(active)):
                    try:
                        next(g)
                    except StopIteration:
                        active.remove(g)
                yield

        def part2(blk):
            if OVERLAP:
                run_streams([main_tile_b(blk, t) for t in range(4)], 2, 2)

        if OVERLAP:
            for _ in mixer1(0):
                pass
            part2(0)
            for blk in range(NBLK):
                mg = mixer1(blk + 1) if blk + 1 < NBLK else None
                for _ in experts(blk):
                    if mg is not None:
                        for _k in range(4):
                            if next(mg, "done") == "done":
                                mg = None
                                break
                final(blk)
                if mg is not None:
                    for _ in mg:
                        pass
                if blk + 1 < NBLK:
                    part2(blk + 1)
        else:
            mg = mixer1(0)
            for blk in range(NBLK):
                for _ in mg:
                    pass
                part2(blk)
                for _ in experts(blk):
                    pass
                mg = mixer1(blk + 1) if blk + 1 < NBLK else iter(())
                for t in range(4):
                    final_tile(blk, t)
                    next(mg, None)
                    next(mg, None)
        P.finish("sp", out_toks)
        P.emit(sems)
    return nc


def make_consts():
    c = np.zeros((128, 8, 128), np.float32)
    s = np.arange(128)[:, None]
    cc = np.arange(128)[None, :]
    c[:, 0] = np.eye(128, dtype=np.float32)
    c[:, 1] = np.where(s <= cc, -1.0 / 16, 0.0)
    c[:, 2] = np.where(s > cc, -1.0 / 16, 0.0)
    c[:, 3] = -1.0 / 16
    for h in range(4):
        c[:, 4 + h] = (s <= cc).astype(np.float32)
    return c


def make_in_maps(inp, seq):
    B = inp["x"].shape[0]
    half_len = seq // 2
    NPRE = half_len // 128 + 1
    f = lambda a: np.ascontiguousarray(np.asarray(a, dtype=np.float32))
    x = f(inp["x"])
    meta = f(inp["meta_tokens"])
    shared = {
        "w_in": f(inp["w_in"][0]),
        "w_gu_b": f(np.concatenate([inp["w_gate_up"][0], inp["b_gate"][0][None, :]], axis=0)),
        "gnw": f(inp["gla_norm_w"][0].reshape(128, 1)),
        "pool_w": f(inp["pool_w"][0]),
        "pscale": f(inp["pool_scale"][0].reshape(4, 128).T),
        "w_out": f(inp["w_out"][0]),
        "nmw_pk": f(inp["norm_mix_w"][0].reshape(8, 128).T),
        "nfw_pk": f(inp["norm_ffn_w"][0].reshape(8, 128).T),
        "fnw": f(inp["final_norm_w"]),
        "w_r": f(np.concatenate([inp["w_router_group"][0], inp["w_router_expert"][0]], axis=1)),
        "b_r": f(np.concatenate([inp["b_router_group"][0], inp["b_router_expert"][0]], axis=0)),
        "w_eg": f(inp["w_expert_gate"][0]),
        "w_eu": f(inp["w_expert_up"][0]),
        "w_ed": f(inp["w_expert_down"][0]),
        "consts": make_consts(),
    }
    maps = []
    for b in range(B):
        for half in range(2):
            xp = np.zeros((NPRE * 128, D), np.float32)
            mask = np.zeros((NPRE * 128,), np.float32)
            if half == 0:
                xp[-16:] = meta
                mask[-16:] = 1.0
            else:
                xp[112:128] = meta
                xp[128:] = x[b, :half_len]
                mask[112:] = 1.0
            m = dict(shared)
            m["xp"] = xp
            m["xm"] = np.ascontiguousarray(x[b, half * half_len:(half + 1) * half_len])
            m["pmask"] = np.ascontiguousarray(mask.reshape(NPRE, 128).T)
            maps.append(m)
    return maps, NPRE, half_len // 512


def kernel(**inputs):
    x = np.asarray(inputs["x"])
    B, seq, _ = x.shape
    maps, NPRE, NBLK = make_in_maps(inputs, seq)
    nc = build(NPRE, NBLK)
    res = run_bass_kernel_spmd(nc, maps, core_ids=list(range(len(maps))))
    half_len = seq // 2
    out = np.empty((B, seq, D), np.float32)
    for b in range(B):
        for half in range(2):
            out[b, half * half_len:(half + 1) * half_len] = res.results[2 * b + half]["out"]
    return out
```

```python
import numpy as np
from contextlib import ExitStack
import concourse.bass as bass
import concourse.mybir as mybir
from concourse.bass_utils import run_bass_kernel_spmd

F32 = mybir.dt.float32
BF16 = mybir.dt.bfloat16
AF = mybir.ActivationFunctionType
ALU = mybir.AluOpType
AX = mybir.AxisListType

D = 1024
NIN = 2064
NE = 16
EH = 256
EPS = 1e-6
BIG = 1.0e4
ENGS = ("pe", "act", "dve", "pool", "sp")
OVERLAP = False
MIX_WIDTH = 4
MIX_STAGGER = 2
PRE_WIDTH = 3


class _Rec:
    def __init__(self):
        self.call = None

    def __getattr__(self, name):
        def f(*a, **k):
            self.call = (name, a, k)
            return self
        return f


class Prog:
    def __init__(self, nc, dma_sems=()):
        self.nc = nc
        self.ops = {e: [] for e in ENGS}
        self.cnt = {e: 0 for e in ENGS}
        for s in dma_sems:
            self.cnt[s] = 0
        self.seen = {e: {} for e in ENGS}
        self.lastw = {}
        self.readers = {}
        self.stopped = False
        import os
        self.debug = bool(os.environ.get("TRKDEBUG"))

    def op(self, eng, fn, reads=(), writes=(), sig=True, dma_sem=None):
        if self.stopped:
            return None
        waits = {}

        def need(tok):
            if tok is None:
                return
            s, v = tok
            if s == eng and eng == "pe":
                return
            if self.seen[eng].get(s, 0) >= v:
                return
            if waits.get(s, 0) < v:
                waits[s] = v

        for b in reads:
            need(self.lastw.get(b))
        for b in writes:
            need(self.lastw.get(b))
            for t in self.readers.get(b, ()):
                need(t)
        for s, v in waits.items():
            self.seen[eng][s] = v
        if dma_sem is not None:
            self.cnt[dma_sem] += 16
            tok = (dma_sem, self.cnt[dma_sem])
            inc = (dma_sem, 16)
        elif sig:
            self.cnt[eng] += 1
            tok = (eng, self.cnt[eng])
            inc = (eng, 1)
        else:
            tok = (eng, self.cnt[eng] + 1)
            inc = None
        for b in reads:
            self.readers.setdefault(b, []).append(tok)
        for b in writes:
            self.lastw[b] = tok
            self.readers[b] = []
        rec = _Rec()
        fn(rec)
        call = rec.call
        self.ops[eng].append((sorted(waits.items()), call, inc))
        if self.debug:
            import sys
            ln = sys._getframe(1).f_lineno
            print(f"OP {eng:4s} L{ln} waits={sorted(waits.items())} tok={tok} r={list(reads)} w={list(writes)}")
        return tok

    def finish(self, eng, toks):
        w = {}
        for tk in toks:
            if tk is None:
                continue
            s, v = tk
            w[s] = max(w.get(s, 0), v)
        for s in self.cnt:
            if self.cnt[s] > 0:
                w[s] = max(w.get(s, 0), self.cnt[s])
        self.ops[eng].append((sorted(w.items()), None, None))

    def emit(self, sems):
        nc = self.nc
        engobj = {"pe": "tensor", "act": "scalar", "dve": "vector", "pool": "gpsimd", "sp": "sync"}
        with nc.Block() as block:
            for e in ENGS:
                ops = self.ops[e]

                def body(eng, ops=ops):
                    for waits, fn, inc in ops:
                        for s, v in waits:
                            eng.wait_ge(sems[s], v)
                        if fn is None:
                            continue
                        ins = getattr(eng, fn[0])(*fn[1], **fn[2])
                        if inc is not None:
                            ins.then_inc(sems[inc[0]], inc[1])

                getattr(block, engobj[e])(body)


def build(NPRE, NBLK, STAGE=99):
    nc = bass.Bass("TRN2", target_bir_lowering=False)
    TP, TM = NPRE * 128, NBLK * 512
    dt_in = lambda name, shape: nc.dram_tensor(name, list(shape), F32, kind="ExternalInput").ap()
    xp_d = dt_in("xp", (TP, D))
    xm_d = dt_in("xm", (TM, D))
    pmask_d = dt_in("pmask", (128, NPRE))
    w_in_d = dt_in("w_in", (D, NIN))
    w_gu_d = dt_in("w_gu_b", (17, 256))
    gnw_d = dt_in("gnw", (128, 1))
    pool_w_d = dt_in("pool_w", (4, 128, 128))
    pscale_d = dt_in("pscale", (128, 4))
    w_out_d = dt_in("w_out", (D, D))
    nmw_d = dt_in("nmw_pk", (128, 8))
    nfw_d = dt_in("nfw_pk", (128, 8))
    fnw_d = dt_in("fnw", (D,))
    w_r_d = dt_in("w_r", (D, 20))
    b_r_d = dt_in("b_r", (20,))
    w_eg_d = dt_in("w_eg", (NE, D, EH))
    w_eu_d = dt_in("w_eu", (NE, D, EH))
    w_ed_d = dt_in("w_ed", (NE, EH, D))
    consts_d = dt_in("consts", (128, 8, 128))
    out_d = nc.dram_tensor("out", [TM, D], F32, kind="ExternalOutput").ap()
    wgu_s = nc.dram_tensor("wgu_scratch", [NE, 128, 8, 512], BF16, kind="Internal").ap()
    wd_s = nc.dram_tensor("wd_scratch", [NE, 128, 2, D], BF16, kind="Internal").ap()

    with ExitStack() as st:
        sb = lambda name, shape, dt: st.enter_context(nc.sbuf_tensor(name, list(shape), dt))
        w_in = sb("w_in_sb", (128, 8, NIN), BF16)
        w_out = sb("w_out_sb", (128, 8, D), BF16)
        pool_w = sb("pool_w_sb", (128, 4, 128), BF16)
        w_r = sb("w_r_sb", (128, 8, 20), F32)
        b_r = sb("b_r_sb", (128, 20), F32)
        w_gu = sb("w_gu_sb", (17, 256), F32)
        gnw = sb("gnw_sb", (128, 1), F32)
        pscale = sb("pscale_sb", (128, 4), F32)
        pmask = sb("pmask_sb", (128, NPRE), F32)
        fnw = sb("fnw_b", (128, D), F32)
        consts = sb("consts_sb", (128, 8, 128), F32)
        idb = sb("idb", (128, 128), BF16)
        NSLOT = 2
        wgu = [sb(f"wgu{i}", (128, 8, 512), BF16) for i in range(NSLOT)]
        wd = [sb(f"wd{i}", (128, 2, D), BF16) for i in range(NSLOT)]
        xb = [sb(f"x{i}", (128, 4, D), F32) for i in range(2)]
        x = xb[0]
        xn2T = sb("xn2T", (128, 8, 512), BF16)
        nmw_pk = sb("nmw_pk_sb", (128, 8), F32)
        nfw_pk = sb("nfw_pk_sb", (128, 8), F32)
        junk = sb("junk", (128, D), BF16)
        xn = [sb(f"xn{i}", (128, D), BF16) for i in range(2)]
        xnT = sb("xnT", (128, 8, 512), BF16)
        xn2f = sb("xn2f", (128, D), F32)
        xn2Tf = sb("xn2Tf", (128, 8, 128), F32)
        pvT = sb("pvT", (128, 4, 528), F32)
        pa = sb("pa", (128, 528), F32)
        pb = sb("pb", (128, 528), F32)
        pooledT = sb("pooledT", (128, 4, 512), BF16)
        ymT = sb("ymT", (128, 8, 512), BF16)
        qT = sb("qT", (128, 2, 512), BF16)
        kT = sb("kT", (128, 2, 512), BF16)
        srT = sb("srT", (128, 4, 512), BF16)
        g1 = sb("g1", (32, 512), F32)
        vtok = [sb(f"vtok{i}", (128, 512), BF16) for i in range(2)]
        sp = [sb(f"sp{i}", (128, 256), F32) for i in range(2)]
        eb = [sb(f"eb{i}", (128, 2, 128), F32) for i in range(2)]
        enb = [sb(f"enb{i}", (128, 2, 128), F32) for i in range(2)]
        eend = [sb(f"eend{i}", (128, 256), F32) for i in range(2)]
        qtX = [[sb(f"qt{c}{i}", (128, 2, 128), BF16) for c in "AB"] for i in range(2)]
        ktX = [[sb(f"kt{c}{i}", (128, 2, 128), BF16) for c in "AB"] for i in range(2)]
        kend = [sb(f"kend{i}", (128, 256), BF16) for i in range(2)]
        scT = sb("scT", (128, 4, 128), BF16)
        on = sb("on", (128, 512), BF16)
        S = sb("S", (128, 2, 128), F32)
        Sb = [sb(f"Sb{i}", (128, 2, 128), BF16) for i in range(2)]
        stat = sb("stat", (128, 4, 16), F32)
        stat4b = [sb(f"stat4{i}", (128, 16), F32) for i in range(2)]
        sg = sb("sg", (128, 2, 512), BF16)
        hT = [sb(f"hT{i}", (128, 2, 512), BF16) for i in range(2)]
        comb = sb("comb", (128, 4, 16), F32)
        rtb = [sb(f"rt{i}", (128, 128), F32) for i in range(2)]
        ps = st.enter_context(nc.psum_tensor("ps", [128, 7, 512], F32))

        dma_sems = ["ldc", "ldp"] + [f"ldx{p}{i}" for p in range(2) for i in range(4)] + [f"st{p}{i}" for p in range(2) for i in range(4)]
        for i in range(2):
            dma_sems += [f"wlg{i}", f"wld{i}", f"cvg{i}", f"cvu{i}", f"cvd{i}", f"csg{i}", f"csd{i}"]
        sems = {}
        for s in list(ENGS) + dma_sems:
            sems[s] = st.enter_context(nc.semaphore(s))
        P = Prog(nc, dma_sems=dma_sems)
        bankctr = [0]

        hard = [False]

        def stage(n):
            P.stopped = (STAGE < n) or hard[0]

        def nb():
            b = bankctr[0] % 7
            bankctr[0] += 1
            return b

        def pe_fence(b):
            P.op("pe", lambda e: e.matmul(ps[:, b, 508:512], lhsT=idb[0:1, :], rhs=idb[0:1, 0:4], start=True, stop=True),
                 reads=["idb"], writes=[f"ps{b}"])

        IDF = consts[:, 0, :]
        TRI_INC = consts[:, 1, :]
        TRI_STR = consts[:, 2, :]
        NEG16 = consts[:, 3, 0:1]
        CAUS4 = consts[:, 4:8, :]

        def ld(eng, out, in_, name, sem="ldc"):
            P.op(eng, lambda e: e.dma_start(out=out, in_=in_), writes=[name], dma_sem=sem)

        ld("sp", consts[:], consts_d, "consts")
        ld("sp", pmask[:], pmask_d, "pmask")
        ld("sp", w_gu[:], w_gu_d, "w_gu")
        ld("sp", gnw[:], gnw_d, "gnw")
        ld("sp", pscale[:], pscale_d, "pscale")
        ld("sp", nmw_pk[:], nmw_d, "nmw")
        ld("sp", nfw_pk[:], nfw_d, "nfw")
        ld("sp", fnw[:], fnw_d.partition_broadcast(128), "fnw")
        ld("sp", b_r[:], b_r_d.partition_broadcast(128), "b_r")
        ld("sp", w_r[:], w_r_d.rearrange("(kc p) n -> p kc n", p=128), "w_r")
        for kc in range(8):
            ld("pool", w_in[:, kc, :], w_in_d[kc * 128:(kc + 1) * 128, :], "w_in", "ldp")
        ld("pool", pool_w[:], pool_w_d.rearrange("g c d -> c g d"), "pool_w", "ldp")
        for kc in range(8):
            ld("pool", w_out[:, kc, :], w_out_d[kc * 128:(kc + 1) * 128, :], "w_out", "ldp")
        for name in ("consts", "pmask", "w_gu", "gnw", "pscale", "nmw", "nfw", "fnw", "b_r", "w_r"):
            P.lastw[name] = ("ldc", P.cnt["ldc"])
        for name in ("w_in", "pool_w", "w_out"):
            P.lastw[name] = ("ldp", P.cnt["ldp"])
        for kc in range(8):
            P.op("dve", lambda e: e.tensor_scalar(out=w_in[:, kc, :], in0=w_in[:, kc, :], scalar1=nmw_pk[:, kc:kc + 1],
                                                   scalar2=None, op0=ALU.mult), reads=["w_in", "nmw"], writes=["w_in"])
            P.op("dve", lambda e: e.tensor_scalar(out=w_r[:, kc, :], in0=w_r[:, kc, :], scalar1=nfw_pk[:, kc:kc + 1],
                                                  scalar2=None, op0=ALU.mult), reads=["w_r", "nfw"], writes=["w_r"])
        P.op("dve", lambda e: e.tensor_copy(out=idb[:], in_=IDF), reads=["consts"], writes=["idb"])
        P.op("dve", lambda e: e.memset(S[:], 0.0), writes=["S"])
        P.op("dve", lambda e: e.memset(g1[:], 1.0), writes=[f"g1_{i}" for i in range(4)])
        for p_ in range(2):
            for i_ in range(2):
                P.op("pool", lambda e: e.memset(qtX[p_][i_][:], 0.0), writes=[f"qt{p_}"])
                P.op("pool", lambda e: e.memset(ktX[p_][i_][:], 0.0), writes=[f"kt{p_}"])
        P.op("dve", lambda e: e.memset(pvT[:], 0.0), writes=["pvT"])

        cvt_tok = {}

        def convert_load(e_):
            s_ = e_ % NSLOT
            P.op("pool", lambda e: e.dma_start(out=wgu[s_][:, :, 0:256],
                                               in_=w_eg_d[e_].rearrange("(kc p) n -> p kc n", p=128)),
                 writes=[f"wgu{s_}a"], dma_sem=f"cvg{s_}")
            P.op("pool", lambda e: e.dma_start(out=wgu[s_][:, :, 256:512],
                                               in_=w_eu_d[e_].rearrange("(kc p) n -> p kc n", p=128)),
                 writes=[f"wgu{s_}b"], dma_sem=f"cvu{s_}")
            P.op("pool", lambda e: e.dma_start(out=wd[s_][:], in_=w_ed_d[e_].rearrange("(hc p) n -> p hc n", p=128)),
                 writes=[f"wd{s_}"], dma_sem=f"cvd{s_}")

        def convert_store(e_):
            s_ = e_ % NSLOT
            for kc in range(8):
                P.op("dve", lambda e: e.tensor_scalar(out=wgu[s_][:, kc, :], in0=wgu[s_][:, kc, :],
                                                      scalar1=nfw_pk[:, kc:kc + 1], scalar2=None, op0=ALU.mult),
                     reads=[f"wgu{s_}a", f"wgu{s_}b", "nfw"], writes=[f"wgu{s_}a", f"wgu{s_}b"])
            P.op("pool", lambda e: e.dma_start(out=wgu_s[e_], in_=wgu[s_][:]), reads=[f"wgu{s_}a", f"wgu{s_}b"],
                 writes=[f"wgus{e_}"], dma_sem=f"csg{s_}")
            P.op("pool", lambda e: e.dma_start(out=wd_s[e_], in_=wd[s_][:]), reads=[f"wd{s_}"],
                 writes=[f"wds{e_}"], dma_sem=f"csd{s_}")

        def run_streams(gens, width, stagger=1):
            it = iter(gens)
            active = []
            since = stagger
            done = False
            while True:
                if not done and len(active) < width and since >= stagger:
                    g = next(it, None)
                    if g is None:
                        done = True
                    else:
                        active.append(g)
                        since = 0
                if not active:
                    if done:
                        break
                    since = stagger
                    continue
                since += 1
                for g in list(reversed(active)):
                    try:
                        next(g)
                    except StopIteration:
                        active.remove(g)

        def rmsnorm(x_ap, xname, wb, wname, out_ap, oname, sidx):
            stn = f"stat{sidx}"
            P.op("dve", lambda e: e.memset(stat[:, sidx, 0:1], 0.0), writes=[stn])
            P.op("act", lambda e: e.activation(out=junk[:], in_=x_ap, func=AF.Square, accum_out=stat[:, sidx, 0:1]),
                 reads=[xname], writes=["junk", stn])
            P.op("act", lambda e: e.activation(out=stat[:, sidx, 1:2], in_=stat[:, sidx, 0:1], func=AF.Ln,
                                               scale=1.0 / D, bias=EPS), reads=[stn], writes=[stn])
            P.op("act", lambda e: e.activation(out=stat[:, sidx, 2:3], in_=stat[:, sidx, 1:2], func=AF.Exp, scale=-0.5),
                 reads=[stn], writes=[stn])
            if wb is None:
                P.op("dve", lambda e: e.tensor_scalar(out=out_ap, in0=x_ap, scalar1=stat[:, sidx, 2:3], scalar2=None,
                                                      op0=ALU.mult), reads=[xname, stn], writes=[oname])
            else:
                P.op("dve", lambda e: e.scalar_tensor_tensor(out=out_ap, in0=x_ap, scalar=stat[:, sidx, 2:3], in1=wb[:],
                                                             op0=ALU.mult, op1=ALU.mult),
                     reads=[xname, stn, wname], writes=[oname])

        def transpose_bf(src, sname, dst3, dname):
            for half in range(2):
                b = nb()
                for c in range(4):
                    kc = half * 4 + c
                    P.op("pe", lambda e: e.matmul(ps[:, b, c * 128:(c + 1) * 128],
                                                  lhsT=src[:, kc * 128:(kc + 1) * 128], rhs=idb[:], start=True, stop=True),
                         reads=[sname, "idb"], writes=[f"ps{b}"], sig=(c == 3))
                P.op("dve", lambda e: e.tensor_copy(
                    out=dst3[:, half * 4:(half + 1) * 4, :], in_=ps[:, b, :].rearrange("p (c n) -> p c n", c=4)),
                    reads=[f"ps{b}"], writes=[dname])

        def kv_tokmajor(c0, xtn):
            bk = nb()
            for kc in range(8):
                P.op("pe", lambda e: e.matmul(ps[:, bk, 0:256], lhsT=xnT[:, kc, c0:c0 + 128],
                                              rhs=w_in[:, kc, 768:1024], start=(kc == 0), stop=(kc == 7)),
                     reads=[xtn, "w_in"], writes=[f"ps{bk}"], sig=(kc == 7))
            bv = nb()
            for kc in range(8):
                P.op("pe", lambda e: e.matmul(ps[:, bv, 0:512], lhsT=xnT[:, kc, c0:c0 + 128],
                                              rhs=w_in[:, kc, 1024:1536], start=(kc == 0), stop=(kc == 7)),
                     reads=[xtn, "w_in"], writes=[f"ps{bv}"], sig=(kc == 7))
            return bk, bv

        def softplus_neg(gc0, g1name, par, mask_col=None):
            bl = nb()
            P.op("pe", lambda e: e.matmul(ps[:, bl, 0:256], lhsT=g1[0:17, gc0:gc0 + 128], rhs=w_gu[:, :],
                                          start=True, stop=True), reads=[g1name, "w_gu"], writes=[f"ps{bl}"], sig=False)
            pe_fence(bl)
            P.op("act", lambda e: e.activation(out=sp[par][:], in_=ps[:, bl, 0:256], func=AF.Exp, scale=-1.0),
                 reads=[f"ps{bl}"], writes=[f"sp{par}"])
            P.op("act", lambda e: e.activation(out=sp[par][:], in_=sp[par][:], func=AF.Ln, bias=1.0),
                 reads=[f"sp{par}"], writes=[f"sp{par}"])
            if mask_col is not None:
                P.op("dve", lambda e: e.tensor_scalar(out=sp[par][:], in0=sp[par][:], scalar1=pmask[:, mask_col:mask_col + 1],
                                                      scalar2=None, op0=ALU.mult),
                     reads=[f"sp{par}", "pmask"], writes=[f"sp{par}"])

        def kend_and_v(bk, bv, par):
            be = nb()
            P.op("pe", lambda e: e.matmul(ps[:, be, 0:256], lhsT=TRI_STR, rhs=sp[par][:], start=True, stop=True),
                 reads=["consts", f"sp{par}"], writes=[f"ps{be}"], sig=False)
            pe_fence(be)
            P.op("act", lambda e: e.activation(out=eend[par][:], in_=ps[:, be, 0:256], func=AF.Exp),
                 reads=[f"ps{be}"], writes=[f"eend{par}"])
            P.op("dve", lambda e: e.tensor_tensor(out=kend[par][:], in0=ps[:, bk, 0:256], in1=eend[par][:], op=ALU.mult),
                 reads=[f"ps{bk}", f"eend{par}"], writes=[f"kend{par}"])
            P.op("dve", lambda e: e.tensor_copy(out=vtok[par][:], in_=ps[:, bv, 0:512]),
                 reads=[f"ps{bv}"], writes=[f"vtok{par}"])

        def cum_decay(par, want_neg):
            bb = nb()
            for j in range(2):
                P.op("pe", lambda e: e.matmul(ps[:, bb, j * 128:(j + 1) * 128], lhsT=sp[par][:, j * 128:(j + 1) * 128],
                                              rhs=TRI_INC, start=True, stop=True),
                     reads=[f"sp{par}", "consts"], writes=[f"ps{bb}"], sig=False)
            pe_fence(bb)
            P.op("act", lambda e: e.activation(out=eb[par][:], in_=ps[:, bb, 0:256].rearrange("p (j n) -> p j n", j=2),
                                               func=AF.Exp), reads=[f"ps{bb}"], writes=[f"eb{par}"])
            if want_neg:
                P.op("act", lambda e: e.activation(out=enb[par][:], in_=ps[:, bb, 0:256].rearrange("p (j n) -> p j n", j=2),
                                                   func=AF.Exp, scale=-1.0), reads=[f"ps{bb}"], writes=[f"enb{par}"])

        def state_update(par):
            bs = nb()
            for j in range(2):
                P.op("pe", lambda e: e.matmul(ps[:, bs, j * 256:(j + 1) * 256], lhsT=kend[par][:, j * 128:(j + 1) * 128],
                                              rhs=vtok[par][:, j * 256:(j + 1) * 256], start=True, stop=True),
                     reads=[f"kend{par}", f"vtok{par}"], writes=[f"ps{bs}"], sig=(j == 1))
            for j in range(2):
                for hh in range(2):
                    r0 = hh * 64
                    P.op("dve", lambda e: e.scalar_tensor_tensor(
                        out=S[r0:r0 + 64, j, :], in0=S[r0:r0 + 64, j, :], scalar=eb[par][r0:r0 + 64, j, 127:128],
                        in1=ps[r0:r0 + 64, bs, j * 256 + hh * 128:j * 256 + hh * 128 + 128],
                        op0=ALU.mult, op1=ALU.add), reads=["S", f"eb{par}", f"ps{bs}"], writes=["S"])

        def prefix_tile(i):
            if i % 2 == 0 and i // 2 < NE:
                convert_load(i // 2)
            if i % 2 == 1 and i // 2 < NE:
                convert_store(i // 2)
            slot = i % 4
            par = i % 2
            c0 = slot * 128
            P.op("sp", lambda e: e.dma_start(out=x[:, slot, :], in_=xp_d[i * 128:(i + 1) * 128, :]),
                 writes=[f"x0{slot}"], dma_sem=f"ldx0{slot}")
            rmsnorm(x[:, slot, :], f"x0{slot}", None, None, xn[par][:], f"xn{par}", slot)
            yield
            transpose_bf(xn[par], f"xn{par}", xnT[:, :, c0:c0 + 128], f"xnT{slot}")
            yield
            bk, bv = kv_tokmajor(c0, f"xnT{slot}")
            bg = nb()
            for kc in range(8):
                P.op("pe", lambda e: e.matmul(ps[0:16, bg, 0:128], lhsT=w_in[:, kc, 2048:2064],
                                              rhs=xnT[:, kc, c0:c0 + 128], start=(kc == 0), stop=(kc == 7)),
                     reads=[f"xnT{slot}", "w_in"], writes=[f"ps{bg}"], sig=(kc == 7))
            P.op("act", lambda e: e.activation(out=g1[0:16, c0:c0 + 128], in_=ps[0:16, bg, 0:128], func=AF.Copy),
                 reads=[f"ps{bg}"], writes=[f"g1_{slot}"])
            softplus_neg(c0, f"g1_{slot}", par, mask_col=i)
            kend_and_v(bk, bv, par)
            cum_decay(par, False)
            state_update(par)
            if i == NPRE - 1:
                bh = nb()
                for g in range(4):
                    for kc in range(8):
                        P.op("pe", lambda e: e.matmul(ps[:, bh, g * 16:(g + 1) * 16],
                                                      lhsT=w_in[:, kc, g * 128:(g + 1) * 128],
                                                      rhs=xnT[:, kc, c0 + 112:c0 + 128],
                                                      start=(kc == 0), stop=(kc == 7)),
                             reads=[f"xnT{slot}", "w_in"], writes=[f"ps{bh}"], sig=(g == 3 and kc == 7))
                P.op("act", lambda e: e.activation(out=pvT[:, :, 0:16],
                                                   in_=ps[:, bh, 0:64].rearrange("p (g n) -> p g n", g=4), func=AF.Copy),
                     reads=[f"ps{bh}"], writes=["pvT"])

        run_streams([prefix_tile(i) for i in range(NPRE)], PRE_WIDTH, 1)
        for e_ in range(NE):
            if e_ == NPRE // 2 and NPRE % 2 == 1:
                convert_store(e_)
            elif e_ >= (NPRE + 1) // 2:
                convert_load(e_)
                convert_store(e_)
        P.op("act", lambda e: e.activation(out=Sb[0][:], in_=S[:], func=AF.Copy), reads=["S"], writes=["Sb0"])

        XT = [f"xnT{t}" for t in range(4)]
        G1ALL = [f"g1_{t}" for t in range(4)]
        out_toks = []

        def load_expert(ge):
            e_ = ge % NE
            s_ = ge % NSLOT
            P.op("sp", lambda e: e.dma_start(out=wgu[s_][:], in_=wgu_s[e_]), reads=[f"wgus{e_}"],
                 writes=[f"wgu{s_}a", f"wgu{s_}b"], dma_sem=f"wlg{s_}")
            P.op("sp", lambda e: e.dma_start(out=wd[s_][:], in_=wd_s[e_]), reads=[f"wds{e_}"],
                 writes=[f"wd{s_}"], dma_sem=f"wld{s_}")

        NGE = NBLK * NE
        load_expert(0)

        def front_tile(blk, t):
            r0 = blk * 512 + t * 128
            par = t % 2
            pb_ = blk % 2
            x = xb[pb_]
            P.op("sp", lambda e: e.dma_start(out=x[:, t, :], in_=xm_d[r0:r0 + 128, :]),
                 writes=[f"x{pb_}{t}"], dma_sem=f"ldx{pb_}{t}")
            rmsnorm(x[:, t, :], f"x{pb_}{t}", None, None, xn[par][:], f"xn{par}", t)
            yield
            transpose_bf(xn[par], f"xn{par}", xnT[:, :, t * 128:(t + 1) * 128], f"xnT{t}")

        def proj_chunk(col0, m):
            b = nb()
            for kc in range(8):
                P.op("pe", lambda e: e.matmul(ps[0:m, b, :], lhsT=w_in[:, kc, col0:col0 + m], rhs=xnT[:, kc, :],
                                              start=(kc == 0), stop=(kc == 7)),
                     reads=XT + ["w_in"], writes=[f"ps{b}"], sig=(kc == 7))
            return b

        def block_front(blk):
            fts = [front_tile(blk, t) for t in range(4)]
            next(fts[0])
            yield
            for t in range(4):
                if t + 1 < 4:
                    next(fts[t + 1])
                    yield
                for _ in fts[t]:
                    pass
                yield
            for g in range(4):
                b = proj_chunk(g * 128, 128)
                P.op("act", lambda e: e.activation(out=pvT[:, g, 16:528], in_=ps[:, b, :], func=AF.Copy),
                     reads=[f"ps{b}"], writes=["pvT"])
                yield
            for j in range(2):
                b = proj_chunk(512 + j * 128, 128)
                P.op("act", lambda e: e.activation(out=qT[:, j, :], in_=ps[:, b, :], func=AF.Copy),
                     reads=[f"ps{b}"], writes=["qT"])
                yield
            for j in range(2):
                b = proj_chunk(768 + j * 128, 128)
                P.op("act", lambda e: e.activation(out=kT[:, j, :], in_=ps[:, b, :], func=AF.Copy),
                     reads=[f"ps{b}"], writes=["kT"])
                yield
            b = proj_chunk(2048, 16)
            P.op("act", lambda e: e.activation(out=g1[0:16, :], in_=ps[0:16, b, :], func=AF.Copy),
                 reads=[f"ps{b}"], writes=G1ALL)
            for h in range(4):
                b = proj_chunk(1536 + h * 128, 128)
                P.op("act", lambda e: e.activation(out=srT[:, h, :], in_=ps[:, b, :], func=AF.Silu),
                     reads=[f"ps{b}"], writes=["srT"])
                yield
            for g in range(4):
                p_ = pvT[:, g, :]
                w_ = 2 << g
                P.op("pool", lambda e: e.tensor_tensor(out=pa[:, 1:528], in0=p_[:, 1:528], in1=p_[:, 0:527], op=ALU.add),
                     reads=["pvT"], writes=["pa"])
                cur, curname = pa, "pa"
                if g >= 1:
                    P.op("pool", lambda e: e.tensor_tensor(out=pb[:, 3:528], in0=pa[:, 3:528], in1=pa[:, 1:526], op=ALU.add),
                         reads=["pa"], writes=["pb"])
                    cur, curname = pb, "pb"
                if g >= 2:
                    P.op("pool", lambda e: e.tensor_tensor(out=pa[:, 7:528], in0=pb[:, 7:528], in1=pb[:, 3:524], op=ALU.add),
                         reads=["pb"], writes=["pa"])
                    cur, curname = pa, "pa"
                if g >= 3:
                    P.op("pool", lambda e: e.tensor_tensor(out=pb[:, 15:528], in0=pa[:, 15:528], in1=pa[:, 7:520], op=ALU.add),
                         reads=["pa"], writes=["pb"])
                    cur, curname = pb, "pb"
                P.op("dve", lambda e: e.scalar_tensor_tensor(
                    out=pooledT[:, g, :], in0=cur[:, 16:528], scalar=1.0 / w_, in1=p_[:, 16:528],
                    op0=ALU.mult, op1=ALU.subtract), reads=[curname, "pvT"], writes=["pooledT"])
                yield
            P.op("pool", lambda e: e.tensor_copy(out=pvT[:, :, 0:16], in_=pvT[:, :, 512:528]), reads=["pvT"], writes=["pvT"])
            for g in range(4):
                b = nb()
                P.op("pe", lambda e: e.matmul(ps[:, b, :], lhsT=pool_w[:, g, :], rhs=pooledT[:, g, :], start=True, stop=True),
                     reads=["pool_w", "pooledT"], writes=[f"ps{b}"])
                P.op("act", lambda e: e.activation(out=ymT[:, g, :], in_=ps[:, b, :], func=AF.Copy, scale=pscale[:, g:g + 1]),
                     reads=[f"ps{b}", "pscale"], writes=["ymTp"])
            yield

        tile_ctr = [0]

        def main_tile(blk, t):
            c0 = t * 128
            par = tile_ctr[0] % 2
            tile_ctr[0] += 1
            stat4 = stat4b[par]
            st4n = f"stat4{par}"
            pb_ = blk % 2
            x = xb[pb_]
            xn_ = f"x{pb_}{t}"
            bk, bv = kv_tokmajor(c0, f"xnT{t}")
            softplus_neg(c0, f"g1_{t}", par)
            kend_and_v(bk, bv, par)
            cum_decay(par, True)
            for i_ in range(2):
                r0 = i_ * 64
                P.op("dve", lambda e: e.scalar_tensor_tensor(out=qtX[par][i_][r0:r0 + 64], in0=qT[r0:r0 + 64, :, c0:c0 + 128],
                                                             scalar=0.125, in1=eb[par][r0:r0 + 64], op0=ALU.mult, op1=ALU.mult),
                     reads=["qT", f"eb{par}"], writes=[f"qt{par}"])
                P.op("dve", lambda e: e.tensor_tensor(out=ktX[par][i_][r0:r0 + 64], in0=kT[r0:r0 + 64, :, c0:c0 + 128],
                                                      in1=enb[par][r0:r0 + 64], op=ALU.mult),
                     reads=["kT", f"enb{par}"], writes=[f"kt{par}"])
            yield
            bsc = nb()
            for h in range(4):
                j = h // 2
                P.op("pe", lambda e: e.matmul(ps[:, bsc, h * 128:(h + 1) * 128], lhsT=ktX[par][h % 2][:, j, :],
                                              rhs=qtX[par][h % 2][:, j, :], start=True, stop=True),
                     reads=[f"kt{par}", f"qt{par}"], writes=[f"ps{bsc}"], sig=(h == 3))
            P.op("dve", lambda e: e.tensor_tensor(out=scT[:], in0=ps[:, bsc, :].rearrange("p (h n) -> p h n", h=4),
                                                  in1=CAUS4, op=ALU.mult),
                 reads=[f"ps{bsc}", "consts"], writes=["scT"])
            yield
            bo = nb()
            for h in range(4):
                j = h // 2
                P.op("pe", lambda e: e.matmul(ps[:, bo, h * 128:(h + 1) * 128], lhsT=scT[:, h, :],
                                              rhs=vtok[par][:, h * 128:(h + 1) * 128], start=True, stop=False),
                     reads=["scT", f"vtok{par}"], writes=[f"ps{bo}"], sig=False)
                P.op("pe", lambda e: e.matmul(ps[:, bo, h * 128:(h + 1) * 128], lhsT=qtX[par][h % 2][:, j, :],
                                              rhs=Sb[par][:, j, :], start=False, stop=True),
                     reads=[f"qt{par}", f"Sb{par}"], writes=[f"ps{bo}"], sig=(h == 3))
            state_update(par)
            P.op("act", lambda e: e.activation(out=Sb[1 - par][:], in_=S[:], func=AF.Copy),
                 reads=["S"], writes=[f"Sb{1 - par}"])
            P.op("dve", lambda e: e.memset(stat4[:, 0:4], 0.0), writes=[st4n])
            for h in range(4):
                P.op("act", lambda e: e.activation(out=junk[:, 0:128], in_=ps[:, bo, h * 128:(h + 1) * 128],
                                                   func=AF.Square, accum_out=stat4[:, h:h + 1]),
                     reads=[f"ps{bo}"], writes=["junk", st4n])
            P.op("act", lambda e: e.activation(out=stat4[:, 4:8], in_=stat4[:, 0:4], func=AF.Ln, scale=1.0 / 128,
                                               bias=EPS), reads=[st4n], writes=[st4n])
            P.op("act", lambda e: e.activation(out=stat4[:, 8:12], in_=stat4[:, 4:8], func=AF.Exp, scale=-0.5),
                 reads=[st4n], writes=[st4n])
            for h in range(4):
                P.op("act", lambda e: e.activation(out=on[:, h * 128:(h + 1) * 128], in_=ps[:, bo, h * 128:(h + 1) * 128],
                                                   func=AF.Copy, scale=stat4[:, 8 + h:9 + h]),
                     reads=[f"ps{bo}", st4n], writes=["on"])
            yield
            bt = nb()
            for h in range(4):
                P.op("pe", lambda e: e.matmul(ps[:, bt, h * 128:(h + 1) * 128], lhsT=on[:, h * 128:(h + 1) * 128],
                                              rhs=idb[:], start=True, stop=True),
                     reads=["on", "idb"], writes=[f"ps{bt}"], sig=(h == 3))
            P.op("dve", lambda e: e.scalar_tensor_tensor(out=ymT[:, 4:8, c0:c0 + 128],
                                                         in0=ps[:, bt, :].rearrange("p (h n) -> p h n", h=4),
                                                         scalar=gnw[:, 0:1], in1=srT[:, :, c0:c0 + 128],
                                                         op0=ALU.mult, op1=ALU.mult),
                 reads=[f"ps{bt}", "gnw", "srT"], writes=[f"ymTg{t}"])
            yield
            for half in range(2):
                b = nb()
                for kc in range(8):
                    P.op("pe", lambda e: e.matmul(
                        ps[:, b, :], lhsT=ymT[:, kc, c0:c0 + 128], rhs=w_out[:, kc, half * 512:(half + 1) * 512],
                        start=(kc == 0), stop=(kc == 7)), reads=["ymTp", f"ymTg{t}", "w_out"], writes=[f"ps{b}"],
                        sig=(kc == 7))
                P.op("dve", lambda e: e.tensor_tensor(
                    out=x[:, t, half * 512:(half + 1) * 512], in0=ps[:, b, :], in1=x[:, t, half * 512:(half + 1) * 512],
                    op=ALU.add), reads=[f"ps{b}", xn_], writes=[xn_])
            if not OVERLAP:
                yield
                for _ in main_tile_b(blk, t):
                    yield

        def main_tile_b(blk, t):
            c0 = t * 128
            par = t % 2
            rt = rtb[par]
            rtn = f"rt{par}"
            pb_ = blk % 2
            x = xb[pb_]
            rmsnorm(x[:, t, :], f"x{pb_}{t}", None, None, xn2f[:], "xn2f", t)
            yield
            for half in range(2):
                b = nb()
                for c in range(4):
                    kc = half * 4 + c
                    P.op("pe", lambda e: e.matmul(ps[:, b, c * 128:(c + 1) * 128],
                                                  lhsT=xn2f[:, kc * 128:(kc + 1) * 128], rhs=IDF, start=True, stop=True),
                         reads=["xn2f", "consts"], writes=[f"ps{b}"], sig=(c == 3))
                P.op("dve", lambda e: e.tensor_copy(
                    out=xn2Tf[:, half * 4:(half + 1) * 4, :], in_=ps[:, b, :].rearrange("p (c n) -> p c n", c=4)),
                    reads=[f"ps{b}"], writes=["xn2Tf"])
                P.op("act", lambda e: e.activation(
                    out=xn2T[:, half * 4:(half + 1) * 4, c0:c0 + 128], in_=xn2Tf[:, half * 4:(half + 1) * 4, :],
                    func=AF.Copy), reads=["xn2Tf"], writes=[f"xn2T{t}"])
            yield
            br = nb()
            for kc in range(8):
                P.op("pe", lambda e: e.matmul(ps[:, br, 0:20], lhsT=xn2Tf[:, kc, :], rhs=w_r[:, kc, :],
                                              start=(kc == 0), stop=(kc == 7)),
                     reads=["xn2Tf", "w_r"], writes=[f"ps{br}"], sig=False)
            pe_fence(br)
            V = lambda fn: P.op("dve", fn, reads=[rtn], writes=[rtn])
            P.op("dve", lambda e: e.tensor_tensor(out=rt[:, 0:20], in0=ps[:, br, 0:20], in1=b_r[:], op=ALU.add),
                 reads=[f"ps{br}", "b_r"], writes=[rtn])
            V(lambda e: e.tensor_reduce(out=rt[:, 20:21], in_=rt[:, 0:4], axis=AX.X, op=ALU.max))
            V(lambda e: e.tensor_scalar(out=rt[:, 24:28], in0=rt[:, 0:4], scalar1=rt[:, 20:21], scalar2=None, op0=ALU.is_equal))
            V(lambda e: e.tensor_scalar(out=rt[:, 21:22], in0=rt[:, 20:21], scalar1=-1.0, scalar2=None, op0=ALU.mult))
            P.op("dve", lambda e: e.memset(rt[:, 22:23], 0.0), reads=[rtn], writes=[rtn])
            P.op("act", lambda e: e.activation(out=rt[:, 28:32], in_=rt[:, 0:4], func=AF.Exp, bias=rt[:, 21:22],
                                               accum_out=rt[:, 22:23]), reads=[rtn], writes=[rtn])
            V(lambda e: e.reciprocal(out=rt[:, 23:24], in_=rt[:, 22:23]))
            V(lambda e: e.tensor_scalar(out=rt[:, 32:36], in0=rt[:, 24:28], scalar1=BIG, scalar2=-BIG, op0=ALU.mult, op1=ALU.add))
            for g in range(4):
                V(lambda e: e.tensor_scalar(out=rt[:, 40 + 4 * g:44 + 4 * g], in0=rt[:, 4 + 4 * g:8 + 4 * g],
                                            scalar1=rt[:, 32 + g:33 + g], scalar2=None, op0=ALU.add))
            V(lambda e: e.tensor_reduce(out=rt[:, 36:37], in_=rt[:, 40:56], axis=AX.X, op=ALU.max))
            V(lambda e: e.tensor_scalar(out=rt[:, 56:72], in0=rt[:, 40:56], scalar1=rt[:, 36:37], scalar2=None, op0=ALU.is_equal))
            V(lambda e: e.scalar_tensor_tensor(out=rt[:, 72:88], in0=rt[:, 56:72], scalar=-BIG, in1=rt[:, 40:56],
                                               op0=ALU.mult, op1=ALU.add))
            V(lambda e: e.tensor_reduce(out=rt[:, 37:38], in_=rt[:, 72:88], axis=AX.X, op=ALU.max))
            V(lambda e: e.tensor_scalar(out=rt[:, 88:104], in0=rt[:, 72:88], scalar1=rt[:, 37:38], scalar2=None, op0=ALU.is_equal))
            V(lambda e: e.tensor_tensor(out=rt[:, 38:39], in0=rt[:, 37:38], in1=rt[:, 36:37], op=ALU.subtract))
            P.op("act", lambda e: e.activation(out=rt[:, 39:40], in_=rt[:, 38:39], func=AF.Exp), reads=[rtn], writes=[rtn])
            V(lambda e: e.tensor_scalar(out=rt[:, 104:105], in0=rt[:, 39:40], scalar1=1.0, scalar2=None, op0=ALU.add))
            V(lambda e: e.reciprocal(out=rt[:, 105:106], in_=rt[:, 104:105]))
            V(lambda e: e.tensor_tensor(out=rt[:, 106:107], in0=rt[:, 105:106], in1=rt[:, 23:24], op=ALU.mult))
            V(lambda e: e.tensor_tensor(out=rt[:, 107:108], in0=rt[:, 23:24], in1=rt[:, 106:107], op=ALU.subtract))
            V(lambda e: e.tensor_scalar(out=rt[:, 108:124], in0=rt[:, 56:72], scalar1=rt[:, 106:107], scalar2=None, op0=ALU.mult))
            P.op("dve", lambda e: e.scalar_tensor_tensor(out=comb[:, t, :], in0=rt[:, 88:104], scalar=rt[:, 107:108],
                                                         in1=rt[:, 108:124], op0=ALU.mult, op1=ALU.add),
                 reads=[rtn], writes=[f"comb{t}"])

        X2T = [f"xn2T{t}" for t in range(4)]

        def experts(blk):
            pb_ = blk % 2
            x = xb[pb_]
            for e_ in range(NE):
                ge = blk * NE + e_
                s_ = ge % NSLOT
                if ge + 1 < NGE:
                    load_expert(ge + 1)
                hp = ge % 2
                for hc in range(2):
                    bb2 = []
                    for gu in range(2):
                        b = nb()
                        bb2.append(b)
                        col = gu * 256 + hc * 128
                        for kc in range(8):
                            P.op("pe", lambda e: e.matmul(
                                ps[:, b, :], lhsT=wgu[s_][:, kc, col:col + 128], rhs=xn2T[:, kc, :],
                                start=(kc == 0), stop=(kc == 7)), reads=X2T + [f"wgu{s_}" + "ab"[gu]], writes=[f"ps{b}"],
                                sig=(kc == 7))
                    bg_, bu_ = bb2
                    P.op("act", lambda e: e.activation(out=sg[:, hc, :], in_=ps[:, bg_, :], func=AF.Silu),
                         reads=[f"ps{bg_}"], writes=[f"sg{hc}"])
                    P.op("dve", lambda e: e.tensor_tensor(out=hT[hp][:, hc, :], in0=ps[:, bu_, :], in1=sg[:, hc, :], op=ALU.mult),
                         reads=[f"ps{bu_}", f"sg{hc}"], writes=[f"hT{hp}"])
                for t in range(4):
                    for half in range(2):
                        b = nb()
                        for hc in range(2):
                            P.op("pe", lambda e: e.matmul(
                                ps[:, b, :], lhsT=hT[hp][:, hc, t * 128:(t + 1) * 128],
                                rhs=wd[s_][:, hc, half * 512:(half + 1) * 512], start=(hc == 0), stop=(hc == 1)),
                                reads=[f"hT{hp}", f"wd{s_}"], writes=[f"ps{b}"], sig=(hc == 1))
                        P.op("dve", lambda e: e.scalar_tensor_tensor(
                            out=x[:, t, half * 512:(half + 1) * 512], in0=ps[:, b, :], scalar=comb[:, t, e_:e_ + 1],
                            in1=x[:, t, half * 512:(half + 1) * 512], op0=ALU.mult, op1=ALU.add),
                            reads=[f"ps{b}", f"comb{t}", f"x{pb_}{t}"], writes=[f"x{pb_}{t}"])
                yield

        def final(blk):
            pb_ = blk % 2
            x = xb[pb_]
            for t in range(4):
                r0 = blk * 512 + t * 128
                rmsnorm(x[:, t, :], f"x{pb_}{t}", fnw, "fnw", x[:, t, :], f"x{pb_}{t}", t)
                out_toks.append(P.op("sp", lambda e: e.dma_start(out=out_d[r0:r0 + 128, :], in_=x[:, t, :]),
                                     reads=[f"x{pb_}{t}"], dma_sem=f"st{pb_}{t}"))

        def mixer1(blk):
            for _ in block_front(blk):
                yield
            gens = [main_tile(blk, t) for t in range(4)]
            active, nxt, since = [], 0, MIX_STAGGER
            while active or nxt < 4:
                if nxt < 4 and len(active) < MIX_WIDTH and since >= MIX_STAGGER:
                    active.append(gens[nxt])
                    nxt += 1
                    since = 0
                since += 1
                for g in list(reversed(active)):
                    try:
                        next(g)
                    except StopIteration:
                        active.remove(g)
                yield

        def part2(blk):
            if OVERLAP:
                run_streams([main_tile_b(blk, t) for t in range(4)], 2, 2)

        if OVERLAP:
            for _ in mixer1(0):
                pass
            part2(0)
            for blk in range(NBLK):
                mg = mixer1(blk + 1) if blk + 1 < NBLK else None
                for _ in experts(blk):
                    if mg is not None:
                        for _k in range(4):
                            if next(mg, "done") == "done":
                                mg = None
                                break
                final(blk)
                if mg is not None:
                    for _ in mg:
                        pass
                if blk + 1 < NBLK:
                    part2(blk + 1)
        else:
            for blk in range(NBLK):
                for _ in mixer1(blk):
                    pass
                part2(blk)
                for _ in experts(blk):
                    pass
                final(blk)
        P.finish("sp", out_toks)
        P.emit(sems)
    return nc


def make_consts():
    c = np.zeros((128, 8, 128), np.float32)
    s = np.arange(128)[:, None]
    cc = np.arange(128)[None, :]
    c[:, 0] = np.eye(128, dtype=np.float32)
    c[:, 1] = np.where(s <= cc, -1.0 / 16, 0.0)
    c[:, 2] = np.where(s > cc, -1.0 / 16, 0.0)
    c[:, 3] = -1.0 / 16
    for h in range(4):
        c[:, 4 + h] = (s <= cc).astype(np.float32)
    return c


def make_in_maps(inp, seq):
    B = inp["x"].shape[0]
    half_len = seq // 2
    NPRE = half_len // 128 + 1
    f = lambda a: np.ascontiguousarray(np.asarray(a, dtype=np.float32))
    x = f(inp["x"])
    meta = f(inp["meta_tokens"])
    shared = {
        "w_in": f(inp["w_in"][0]),
        "w_gu_b": f(np.concatenate([inp["w_gate_up"][0], inp["b_gate"][0][None, :]], axis=0)),
        "gnw": f(inp["gla_norm_w"][0].reshape(128, 1)),
        "pool_w": f(inp["pool_w"][0]),
        "pscale": f(inp["pool_scale"][0].reshape(4, 128).T),
        "w_out": f(inp["w_out"][0]),
        "nmw_pk": f(inp["norm_mix_w"][0].reshape(8, 128).T),
        "nfw_pk": f(inp["norm_ffn_w"][0].reshape(8, 128).T),
        "fnw": f(inp["final_norm_w"]),
        "w_r": f(np.concatenate([inp["w_router_group"][0], inp["w_router_expert"][0]], axis=1)),
        "b_r": f(np.concatenate([inp["b_router_group"][0], inp["b_router_expert"][0]], axis=0)),
        "w_eg": f(inp["w_expert_gate"][0]),
        "w_eu": f(inp["w_expert_up"][0]),
        "w_ed": f(inp["w_expert_down"][0]),
        "consts": make_consts(),
    }
    maps = []
    for b in range(B):
        for half in range(2):
            xp = np.zeros((NPRE * 128, D), np.float32)
            mask = np.zeros((NPRE * 128,), np.float32)
            if half == 0:
                xp[-16:] = meta
                mask[-16:] = 1.0
            else:
                xp[112:128] = meta
                xp[128:] = x[b, :half_len]
                mask[112:] = 1.0
            m = dict(shared)
            m["xp"] = xp
            m["xm"] = np.ascontiguousarray(x[b, half * half_len:(half + 1) * half_len])
            m["pmask"] = np.ascontiguousarray(mask.reshape(NPRE, 128).T)
            maps.append(m)
    return maps, NPRE, half_len // 512


def kernel(**inputs):
    x = np.asarray(inputs["x"])
    B, seq, _ = x.shape
    maps, NPRE, NBLK = make_in_maps(inputs, seq)
    nc = build(NPRE, NBLK)
    res = run_bass_kernel_spmd(nc, maps, core_ids=list(range(len(maps))))
    half_len = seq // 2
    out = np.empty((B, seq, D), np.float32)
    for b in range(B):
        for half in range(2):
            out[b, half * half_len:(half + 1) * half_len] = res.results[2 * b + half]["out"]
    return out
```

```python
import numpy as np
from contextlib import ExitStack
import concourse.bass as bass
import concourse.mybir as mybir
from concourse.bass_utils import run_bass_kernel_spmd

F32 = mybir.dt.float32
BF16 = mybir.dt.bfloat16
AF = mybir.ActivationFunctionType
ALU = mybir.AluOpType
AX = mybir.AxisListType

D = 1024
NIN = 2064
NE = 16
EH = 256
EPS = 1e-6
BIG = 1.0e4
ENGS = ("pe", "act", "dve", "pool", "sp")
OVERLAP = False
MIX_WIDTH = 4
MIX_STAGGER = 2
PRE_WIDTH = 3


class _Rec:
    def __init__(self):
        self.call = None

    def __getattr__(self, name):
        def f(*a, **k):
            self.call = (name, a, k)
            return self
        return f


class Prog:
    def __init__(self, nc, dma_sems=()):
        self.nc = nc
        self.ops = {e: [] for e in ENGS}
        self.cnt = {e: 0 for e in ENGS}
        for s in dma_sems:
            self.cnt[s] = 0
        self.seen = {e: {} for e in ENGS}
        self.lastw = {}
        self.readers = {}
        self.stopped = False
        import os
        self.debug = bool(os.environ.get("TRKDEBUG"))

    def op(self, eng, fn, reads=(), writes=(), sig=True, dma_sem=None):
        if self.stopped:
            return None
        waits = {}

        def need(tok):
            if tok is None:
                return
            s, v = tok
            if s == eng and eng == "pe":
                return
            if self.seen[eng].get(s, 0) >= v:
                return
            if waits.get(s, 0) < v:
                waits[s] = v

        for b in reads:
            need(self.lastw.get(b))
        for b in writes:
            need(self.lastw.get(b))
            for t in self.readers.get(b, ()):
                need(t)
        for s, v in waits.items():
            self.seen[eng][s] = v
        if dma_sem is not None:
            self.cnt[dma_sem] += 16
            tok = (dma_sem, self.cnt[dma_sem])
            inc = (dma_sem, 16)
        elif sig:
            self.cnt[eng] += 1
            tok = (eng, self.cnt[eng])
            inc = (eng, 1)
        else:
            tok = (eng, self.cnt[eng] + 1)
            inc = None
        for b in reads:
            self.readers.setdefault(b, []).append(tok)
        for b in writes:
            self.lastw[b] = tok
            self.readers[b] = []
        rec = _Rec()
        fn(rec)
        call = rec.call
        self.ops[eng].append((sorted(waits.items()), call, inc))
        if self.debug:
            import sys
            ln = sys._getframe(1).f_lineno
            print(f"OP {eng:4s} L{ln} waits={sorted(waits.items())} tok={tok} r={list(reads)} w={list(writes)}")
        return tok

    def finish(self, eng, toks):
        w = {}
        for tk in toks:
            if tk is None:
                continue
            s, v = tk
            w[s] = max(w.get(s, 0), v)
        for s in self.cnt:
            if self.cnt[s] > 0:
                w[s] = max(w.get(s, 0), self.cnt[s])
        self.ops[eng].append((sorted(w.items()), None, None))

    def emit(self, sems):
        nc = self.nc
        engobj = {"pe": "tensor", "act": "scalar", "dve": "vector", "pool": "gpsimd", "sp": "sync"}
        with nc.Block() as block:
            for e in ENGS:
                ops = self.ops[e]

                def body(eng, ops=ops):
                    for waits, fn, inc in ops:
                        for s, v in waits:
                            eng.wait_ge(sems[s], v)
                        if fn is None:
                            continue
                        ins = getattr(eng, fn[0])(*fn[1], **fn[2])
                        if inc is not None:
                            ins.then_inc(sems[inc[0]], inc[1])

                getattr(block, engobj[e])(body)


def build(NPRE, NBLK, STAGE=99):
    nc = bass.Bass("TRN2", target_bir_lowering=False)
    TP, TM = NPRE * 128, NBLK * 512
    dt_in = lambda name, shape: nc.dram_tensor(name, list(shape), F32, kind="ExternalInput").ap()
    xp_d = dt_in("xp", (TP, D))
    xm_d = dt_in("xm", (TM, D))
    pmask_d = dt_in("pmask", (128, NPRE))
    w_in_d = dt_in("w_in", (D, NIN))
    w_gu_d = dt_in("w_gu_b", (17, 256))
    gnw_d = dt_in("gnw", (128, 1))
    pool_w_d = dt_in("pool_w", (4, 128, 128))
    pscale_d = dt_in("pscale", (128, 4))
    w_out_d = dt_in("w_out", (D, D))
    nmw_d = dt_in("nmw_pk", (128, 8))
    nfw_d = dt_in("nfw_pk", (128, 8))
    fnw_d = dt_in("fnw", (D,))
    w_r_d = dt_in("w_r", (D, 20))
    b_r_d = dt_in("b_r", (20,))
    w_eg_d = dt_in("w_eg", (NE, D, EH))
    w_eu_d = dt_in("w_eu", (NE, D, EH))
    w_ed_d = dt_in("w_ed", (NE, EH, D))
    consts_d = dt_in("consts", (128, 8, 128))
    out_d = nc.dram_tensor("out", [TM, D], F32, kind="ExternalOutput").ap()
    wgu_s = nc.dram_tensor("wgu_scratch", [NE, 128, 8, 512], BF16, kind="Internal").ap()
    wd_s = nc.dram_tensor("wd_scratch", [NE, 128, 2, D], BF16, kind="Internal").ap()

    with ExitStack() as st:
        sb = lambda name, shape, dt: st.enter_context(nc.sbuf_tensor(name, list(shape), dt))
        w_in = sb("w_in_sb", (128, 8, NIN), BF16)
        w_out = sb("w_out_sb", (128, 8, D), BF16)
        pool_w = sb("pool_w_sb", (128, 4, 128), BF16)
        w_r = sb("w_r_sb", (128, 8, 20), F32)
        b_r = sb("b_r_sb", (128, 20), F32)
        w_gu = sb("w_gu_sb", (17, 256), F32)
        gnw = sb("gnw_sb", (128, 1), F32)
        pscale = sb("pscale_sb", (128, 4), F32)
        pmask = sb("pmask_sb", (128, NPRE), F32)
        fnw = sb("fnw_b", (128, D), F32)
        consts = sb("consts_sb", (128, 8, 128), F32)
        idb = sb("idb", (128, 128), BF16)
        NSLOT = 2
        wgu = [sb(f"wgu{i}", (128, 8, 512), BF16) for i in range(NSLOT)]
        wd = [sb(f"wd{i}", (128, 2, D), BF16) for i in range(NSLOT)]
        xb = [sb(f"x{i}", (128, 4, D), F32) for i in range(2)]
        x = xb[0]
        xn2T = sb("xn2T", (128, 8, 512), BF16)
        nmw_pk = sb("nmw_pk_sb", (128, 8), F32)
        nfw_pk = sb("nfw_pk_sb", (128, 8), F32)
        junk = sb("junk", (128, D), BF16)
        xn = [sb(f"xn{i}", (128, D), BF16) for i in range(2)]
        xnT = sb("xnT", (128, 8, 512), BF16)
        xn2f = sb("xn2f", (128, D), F32)
        xn2Tf = sb("xn2Tf", (128, 8, 128), F32)
        pvT = sb("pvT", (128, 4, 528), F32)
        pa = sb("pa", (128, 528), F32)
        pb = sb("pb", (128, 528), F32)
        pooledT = sb("pooledT", (128, 4, 512), BF16)
        ymT = sb("ymT", (128, 8, 512), BF16)
        qT = sb("qT", (128, 2, 512), BF16)
        kT = sb("kT", (128, 2, 512), BF16)
        srT = sb("srT", (128, 4, 512), BF16)
        g1 = sb("g1", (32, 512), F32)
        vtok = [sb(f"vtok{i}", (128, 512), BF16) for i in range(2)]
        sp = [sb(f"sp{i}", (128, 256), F32) for i in range(2)]
        eb = [sb(f"eb{i}", (128, 2, 128), F32) for i in range(2)]
        enb = [sb(f"enb{i}", (128, 2, 128), F32) for i in range(2)]
        eend = [sb(f"eend{i}", (128, 256), F32) for i in range(2)]
        qtX = [[sb(f"qt{c}{i}", (128, 2, 128), BF16) for c in "AB"] for i in range(2)]
        ktX = [[sb(f"kt{c}{i}", (128, 2, 128), BF16) for c in "AB"] for i in range(2)]
        kend = [sb(f"kend{i}", (128, 256), BF16) for i in range(2)]
        scT = sb("scT", (128, 4, 128), BF16)
        on = sb("on", (128, 512), BF16)
        S = sb("S", (128, 2, 128), F32)
        Sb = [sb(f"Sb{i}", (128, 2, 128), BF16) for i in range(2)]
        stat = sb("stat", (128, 4, 16), F32)
        stat4b = [sb(f"stat4{i}", (128, 16), F32) for i in range(2)]
        sg = sb("sg", (128, 2, 512), BF16)
        hT = [sb(f"hT{i}", (128, 2, 512), BF16) for i in range(2)]
        comb = sb("comb", (128, 4, 16), F32)
        rtb = [sb(f"rt{i}", (128, 128), F32) for i in range(2)]
        ps = st.enter_context(nc.psum_tensor("ps", [128, 8, 512], F32))

        dma_sems = ["ldc", "ldp"] + [f"ldx{p}{i}" for p in range(2) for i in range(4)] + [f"st{p}{i}" for p in range(2) for i in range(4)]
        for i in range(2):
            dma_sems += [f"wlg{i}", f"wld{i}", f"cvg{i}", f"cvu{i}", f"cvd{i}", f"csg{i}", f"csd{i}"]
        sems = {}
        for s in list(ENGS) + dma_sems:
            sems[s] = st.enter_context(nc.semaphore(s))
        P = Prog(nc, dma_sems=dma_sems)
        bankctr = [0]

        hard = [False]

        def stage(n):
            P.stopped = (STAGE < n) or hard[0]

        def nb():
            b = bankctr[0] % 8
            bankctr[0] += 1
            return b

        def pe_fence(b):
            P.op("pe", lambda e: e.matmul(ps[:, b, 508:512], lhsT=idb[0:1, :], rhs=idb[0:1, 0:4], start=True, stop=True),
                 reads=["idb"], writes=[f"ps{b}"])

        IDF = consts[:, 0, :]
        TRI_INC = consts[:, 1, :]
        TRI_STR = consts[:, 2, :]
        NEG16 = consts[:, 3, 0:1]
        CAUS4 = consts[:, 4:8, :]

        def ld(eng, out, in_, name, sem="ldc"):
            P.op(eng, lambda e: e.dma_start(out=out, in_=in_), writes=[name], dma_sem=sem)

        ld("sp", consts[:], consts_d, "consts")
        ld("sp", pmask[:], pmask_d, "pmask")
        ld("sp", w_gu[:], w_gu_d, "w_gu")
        ld("sp", gnw[:], gnw_d, "gnw")
        ld("sp", pscale[:], pscale_d, "pscale")
        ld("sp", nmw_pk[:], nmw_d, "nmw")
        ld("sp", nfw_pk[:], nfw_d, "nfw")
        ld("sp", fnw[:], fnw_d.partition_broadcast(128), "fnw")
        ld("sp", b_r[:], b_r_d.partition_broadcast(128), "b_r")
        ld("sp", w_r[:], w_r_d.rearrange("(kc p) n -> p kc n", p=128), "w_r")
        for kc in range(8):
            ld("pool", w_in[:, kc, :], w_in_d[kc * 128:(kc + 1) * 128, :], "w_in", "ldp")
        ld("pool", pool_w[:], pool_w_d.rearrange("g c d -> c g d"), "pool_w", "ldp")
        for kc in range(8):
            ld("pool", w_out[:, kc, :], w_out_d[kc * 128:(kc + 1) * 128, :], "w_out", "ldp")
        for name in ("consts", "pmask", "w_gu", "gnw", "pscale", "nmw", "nfw", "fnw", "b_r", "w_r"):
            P.lastw[name] = ("ldc", P.cnt["ldc"])
        for name in ("w_in", "pool_w", "w_out"):
            P.lastw[name] = ("ldp", P.cnt["ldp"])
        for kc in range(8):
            P.op("dve", lambda e: e.tensor_scalar(out=w_in[:, kc, :], in0=w_in[:, kc, :], scalar1=nmw_pk[:, kc:kc + 1],
                                                   scalar2=None, op0=ALU.mult), reads=["w_in", "nmw"], writes=["w_in"])
            P.op("dve", lambda e: e.tensor_scalar(out=w_r[:, kc, :], in0=w_r[:, kc, :], scalar1=nfw_pk[:, kc:kc + 1],
                                                  scalar2=None, op0=ALU.mult), reads=["w_r", "nfw"], writes=["w_r"])
        P.op("dve", lambda e: e.tensor_copy(out=idb[:], in_=IDF), reads=["consts"], writes=["idb"])
        P.op("dve", lambda e: e.memset(S[:], 0.0), writes=["S"])
        P.op("dve", lambda e: e.memset(g1[:], 1.0), writes=[f"g1_{i}" for i in range(4)])
        for p_ in range(2):
            for i_ in range(2):
                P.op("pool", lambda e: e.memset(qtX[p_][i_][:], 0.0), writes=[f"qt{p_}"])
                P.op("pool", lambda e: e.memset(ktX[p_][i_][:], 0.0), writes=[f"kt{p_}"])
        P.op("dve", lambda e: e.memset(pvT[:], 0.0), writes=["pvT"])

        cvt_tok = {}

        def convert_load(e_):
            s_ = e_ % NSLOT
            P.op("pool", lambda e: e.dma_start(out=wgu[s_][:, :, 0:256],
                                               in_=w_eg_d[e_].rearrange("(kc p) n -> p kc n", p=128)),
                 writes=[f"wgu{s_}a"], dma_sem=f"cvg{s_}")
            P.op("pool", lambda e: e.dma_start(out=wgu[s_][:, :, 256:512],
                                               in_=w_eu_d[e_].rearrange("(kc p) n -> p kc n", p=128)),
                 writes=[f"wgu{s_}b"], dma_sem=f"cvu{s_}")
            P.op("pool", lambda e: e.dma_start(out=wd[s_][:], in_=w_ed_d[e_].rearrange("(hc p) n -> p hc n", p=128)),
                 writes=[f"wd{s_}"], dma_sem=f"cvd{s_}")

        def convert_store(e_):
            s_ = e_ % NSLOT
            for kc in range(8):
                P.op("dve", lambda e: e.tensor_scalar(out=wgu[s_][:, kc, :], in0=wgu[s_][:, kc, :],
                                                      scalar1=nfw_pk[:, kc:kc + 1], scalar2=None, op0=ALU.mult),
                     reads=[f"wgu{s_}a", f"wgu{s_}b", "nfw"], writes=[f"wgu{s_}a", f"wgu{s_}b"])
            P.op("pool", lambda e: e.dma_start(out=wgu_s[e_], in_=wgu[s_][:]), reads=[f"wgu{s_}a", f"wgu{s_}b"],
                 writes=[f"wgus{e_}"], dma_sem=f"csg{s_}")
            P.op("pool", lambda e: e.dma_start(out=wd_s[e_], in_=wd[s_][:]), reads=[f"wd{s_}"],
                 writes=[f"wds{e_}"], dma_sem=f"csd{s_}")

        def run_streams(gens, width, stagger=1):
            it = iter(gens)
            active = []
            since = stagger
            done = False
            while True:
                if not done and len(active) < width and since >= stagger:
                    g = next(it, None)
                    if g is None:
                        done = True
                    else:
                        active.append(g)
                        since = 0
                if not active:
                    if done:
                        break
                    since = stagger
                    continue
                since += 1
                for g in list(reversed(active)):
                    try:
                        next(g)
                    except StopIteration:
                        active.remove(g)

        def rmsnorm(x_ap, xname, wb, wname, out_ap, oname, sidx):
            stn = f"stat{sidx}"
            P.op("dve", lambda e: e.memset(stat[:, sidx, 0:1], 0.0), writes=[stn])
            P.op("act", lambda e: e.activation(out=junk[:], in_=x_ap, func=AF.Square, accum_out=stat[:, sidx, 0:1]),
                 reads=[xname], writes=["junk", stn])
            P.op("act", lambda e: e.activation(out=stat[:, sidx, 1:2], in_=stat[:, sidx, 0:1], func=AF.Ln,
                                               scale=1.0 / D, bias=EPS), reads=[stn], writes=[stn])
            P.op("act", lambda e: e.activation(out=stat[:, sidx, 2:3], in_=stat[:, sidx, 1:2], func=AF.Exp, scale=-0.5),
                 reads=[stn], writes=[stn])
            if wb is None:
                P.op("dve", lambda e: e.tensor_scalar(out=out_ap, in0=x_ap, scalar1=stat[:, sidx, 2:3], scalar2=None,
                                                      op0=ALU.mult), reads=[xname, stn], writes=[oname])
            else:
                P.op("dve", lambda e: e.scalar_tensor_tensor(out=out_ap, in0=x_ap, scalar=stat[:, sidx, 2:3], in1=wb[:],
                                                             op0=ALU.mult, op1=ALU.mult),
                     reads=[xname, stn, wname], writes=[oname])

        def transpose_bf(src, sname, dst3, dname):
            for half in range(2):
                b = nb()
                for c in range(4):
                    kc = half * 4 + c
                    P.op("pe", lambda e: e.matmul(ps[:, b, c * 128:(c + 1) * 128],
                                                  lhsT=src[:, kc * 128:(kc + 1) * 128], rhs=idb[:], start=True, stop=True),
                         reads=[sname, "idb"], writes=[f"ps{b}"], sig=(c == 3))
                P.op("dve", lambda e: e.tensor_copy(
                    out=dst3[:, half * 4:(half + 1) * 4, :], in_=ps[:, b, :].rearrange("p (c n) -> p c n", c=4)),
                    reads=[f"ps{b}"], writes=[dname])

        def kv_tokmajor(c0, xtn):
            bk = nb()
            for kc in range(8):
                P.op("pe", lambda e: e.matmul(ps[:, bk, 0:256], lhsT=xnT[:, kc, c0:c0 + 128],
                                              rhs=w_in[:, kc, 768:1024], start=(kc == 0), stop=(kc == 7)),
                     reads=[xtn, "w_in"], writes=[f"ps{bk}"], sig=(kc == 7))
            bv = nb()
            for kc in range(8):
                P.op("pe", lambda e: e.matmul(ps[:, bv, 0:512], lhsT=xnT[:, kc, c0:c0 + 128],
                                              rhs=w_in[:, kc, 1024:1536], start=(kc == 0), stop=(kc == 7)),
                     reads=[xtn, "w_in"], writes=[f"ps{bv}"], sig=(kc == 7))
            return bk, bv

        def softplus_neg(gc0, g1name, par, mask_col=None):
            bl = nb()
            P.op("pe", lambda e: e.matmul(ps[:, bl, 0:256], lhsT=g1[0:17, gc0:gc0 + 128], rhs=w_gu[:, :],
                                          start=True, stop=True), reads=[g1name, "w_gu"], writes=[f"ps{bl}"], sig=False)
            pe_fence(bl)
            P.op("act", lambda e: e.activation(out=sp[par][:], in_=ps[:, bl, 0:256], func=AF.Exp, scale=-1.0),
                 reads=[f"ps{bl}"], writes=[f"sp{par}"])
            P.op("act", lambda e: e.activation(out=sp[par][:], in_=sp[par][:], func=AF.Ln, bias=1.0),
                 reads=[f"sp{par}"], writes=[f"sp{par}"])
            if mask_col is not None:
                P.op("dve", lambda e: e.tensor_scalar(out=sp[par][:], in0=sp[par][:], scalar1=pmask[:, mask_col:mask_col + 1],
                                                      scalar2=None, op0=ALU.mult),
                     reads=[f"sp{par}", "pmask"], writes=[f"sp{par}"])

        def kend_and_v(bk, bv, par):
            be = nb()
            P.op("pe", lambda e: e.matmul(ps[:, be, 0:256], lhsT=TRI_STR, rhs=sp[par][:], start=True, stop=True),
                 reads=["consts", f"sp{par}"], writes=[f"ps{be}"], sig=False)
            pe_fence(be)
            P.op("act", lambda e: e.activation(out=eend[par][:], in_=ps[:, be, 0:256], func=AF.Exp),
                 reads=[f"ps{be}"], writes=[f"eend{par}"])
            P.op("dve", lambda e: e.tensor_tensor(out=kend[par][:], in0=ps[:, bk, 0:256], in1=eend[par][:], op=ALU.mult),
                 reads=[f"ps{bk}", f"eend{par}"], writes=[f"kend{par}"])
            P.op("dve", lambda e: e.tensor_copy(out=vtok[par][:], in_=ps[:, bv, 0:512]),
                 reads=[f"ps{bv}"], writes=[f"vtok{par}"])

        def cum_decay(par, want_neg):
            bb = nb()
            for j in range(2):
                P.op("pe", lambda e: e.matmul(ps[:, bb, j * 128:(j + 1) * 128], lhsT=sp[par][:, j * 128:(j + 1) * 128],
                                              rhs=TRI_INC, start=True, stop=True),
                     reads=[f"sp{par}", "consts"], writes=[f"ps{bb}"], sig=False)
            pe_fence(bb)
            P.op("act", lambda e: e.activation(out=eb[par][:], in_=ps[:, bb, 0:256].rearrange("p (j n) -> p j n", j=2),
                                               func=AF.Exp), reads=[f"ps{bb}"], writes=[f"eb{par}"])
            if want_neg:
                P.op("act", lambda e: e.activation(out=enb[par][:], in_=ps[:, bb, 0:256].rearrange("p (j n) -> p j n", j=2),
                                                   func=AF.Exp, scale=-1.0), reads=[f"ps{bb}"], writes=[f"enb{par}"])

        def state_update(par):
            bs = nb()
            for j in range(2):
                P.op("pe", lambda e: e.matmul(ps[:, bs, j * 256:(j + 1) * 256], lhsT=kend[par][:, j * 128:(j + 1) * 128],
                                              rhs=vtok[par][:, j * 256:(j + 1) * 256], start=True, stop=True),
                     reads=[f"kend{par}", f"vtok{par}"], writes=[f"ps{bs}"], sig=(j == 1))
            for j in range(2):
                for hh in range(2):
                    r0 = hh * 64
                    P.op("dve", lambda e: e.scalar_tensor_tensor(
                        out=S[r0:r0 + 64, j, :], in0=S[r0:r0 + 64, j, :], scalar=eb[par][r0:r0 + 64, j, 127:128],
                        in1=ps[r0:r0 + 64, bs, j * 256 + hh * 128:j * 256 + hh * 128 + 128],
                        op0=ALU.mult, op1=ALU.add), reads=["S", f"eb{par}", f"ps{bs}"], writes=["S"])

        def prefix_tile(i):
            if i % 2 == 0 and i // 2 < NE:
                convert_load(i // 2)
            if i % 2 == 1 and i // 2 < NE:
                convert_store(i // 2)
            slot = i % 4
            par = i % 2
            c0 = slot * 128
            P.op("sp", lambda e: e.dma_start(out=x[:, slot, :], in_=xp_d[i * 128:(i + 1) * 128, :]),
                 writes=[f"x0{slot}"], dma_sem=f"ldx0{slot}")
            rmsnorm(x[:, slot, :], f"x0{slot}", None, None, xn[par][:], f"xn{par}", slot)
            yield
            transpose_bf(xn[par], f"xn{par}", xnT[:, :, c0:c0 + 128], f"xnT{slot}")
            yield
            bk, bv = kv_tokmajor(c0, f"xnT{slot}")
            bg = nb()
            for kc in range(8):
                P.op("pe", lambda e: e.matmul(ps[0:16, bg, 0:128], lhsT=w_in[:, kc, 2048:2064],
                                              rhs=xnT[:, kc, c0:c0 + 128], start=(kc == 0), stop=(kc == 7)),
                     reads=[f"xnT{slot}", "w_in"], writes=[f"ps{bg}"], sig=(kc == 7))
            P.op("act", lambda e: e.activation(out=g1[0:16, c0:c0 + 128], in_=ps[0:16, bg, 0:128], func=AF.Copy),
                 reads=[f"ps{bg}"], writes=[f"g1_{slot}"])
            softplus_neg(c0, f"g1_{slot}", par, mask_col=i)
            kend_and_v(bk, bv, par)
            cum_decay(par, False)
            state_update(par)
            if i == NPRE - 1:
                bh = nb()
                for g in range(4):
                    for kc in range(8):
                        P.op("pe", lambda e: e.matmul(ps[:, bh, g * 16:(g + 1) * 16],
                                                      lhsT=w_in[:, kc, g * 128:(g + 1) * 128],
                                                      rhs=xnT[:, kc, c0 + 112:c0 + 128],
                                                      start=(kc == 0), stop=(kc == 7)),
                             reads=[f"xnT{slot}", "w_in"], writes=[f"ps{bh}"], sig=(g == 3 and kc == 7))
                P.op("act", lambda e: e.activation(out=pvT[:, :, 0:16],
                                                   in_=ps[:, bh, 0:64].rearrange("p (g n) -> p g n", g=4), func=AF.Copy),
                     reads=[f"ps{bh}"], writes=["pvT"])

        run_streams([prefix_tile(i) for i in range(NPRE)], PRE_WIDTH, 1)
        for e_ in range(NE):
            if e_ == NPRE // 2 and NPRE % 2 == 1:
                convert_store(e_)
            elif e_ >= (NPRE + 1) // 2:
                convert_load(e_)
                convert_store(e_)
        P.op("act", lambda e: e.activation(out=Sb[0][:], in_=S[:], func=AF.Copy), reads=["S"], writes=["Sb0"])

        XT = [f"xnT{t}" for t in range(4)]
        G1ALL = [f"g1_{t}" for t in range(4)]
        out_toks = []

        def load_expert(ge):
            e_ = ge % NE
            s_ = ge % NSLOT
            P.op("sp", lambda e: e.dma_start(out=wgu[s_][:], in_=wgu_s[e_]), reads=[f"wgus{e_}"],
                 writes=[f"wgu{s_}a", f"wgu{s_}b"], dma_sem=f"wlg{s_}")
            P.op("sp", lambda e: e.dma_start(out=wd[s_][:], in_=wd_s[e_]), reads=[f"wds{e_}"],
                 writes=[f"wd{s_}"], dma_sem=f"wld{s_}")

        NGE = NBLK * NE
        load_expert(0)

        def front_tile(blk, t):
            r0 = blk * 512 + t * 128
            par = t % 2
            pb_ = blk % 2
            x = xb[pb_]
            P.op("sp", lambda e: e.dma_start(out=x[:, t, :], in_=xm_d[r0:r0 + 128, :]),
                 writes=[f"x{pb_}{t}"], dma_sem=f"ldx{pb_}{t}")
            rmsnorm(x[:, t, :], f"x{pb_}{t}", None, None, xn[par][:], f"xn{par}", t)
            yield
            transpose_bf(xn[par], f"xn{par}", xnT[:, :, t * 128:(t + 1) * 128], f"xnT{t}")

        def proj_chunk(col0, m):
            b = nb()
            for kc in range(8):
                P.op("pe", lambda e: e.matmul(ps[0:m, b, :], lhsT=w_in[:, kc, col0:col0 + m], rhs=xnT[:, kc, :],
                                              start=(kc == 0), stop=(kc == 7)),
                     reads=XT + ["w_in"], writes=[f"ps{b}"], sig=(kc == 7))
            return b

        def block_front(blk):
            fts = [front_tile(blk, t) for t in range(4)]
            next(fts[0])
            yield
            for t in range(4):
                if t + 1 < 4:
                    next(fts[t + 1])
                    yield
                for _ in fts[t]:
                    pass
                yield
            for g in range(4):
                b = proj_chunk(g * 128, 128)
                P.op("act", lambda e: e.activation(out=pvT[:, g, 16:528], in_=ps[:, b, :], func=AF.Copy),
                     reads=[f"ps{b}"], writes=["pvT"])
                yield
            for j in range(2):
                b = proj_chunk(512 + j * 128, 128)
                P.op("act", lambda e: e.activation(out=qT[:, j, :], in_=ps[:, b, :], func=AF.Copy),
                     reads=[f"ps{b}"], writes=["qT"])
                yield
            for j in range(2):
                b = proj_chunk(768 + j * 128, 128)
                P.op("act", lambda e: e.activation(out=kT[:, j, :], in_=ps[:, b, :], func=AF.Copy),
                     reads=[f"ps{b}"], writes=["kT"])
                yield
            b = proj_chunk(2048, 16)
            P.op("act", lambda e: e.activation(out=g1[0:16, :], in_=ps[0:16, b, :], func=AF.Copy),
                 reads=[f"ps{b}"], writes=G1ALL)
            for h in range(4):
                b = proj_chunk(1536 + h * 128, 128)
                P.op("act", lambda e: e.activation(out=srT[:, h, :], in_=ps[:, b, :], func=AF.Silu),
                     reads=[f"ps{b}"], writes=["srT"])
                yield
            for g in range(4):
                p_ = pvT[:, g, :]
                w_ = 2 << g
                P.op("pool", lambda e: e.tensor_tensor(out=pa[:, 1:528], in0=p_[:, 1:528], in1=p_[:, 0:527], op=ALU.add),
                     reads=["pvT"], writes=["pa"])
                cur, curname = pa, "pa"
                if g >= 1:
                    P.op("pool", lambda e: e.tensor_tensor(out=pb[:, 3:528], in0=pa[:, 3:528], in1=pa[:, 1:526], op=ALU.add),
                         reads=["pa"], writes=["pb"])
                    cur, curname = pb, "pb"
                if g >= 2:
                    P.op("pool", lambda e: e.tensor_tensor(out=pa[:, 7:528], in0=pb[:, 7:528], in1=pb[:, 3:524], op=ALU.add),
                         reads=["pb"], writes=["pa"])
                    cur, curname = pa, "pa"
                if g >= 3:
                    P.op("pool", lambda e: e.tensor_tensor(out=pb[:, 15:528], in0=pa[:, 15:528], in1=pa[:, 7:520], op=ALU.add),
                         reads=["pa"], writes=["pb"])
                    cur, curname = pb, "pb"
                P.op("dve", lambda e: e.scalar_tensor_tensor(
                    out=pooledT[:, g, :], in0=cur[:, 16:528], scalar=1.0 / w_, in1=p_[:, 16:528],
                    op0=ALU.mult, op1=ALU.subtract), reads=[curname, "pvT"], writes=["pooledT"])
                yield
            P.op("pool", lambda e: e.tensor_copy(out=pvT[:, :, 0:16], in_=pvT[:, :, 512:528]), reads=["pvT"], writes=["pvT"])
            for g in range(4):
                b = nb()
                P.op("pe", lambda e: e.matmul(ps[:, b, :], lhsT=pool_w[:, g, :], rhs=pooledT[:, g, :], start=True, stop=True),
                     reads=["pool_w", "pooledT"], writes=[f"ps{b}"])
                P.op("act", lambda e: e.activation(out=ymT[:, g, :], in_=ps[:, b, :], func=AF.Copy, scale=pscale[:, g:g + 1]),
                     reads=[f"ps{b}", "pscale"], writes=["ymTp"])
            yield

        tile_ctr = [0]

        def main_tile(blk, t):
            c0 = t * 128
            par = tile_ctr[0] % 2
            tile_ctr[0] += 1
            stat4 = stat4b[par]
            st4n = f"stat4{par}"
            pb_ = blk % 2
            x = xb[pb_]
            xn_ = f"x{pb_}{t}"
            bk, bv = kv_tokmajor(c0, f"xnT{t}")
            softplus_neg(c0, f"g1_{t}", par)
            kend_and_v(bk, bv, par)
            cum_decay(par, True)
            for i_ in range(2):
                r0 = i_ * 64
                P.op("dve", lambda e: e.scalar_tensor_tensor(out=qtX[par][i_][r0:r0 + 64], in0=qT[r0:r0 + 64, :, c0:c0 + 128],
                                                             scalar=0.125, in1=eb[par][r0:r0 + 64], op0=ALU.mult, op1=ALU.mult),
                     reads=["qT", f"eb{par}"], writes=[f"qt{par}"])
                P.op("dve", lambda e: e.tensor_tensor(out=ktX[par][i_][r0:r0 + 64], in0=kT[r0:r0 + 64, :, c0:c0 + 128],
                                                      in1=enb[par][r0:r0 + 64], op=ALU.mult),
                     reads=["kT", f"enb{par}"], writes=[f"kt{par}"])
            yield
            bsc = nb()
            for h in range(4):
                j = h // 2
                P.op("pe", lambda e: e.matmul(ps[:, bsc, h * 128:(h + 1) * 128], lhsT=ktX[par][h % 2][:, j, :],
                                              rhs=qtX[par][h % 2][:, j, :], start=True, stop=True),
                     reads=[f"kt{par}", f"qt{par}"], writes=[f"ps{bsc}"], sig=(h == 3))
            P.op("dve", lambda e: e.tensor_tensor(out=scT[:], in0=ps[:, bsc, :].rearrange("p (h n) -> p h n", h=4),
                                                  in1=CAUS4, op=ALU.mult),
                 reads=[f"ps{bsc}", "consts"], writes=["scT"])
            yield
            bo = nb()
            for h in range(4):
                j = h // 2
                P.op("pe", lambda e: e.matmul(ps[:, bo, h * 128:(h + 1) * 128], lhsT=scT[:, h, :],
                                              rhs=vtok[par][:, h * 128:(h + 1) * 128], start=True, stop=False),
                     reads=["scT", f"vtok{par}"], writes=[f"ps{bo}"], sig=False)
                P.op("pe", lambda e: e.matmul(ps[:, bo, h * 128:(h + 1) * 128], lhsT=qtX[par][h % 2][:, j, :],
                                              rhs=Sb[par][:, j, :], start=False, stop=True),
                     reads=[f"qt{par}", f"Sb{par}"], writes=[f"ps{bo}"], sig=(h == 3))
            state_update(par)
            P.op("act", lambda e: e.activation(out=Sb[1 - par][:], in_=S[:], func=AF.Copy),
                 reads=["S"], writes=[f"Sb{1 - par}"])
            P.op("dve", lambda e: e.memset(stat4[:, 0:4], 0.0), writes=[st4n])
            for h in range(4):
                P.op("act", lambda e: e.activation(out=junk[:, 0:128], in_=ps[:, bo, h * 128:(h + 1) * 128],
                                                   func=AF.Square, accum_out=stat4[:, h:h + 1]),
                     reads=[f"ps{bo}"], writes=["junk", st4n])
            P.op("act", lambda e: e.activation(out=stat4[:, 4:8], in_=stat4[:, 0:4], func=AF.Ln, scale=1.0 / 128,
                                               bias=EPS), reads=[st4n], writes=[st4n])
            P.op("act", lambda e: e.activation(out=stat4[:, 8:12], in_=stat4[:, 4:8], func=AF.Exp, scale=-0.5),
                 reads=[st4n], writes=[st4n])
            for h in range(4):
                P.op("act", lambda e: e.activation(out=on[:, h * 128:(h + 1) * 128], in_=ps[:, bo, h * 128:(h + 1) * 128],
                                                   func=AF.Copy, scale=stat4[:, 8 + h:9 + h]),
                     reads=[f"ps{bo}", st4n], writes=["on"])
            yield
            bt = nb()
            for h in range(4):
                P.op("pe", lambda e: e.matmul(ps[:, bt, h * 128:(h + 1) * 128], lhsT=on[:, h * 128:(h + 1) * 128],
                                              rhs=idb[:], start=True, stop=True),
                     reads=["on", "idb"], writes=[f"ps{bt}"], sig=(h == 3))
            P.op("dve", lambda e: e.scalar_tensor_tensor(out=ymT[:, 4:8, c0:c0 + 128],
                                                         in0=ps[:, bt, :].rearrange("p (h n) -> p h n", h=4),
                                                         scalar=gnw[:, 0:1], in1=srT[:, :, c0:c0 + 128],
                                                         op0=ALU.mult, op1=ALU.mult),
                 reads=[f"ps{bt}", "gnw", "srT"], writes=[f"ymTg{t}"])
            yield
            for half in range(2):
                b = nb()
                for kc in range(8):
                    P.op("pe", lambda e: e.matmul(
                        ps[:, b, :], lhsT=ymT[:, kc, c0:c0 + 128], rhs=w_out[:, kc, half * 512:(half + 1) * 512],
                        start=(kc == 0), stop=(kc == 7)), reads=["ymTp", f"ymTg{t}", "w_out"], writes=[f"ps{b}"],
                        sig=(kc == 7))
                P.op("dve", lambda e: e.tensor_tensor(
                    out=x[:, t, half * 512:(half + 1) * 512], in0=ps[:, b, :], in1=x[:, t, half * 512:(half + 1) * 512],
                    op=ALU.add), reads=[f"ps{b}", xn_], writes=[xn_])
            if not OVERLAP:
                yield
                for _ in main_tile_b(blk, t):
                    yield

        def main_tile_b(blk, t):
            c0 = t * 128
            par = t % 2
            rt = rtb[par]
            rtn = f"rt{par}"
            pb_ = blk % 2
            x = xb[pb_]
            rmsnorm(x[:, t, :], f"x{pb_}{t}", None, None, xn2f[:], "xn2f", t)
            yield
            for half in range(2):
                b = nb()
                for c in range(4):
                    kc = half * 4 + c
                    P.op("pe", lambda e: e.matmul(ps[:, b, c * 128:(c + 1) * 128],
                                                  lhsT=xn2f[:, kc * 128:(kc + 1) * 128], rhs=IDF, start=True, stop=True),
                         reads=["xn2f", "consts"], writes=[f"ps{b}"], sig=(c == 3))
                P.op("dve", lambda e: e.tensor_copy(
                    out=xn2Tf[:, half * 4:(half + 1) * 4, :], in_=ps[:, b, :].rearrange("p (c n) -> p c n", c=4)),
                    reads=[f"ps{b}"], writes=["xn2Tf"])
                P.op("act", lambda e: e.activation(
                    out=xn2T[:, half * 4:(half + 1) * 4, c0:c0 + 128], in_=xn2Tf[:, half * 4:(half + 1) * 4, :],
                    func=AF.Copy), reads=["xn2Tf"], writes=[f"xn2T{t}"])
            yield
            br = nb()
            for kc in range(8):
                P.op("pe", lambda e: e.matmul(ps[:, br, 0:20], lhsT=xn2Tf[:, kc, :], rhs=w_r[:, kc, :],
                                              start=(kc == 0), stop=(kc == 7)),
                     reads=["xn2Tf", "w_r"], writes=[f"ps{br}"], sig=False)
            pe_fence(br)
            V = lambda fn: P.op("dve", fn, reads=[rtn], writes=[rtn])
            P.op("dve", lambda e: e.tensor_tensor(out=rt[:, 0:20], in0=ps[:, br, 0:20], in1=b_r[:], op=ALU.add),
                 reads=[f"ps{br}", "b_r"], writes=[rtn])
            V(lambda e: e.tensor_reduce(out=rt[:, 20:21], in_=rt[:, 0:4], axis=AX.X, op=ALU.max))
            V(lambda e: e.tensor_scalar(out=rt[:, 24:28], in0=rt[:, 0:4], scalar1=rt[:, 20:21], scalar2=None, op0=ALU.is_equal))
            V(lambda e: e.tensor_scalar(out=rt[:, 21:22], in0=rt[:, 20:21], scalar1=-1.0, scalar2=None, op0=ALU.mult))
            P.op("dve", lambda e: e.memset(rt[:, 22:23], 0.0), reads=[rtn], writes=[rtn])
            P.op("act", lambda e: e.activation(out=rt[:, 28:32], in_=rt[:, 0:4], func=AF.Exp, bias=rt[:, 21:22],
                                               accum_out=rt[:, 22:23]), reads=[rtn], writes=[rtn])
            V(lambda e: e.reciprocal(out=rt[:, 23:24], in_=rt[:, 22:23]))
            V(lambda e: e.tensor_scalar(out=rt[:, 32:36], in0=rt[:, 24:28], scalar1=BIG, scalar2=-BIG, op0=ALU.mult, op1=ALU.add))
            for g in range(4):
                V(lambda e: e.tensor_scalar(out=rt[:, 40 + 4 * g:44 + 4 * g], in0=rt[:, 4 + 4 * g:8 + 4 * g],
                                            scalar1=rt[:, 32 + g:33 + g], scalar2=None, op0=ALU.add))
            V(lambda e: e.tensor_reduce(out=rt[:, 36:37], in_=rt[:, 40:56], axis=AX.X, op=ALU.max))
            V(lambda e: e.tensor_scalar(out=rt[:, 56:72], in0=rt[:, 40:56], scalar1=rt[:, 36:37], scalar2=None, op0=ALU.is_equal))
            V(lambda e: e.scalar_tensor_tensor(out=rt[:, 72:88], in0=rt[:, 56:72], scalar=-BIG, in1=rt[:, 40:56],
                                               op0=ALU.mult, op1=ALU.add))
            V(lambda e: e.tensor_reduce(out=rt[:, 37:38], in_=rt[:, 72:88], axis=AX.X, op=ALU.max))
            V(lambda e: e.tensor_scalar(out=rt[:, 88:104], in0=rt[:, 72:88], scalar1=rt[:, 37:38], scalar2=None, op0=ALU.is_equal))
            V(lambda e: e.tensor_tensor(out=rt[:, 38:39], in0=rt[:, 37:38], in1=rt[:, 36:37], op=ALU.subtract))
            P.op("act", lambda e: e.activation(out=rt[:, 39:40], in_=rt[:, 38:39], func=AF.Exp), reads=[rtn], writes=[rtn])
            V(lambda e: e.tensor_scalar(out=rt[:, 104:105], in0=rt[:, 39:40], scalar1=1.0, scalar2=None, op0=ALU.add))
            V(lambda e: e.reciprocal(out=rt[:, 105:106], in_=rt[:, 104:105]))
            V(lambda e: e.tensor_tensor(out=rt[:, 106:107], in0=rt[:, 105:106], in1=rt[:, 23:24], op=ALU.mult))
            V(lambda e: e.tensor_tensor(out=rt[:, 107:108], in0=rt[:, 23:24], in1=rt[:, 106:107], op=ALU.subtract))
            V(lambda e: e.tensor_scalar(out=rt[:, 108:124], in0=rt[:, 56:72], scalar1=rt[:, 106:107], scalar2=None, op0=ALU.mult))
            P.op("dve", lambda e: e.scalar_tensor_tensor(out=comb[:, t, :], in0=rt[:, 88:104], scalar=rt[:, 107:108],
                                                         in1=rt[:, 108:124], op0=ALU.mult, op1=ALU.add),
                 reads=[rtn], writes=[f"comb{t}"])

        X2T = [f"xn2T{t}" for t in range(4)]

        def experts(blk):
            pb_ = blk % 2
            x = xb[pb_]
            for e_ in range(NE):
                ge = blk * NE + e_
                s_ = ge % NSLOT
                if ge + 1 < NGE:
                    load_expert(ge + 1)
                hp = ge % 2
                for hc in range(2):
                    bb2 = []
                    for gu in range(2):
                        b = nb()
                        bb2.append(b)
                        col = gu * 256 + hc * 128
                        for kc in range(8):
                            P.op("pe", lambda e: e.matmul(
                                ps[:, b, :], lhsT=wgu[s_][:, kc, col:col + 128], rhs=xn2T[:, kc, :],
                                start=(kc == 0), stop=(kc == 7)), reads=X2T + [f"wgu{s_}" + "ab"[gu]], writes=[f"ps{b}"],
                                sig=(kc == 7))
                    bg_, bu_ = bb2
                    P.op("act", lambda e: e.activation(out=sg[:, hc, :], in_=ps[:, bg_, :], func=AF.Silu),
                         reads=[f"ps{bg_}"], writes=[f"sg{hc}"])
                    P.op("dve", lambda e: e.tensor_tensor(out=hT[hp][:, hc, :], in0=ps[:, bu_, :], in1=sg[:, hc, :], op=ALU.mult),
                         reads=[f"ps{bu_}", f"sg{hc}"], writes=[f"hT{hp}"])
                for t in range(4):
                    for half in range(2):
                        b = nb()
                        for hc in range(2):
                            P.op("pe", lambda e: e.matmul(
                                ps[:, b, :], lhsT=hT[hp][:, hc, t * 128:(t + 1) * 128],
                                rhs=wd[s_][:, hc, half * 512:(half + 1) * 512], start=(hc == 0), stop=(hc == 1)),
                                reads=[f"hT{hp}", f"wd{s_}"], writes=[f"ps{b}"], sig=(hc == 1))
                        P.op("dve", lambda e: e.scalar_tensor_tensor(
                            out=x[:, t, half * 512:(half + 1) * 512], in0=ps[:, b, :], scalar=comb[:, t, e_:e_ + 1],
                            in1=x[:, t, half * 512:(half + 1) * 512], op0=ALU.mult, op1=ALU.add),
                            reads=[f"ps{b}", f"comb{t}", f"x{pb_}{t}"], writes=[f"x{pb_}{t}"])
                yield

        def final(blk):
            pb_ = blk % 2
            x = xb[pb_]
            for t in range(4):
                r0 = blk * 512 + t * 128
                rmsnorm(x[:, t, :], f"x{pb_}{t}", fnw, "fnw", x[:, t, :], f"x{pb_}{t}", t)
                out_toks.append(P.op("sp", lambda e: e.dma_start(out=out_d[r0:r0 + 128, :], in_=x[:, t, :]),
                                     reads=[f"x{pb_}{t}"], dma_sem=f"st{pb_}{t}"))

        def mixer1(blk):
            for _ in block_front(blk):
                yield
            gens = [main_tile(blk, t) for t in range(4)]
            active, nxt, since = [], 0, MIX_STAGGER
            while active or nxt < 4:
                if nxt < 4 and len(active) < MIX_WIDTH and since >= MIX_STAGGER:
                    active.append(gens[nxt])
                    nxt += 1
                    since = 0
                since += 1
                for g in list(reversed(active)):
                    try:
                        next(g)
                    except StopIteration:
                        active.remove(g)
                yield

        def part2(blk):
            if OVERLAP:
                run_streams([main_tile_b(blk, t) for t in range(4)], 2, 2)

        if OVERLAP:
            for _ in mixer1(0):
                pass
            part2(0)
            for blk in range(NBLK):
                mg = mixer1(blk + 1) if blk + 1 < NBLK else None
                for _ in experts(blk):
                    if mg is not None:
                        for _k in range(4):
                            if next(mg, "done") == "done":
                                mg = None
                                break
                final(blk)
                if mg is not None:
                    for _ in mg:
                        pass
                if blk + 1 < NBLK:
                    part2(blk + 1)
        else:
            for blk in range(NBLK):
                for _ in mixer1(blk):
                    pass
                part2(blk)
                for _ in experts(blk):
                    pass
                final(blk)
        P.finish("sp", out_toks)
        P.emit(sems)
    return nc


def make_consts():
    c = np.zeros((128, 8, 128), np.float32)
    s = np.arange(128)[:, None]
    cc = np.arange(128)[None, :]
    c[:, 0] = np.eye(128, dtype=np.float32)
    c[:, 1] = np.where(s <= cc, -1.0 / 16, 0.0)
    c[:, 2] = np.where(s > cc, -1.0 / 16, 0.0)
    c[:, 3] = -1.0 / 16
    for h in range(4):
        c[:, 4 + h] = (s <= cc).astype(np.float32)
    return c


def make_in_maps(inp, seq):
    B = inp["x"].shape[0]
    half_len = seq // 2
    NPRE = half_len // 128 + 1
    f = lambda a: np.ascontiguousarray(np.asarray(a, dtype=np.float32))
    x = f(inp["x"])
    meta = f(inp["meta_tokens"])
    shared = {
        "w_in": f(inp["w_in"][0]),
        "w_gu_b": f(np.concatenate([inp["w_gate_up"][0], inp["b_gate"][0][None, :]], axis=0)),
        "gnw": f(inp["gla_norm_w"][0].reshape(128, 1)),
        "pool_w": f(inp["pool_w"][0]),
        "pscale": f(inp["pool_scale"][0].reshape(4, 128).T),
        "w_out": f(inp["w_out"][0]),
        "nmw_pk": f(inp["norm_mix_w"][0].reshape(8, 128).T),
        "nfw_pk": f(inp["norm_ffn_w"][0].reshape(8, 128).T),
        "fnw": f(inp["final_norm_w"]),
        "w_r": f(np.concatenate([inp["w_router_group"][0], inp["w_router_expert"][0]], axis=1)),
        "b_r": f(np.concatenate([inp["b_router_group"][0], inp["b_router_expert"][0]], axis=0)),
        "w_eg": f(inp["w_expert_gate"][0]),
        "w_eu": f(inp["w_expert_up"][0]),
        "w_ed": f(inp["w_expert_down"][0]),
        "consts": make_consts(),
    }
    maps = []
    for b in range(B):
        for half in range(2):
            xp = np.zeros((NPRE * 128, D), np.float32)
            mask = np.zeros((NPRE * 128,), np.float32)
            if half == 0:
                xp[-16:] = meta
                mask[-16:] = 1.0
            else:
                xp[112:128] = meta
                xp[128:] = x[b, :half_len]
                mask[112:] = 1.0
            m = dict(shared)
            m["xp"] = xp
            m["xm"] = np.ascontiguousarray(x[b, half * half_len:(half + 1) * half_len])
            m["pmask"] = np.ascontiguousarray(mask.reshape(NPRE, 128).T)
            maps.append(m)
    return maps, NPRE, half_len // 512


def kernel(**inputs):
    x = np.asarray(inputs["x"])
    B, seq, _ = x.shape
    maps, NPRE, NBLK = make_in_maps(inputs, seq)
    nc = build(NPRE, NBLK)
    res = run_bass_kernel_spmd(nc, maps, core_ids=list(range(len(maps))))
    half_len = seq // 2
    out = np.empty((B, seq, D), np.float32)
    for b in range(B):
        for half in range(2):
            out[b, half * half_len:(half + 1) * half_len] = res.results[2 * b + half]["out"]
    return out
```

```python
import numpy as np
from contextlib import ExitStack
import concourse.bass as bass
import concourse.mybir as mybir
from concourse.bass_utils import run_bass_kernel_spmd

F32 = mybir.dt.float32
BF16 = mybir.dt.bfloat16
AF = mybir.ActivationFunctionType
ALU = mybir.AluOpType
AX = mybir.AxisListType

D = 1024
NIN = 2064
NE = 16
EH = 256
EPS = 1e-6
BIG = 1.0e4
ENGS = ("pe", "act", "dve", "pool", "sp")
OVERLAP = False
MIX_WIDTH = 4
MIX_STAGGER = 2
PRE_WIDTH = 6


class _Rec:
    def __init__(self):
        self.call = None

    def __getattr__(self, name):
        def f(*a, **k):
            self.call = (name, a, k)
            return self
        return f


class Prog:
    def __init__(self, nc, dma_sems=()):
        self.nc = nc
        self.ops = {e: [] for e in ENGS}
        self.cnt = {e: 0 for e in ENGS}
        for s in dma_sems:
            self.cnt[s] = 0
        self.seen = {e: {} for e in ENGS}
        self.lastw = {}
        self.readers = {}
        self.stopped = False
        import os
        self.debug = bool(os.environ.get("TRKDEBUG"))

    def op(self, eng, fn, reads=(), writes=(), sig=True, dma_sem=None):
        if self.stopped:
            return None
        waits = {}

        def need(tok):
            if tok is None:
                return
            s, v = tok
            if s == eng and eng == "pe":
                return
            if self.seen[eng].get(s, 0) >= v:
                return
            if waits.get(s, 0) < v:
                waits[s] = v

        for b in reads:
            need(self.lastw.get(b))
        for b in writes:
            need(self.lastw.get(b))
            for t in self.readers.get(b, ()):
                need(t)
        for s, v in waits.items():
            self.seen[eng][s] = v
        if dma_sem is not None:
            self.cnt[dma_sem] += 16
            tok = (dma_sem, self.cnt[dma_sem])
            inc = (dma_sem, 16)
        elif sig:
            self.cnt[eng] += 1
            tok = (eng, self.cnt[eng])
            inc = (eng, 1)
        else:
            tok = (eng, self.cnt[eng] + 1)
            inc = None
        for b in reads:
            self.readers.setdefault(b, []).append(tok)
        for b in writes:
            self.lastw[b] = tok
            self.readers[b] = []
        rec = _Rec()
        fn(rec)
        call = rec.call
        self.ops[eng].append((sorted(waits.items()), call, inc))
        if self.debug:
            import sys
            ln = sys._getframe(1).f_lineno
            print(f"OP {eng:4s} L{ln} waits={sorted(waits.items())} tok={tok} r={list(reads)} w={list(writes)}")
        return tok

    def finish(self, eng, toks):
        w = {}
        for tk in toks:
            if tk is None:
                continue
            s, v = tk
            w[s] = max(w.get(s, 0), v)
        for s in self.cnt:
            if self.cnt[s] > 0:
                w[s] = max(w.get(s, 0), self.cnt[s])
        self.ops[eng].append((sorted(w.items()), None, None))

    def emit(self, sems):
        nc = self.nc
        engobj = {"pe": "tensor", "act": "scalar", "dve": "vector", "pool": "gpsimd", "sp": "sync"}
        with nc.Block() as block:
            for e in ENGS:
                ops = self.ops[e]

                def body(eng, ops=ops):
                    for waits, fn, inc in ops:
                        for s, v in waits:
                            eng.wait_ge(sems[s], v)
                        if fn is None:
                            continue
                        ins = getattr(eng, fn[0])(*fn[1], **fn[2])
                        if inc is not None:
                            ins.then_inc(sems[inc[0]], inc[1])

                getattr(block, engobj[e])(body)


def build(NPRE, NBLK, STAGE=99):
    nc = bass.Bass("TRN2", target_bir_lowering=False)
    TP, TM = NPRE * 128, NBLK * 512
    dt_in = lambda name, shape: nc.dram_tensor(name, list(shape), F32, kind="ExternalInput").ap()
    xp_d = dt_in("xp", (TP, D))
    xm_d = dt_in("xm", (TM, D))
    pmask_d = dt_in("pmask", (128, NPRE))
    w_in_d = dt_in("w_in", (D, NIN))
    w_gu_d = dt_in("w_gu_b", (17, 256))
    gnw_d = dt_in("gnw", (128, 1))
    pool_w_d = dt_in("pool_w", (4, 128, 128))
    pscale_d = dt_in("pscale", (128, 4))
    w_out_d = dt_in("w_out", (D, D))
    nmw_d = dt_in("nmw_pk", (128, 8))
    nfw_d = dt_in("nfw_pk", (128, 8))
    fnw_d = dt_in("fnw", (D,))
    w_r_d = dt_in("w_r", (D, 20))
    b_r_d = dt_in("b_r", (20,))
    w_eg_d = dt_in("w_eg", (NE, D, EH))
    w_eu_d = dt_in("w_eu", (NE, D, EH))
    w_ed_d = dt_in("w_ed", (NE, EH, D))
    consts_d = dt_in("consts", (128, 5, 128))
    out_d = nc.dram_tensor("out", [TM, D], F32, kind="ExternalOutput").ap()
    wgu_s = nc.dram_tensor("wgu_scratch", [NE, 128, 8, 512], BF16, kind="Internal").ap()
    wd_s = nc.dram_tensor("wd_scratch", [NE, 128, 2, D], BF16, kind="Internal").ap()

    with ExitStack() as st:
        sb = lambda name, shape, dt: st.enter_context(nc.sbuf_tensor(name, list(shape), dt))
        w_in = sb("w_in_sb", (128, 8, NIN), BF16)
        w_out = sb("w_out_sb", (128, 8, D), BF16)
        pool_w = sb("pool_w_sb", (128, 4, 128), BF16)
        w_r = sb("w_r_sb", (128, 8, 20), F32)
        b_r = sb("b_r_sb", (128, 20), F32)
        w_gu = sb("w_gu_sb", (17, 256), F32)
        gnw = sb("gnw_sb", (128, 1), F32)
        pscale = sb("pscale_sb", (128, 4), F32)
        pmask = sb("pmask_sb", (128, NPRE), F32)
        fnw = sb("fnw_b", (128, D), F32)
        consts = sb("consts_sb", (128, 5, 128), F32)
        caus4 = sb("caus4", (128, 4, 128), BF16)
        idb = sb("idb", (128, 128), BF16)
        NSLOT = 2
        wgu = [sb(f"wgu{i}", (128, 8, 512), BF16) for i in range(NSLOT)]
        wd = [sb(f"wd{i}", (128, 2, D), BF16) for i in range(NSLOT)]
        xb = [sb(f"x{i}", (128, 4, D), F32) for i in range(2)]
        x = xb[0]
        xn2T = sb("xn2T", (128, 8, 512), BF16)
        nmw_pk = sb("nmw_pk_sb", (128, 8), F32)
        nfw_pk = sb("nfw_pk_sb", (128, 8), F32)
        junk = sb("junk", (128, D), BF16)
        xn = [sb(f"xn{i}", (128, D), BF16) for i in range(2)]
        xnT = sb("xnT", (128, 8, 512), BF16)
        xn2f = sb("xn2f", (128, D), F32)
        xn2Tf = sb("xn2Tf", (128, 8, 128), F32)
        pvT = sb("pvT", (128, 4, 528), F32)
        pa = sb("pa", (128, 528), F32)
        pb = sb("pb", (128, 528), F32)
        pooledT = sb("pooledT", (128, 4, 512), BF16)
        ymT = sb("ymT", (128, 8, 512), BF16)
        qT = sb("qT", (128, 2, 512), BF16)
        kT = sb("kT", (128, 2, 512), BF16)
        srT = sb("srT", (128, 4, 512), BF16)
        g1 = sb("g1", (32, 512), F32)
        vtok = [sb(f"vtok{i}", (128, 512), BF16) for i in range(4)]
        ktok = [sb(f"ktok{i}", (128, 256), BF16) for i in range(3)]
        sp = [sb(f"sp{i}", (128, 256), F32) for i in range(2)]
        eb = [sb(f"eb{i}", (128, 2, 128), F32) for i in range(2)]
        enb = [sb(f"enb{i}", (128, 2, 128), BF16) for i in range(2)]
        eend = [sb(f"eend{i}", (128, 256), BF16) for i in range(2)]
        qtX = [[sb(f"qt{c}{i}", (128, 2, 128), BF16) for c in "AB"] for i in range(2)]
        ktX = [[sb(f"kt{c}{i}", (128, 2, 128), BF16) for c in "AB"] for i in range(2)]
        kend = [sb(f"kend{i}", (128, 256), BF16) for i in range(2)]
        scT = sb("scT", (128, 4, 128), BF16)
        on = sb("on", (128, 512), BF16)
        S = sb("S", (128, 2, 128), F32)
        Sb = [sb(f"Sb{i}", (128, 2, 128), BF16) for i in range(2)]
        stat = sb("stat", (128, 4, 16), F32)
        stat4b = [sb(f"stat4{i}", (128, 16), F32) for i in range(2)]
        sg = sb("sg", (128, 2, 512), BF16)
        hT = [sb(f"hT{i}", (128, 2, 512), BF16) for i in range(2)]
        comb = sb("comb", (128, 4, 16), F32)
        rtb = [sb(f"rt{i}", (128, 128), F32) for i in range(2)]
        ps = st.enter_context(nc.psum_tensor("ps", [128, 7, 512], F32))

        dma_sems = ["ldc", "ldp"] + [f"ldx{p}{i}" for p in range(2) for i in range(4)] + [f"st{p}{i}" for p in range(2) for i in range(4)]
        for i in range(2):
            dma_sems += [f"wlg{i}", f"wld{i}", f"cvg{i}", f"cvu{i}", f"cvd{i}", f"csg{i}", f"csd{i}"]
        sems = {}
        for s in list(ENGS) + dma_sems:
            sems[s] = st.enter_context(nc.semaphore(s))
        P = Prog(nc, dma_sems=dma_sems)
        bankctr = [0]

        hard = [False]

        def stage(n):
            P.stopped = (STAGE < n) or hard[0]

        def nb():
            b = bankctr[0] % 7
            bankctr[0] += 1
            return b

        def pe_fence(b):
            P.op("pe", lambda e: e.matmul(ps[:, b, 508:512], lhsT=idb[0:1, :], rhs=idb[0:1, 0:4], start=True, stop=True),
                 reads=["idb"], writes=[f"ps{b}"])

        IDF = consts[:, 0, :]
        TRI_INC = consts[:, 1, :]
        TRI_STR = consts[:, 2, :]
        NEG16 = consts[:, 3, 0:1]
        CAUS4 = caus4[:]

        def ld(eng, out, in_, name, sem="ldc"):
            P.op(eng, lambda e: e.dma_start(out=out, in_=in_), writes=[name], dma_sem=sem)

        ld("sp", consts[:], consts_d, "consts")
        ld("sp", pmask[:], pmask_d, "pmask")
        ld("sp", w_gu[:], w_gu_d, "w_gu")
        ld("sp", gnw[:], gnw_d, "gnw")
        ld("sp", pscale[:], pscale_d, "pscale")
        ld("sp", nmw_pk[:], nmw_d, "nmw")
        ld("sp", nfw_pk[:], nfw_d, "nfw")
        ld("sp", fnw[:], fnw_d.partition_broadcast(128), "fnw")
        ld("sp", b_r[:], b_r_d.partition_broadcast(128), "b_r")
        ld("sp", w_r[:], w_r_d.rearrange("(kc p) n -> p kc n", p=128), "w_r")
        for kc in range(8):
            ld("pool", w_in[:, kc, :], w_in_d[kc * 128:(kc + 1) * 128, :], "w_in", "ldp")
        ld("pool", pool_w[:], pool_w_d.rearrange("g c d -> c g d"), "pool_w", "ldp")
        for kc in range(8):
            ld("pool", w_out[:, kc, :], w_out_d[kc * 128:(kc + 1) * 128, :], "w_out", "ldp")
        for name in ("consts", "pmask", "w_gu", "gnw", "pscale", "nmw", "nfw", "fnw", "b_r", "w_r"):
            P.lastw[name] = ("ldc", P.cnt["ldc"])
        for name in ("w_in", "pool_w", "w_out"):
            P.lastw[name] = ("ldp", P.cnt["ldp"])
        for kc in range(8):
            P.op("dve", lambda e: e.tensor_scalar(out=w_in[:, kc, :], in0=w_in[:, kc, :], scalar1=nmw_pk[:, kc:kc + 1],
                                                   scalar2=None, op0=ALU.mult), reads=["w_in", "nmw"], writes=["w_in"])
            P.op("dve", lambda e: e.tensor_scalar(out=w_r[:, kc, :], in0=w_r[:, kc, :], scalar1=nfw_pk[:, kc:kc + 1],
                                                  scalar2=None, op0=ALU.mult), reads=["w_r", "nfw"], writes=["w_r"])
        P.op("dve", lambda e: e.tensor_copy(out=idb[:], in_=IDF), reads=["consts"], writes=["idb"])
        for h_ in range(4):
            P.op("dve", lambda e: e.tensor_copy(out=caus4[:, h_, :], in_=consts[:, 4, :]), reads=["consts"], writes=["caus4"])
        P.op("dve", lambda e: e.memset(S[:], 0.0), writes=["S"])
        P.op("dve", lambda e: e.memset(g1[:], 1.0), writes=[f"g1_{i}" for i in range(4)])
        for p_ in range(2):
            for i_ in range(2):
                P.op("pool", lambda e: e.memset(qtX[p_][i_][:], 0.0), writes=[f"qt{p_}"])
                P.op("pool", lambda e: e.memset(ktX[p_][i_][:], 0.0), writes=[f"kt{p_}"])
        P.op("dve", lambda e: e.memset(pvT[:], 0.0), writes=["pvT"])

        cvt_tok = {}

        def convert_load(e_):
            s_ = e_ % NSLOT
            P.op("pool", lambda e: e.dma_start(out=wgu[s_][:, :, 0:256],
                                               in_=w_eg_d[e_].rearrange("(kc p) n -> p kc n", p=128)),
                 writes=[f"wgu{s_}a"], dma_sem=f"cvg{s_}")
            P.op("pool", lambda e: e.dma_start(out=wgu[s_][:, :, 256:512],
                                               in_=w_eu_d[e_].rearrange("(kc p) n -> p kc n", p=128)),
                 writes=[f"wgu{s_}b"], dma_sem=f"cvu{s_}")
            P.op("pool", lambda e: e.dma_start(out=wd[s_][:], in_=w_ed_d[e_].rearrange("(hc p) n -> p hc n", p=128)),
                 writes=[f"wd{s_}"], dma_sem=f"cvd{s_}")

        def convert_store(e_):
            s_ = e_ % NSLOT
            for kc in range(8):
                P.op("dve", lambda e: e.tensor_scalar(out=wgu[s_][:, kc, :], in0=wgu[s_][:, kc, :],
                                                      scalar1=nfw_pk[:, kc:kc + 1], scalar2=None, op0=ALU.mult),
                     reads=[f"wgu{s_}a", f"wgu{s_}b", "nfw"], writes=[f"wgu{s_}a", f"wgu{s_}b"])
            P.op("pool", lambda e: e.dma_start(out=wgu_s[e_], in_=wgu[s_][:]), reads=[f"wgu{s_}a", f"wgu{s_}b"],
                 writes=[f"wgus{e_}"], dma_sem=f"csg{s_}")
            P.op("pool", lambda e: e.dma_start(out=wd_s[e_], in_=wd[s_][:]), reads=[f"wd{s_}"],
                 writes=[f"wds{e_}"], dma_sem=f"csd{s_}")

        def run_streams(gens, width, stagger=1):
            it = iter(gens)
            active = []
            since = stagger
            done = False
            while True:
                if not done and len(active) < width and since >= stagger:
                    g = next(it, None)
                    if g is None:
                        done = True
                    else:
                        active.append(g)
                        since = 0
                if not active:
                    if done:
                        break
                    since = stagger
                    continue
                since += 1
                for g in list(reversed(active)):
                    try:
                        next(g)
                    except StopIteration:
                        active.remove(g)

        def rmsnorm(x_ap, xname, wb, wname, out_ap, oname, sidx):
            stn = f"stat{sidx}"
            P.op("dve", lambda e: e.memset(stat[:, sidx, 0:1], 0.0), writes=[stn])
            P.op("act", lambda e: e.activation(out=junk[:], in_=x_ap, func=AF.Square, accum_out=stat[:, sidx, 0:1]),
                 reads=[xname], writes=["junk", stn])
            P.op("act", lambda e: e.activation(out=stat[:, sidx, 1:2], in_=stat[:, sidx, 0:1], func=AF.Ln,
                                               scale=1.0 / D, bias=EPS), reads=[stn], writes=[stn])
            P.op("act", lambda e: e.activation(out=stat[:, sidx, 2:3], in_=stat[:, sidx, 1:2], func=AF.Exp, scale=-0.5),
                 reads=[stn], writes=[stn])
            if wb is None:
                P.op("dve", lambda e: e.tensor_scalar(out=out_ap, in0=x_ap, scalar1=stat[:, sidx, 2:3], scalar2=None,
                                                      op0=ALU.mult), reads=[xname, stn], writes=[oname])
            else:
                P.op("dve", lambda e: e.scalar_tensor_tensor(out=out_ap, in0=x_ap, scalar=stat[:, sidx, 2:3], in1=wb[:],
                                                             op0=ALU.mult, op1=ALU.mult),
                     reads=[xname, stn, wname], writes=[oname])

        def transpose_bf(src, sname, dst3, dname):
            for half in range(2):
                b = nb()
                for c in range(4):
                    kc = half * 4 + c
                    P.op("pe", lambda e: e.matmul(ps[:, b, c * 128:(c + 1) * 128],
                                                  lhsT=src[:, kc * 128:(kc + 1) * 128], rhs=idb[:], start=True, stop=True),
                         reads=[sname, "idb"], writes=[f"ps{b}"], sig=(c == 3))
                P.op("dve", lambda e: e.tensor_copy(
                    out=dst3[:, half * 4:(half + 1) * 4, :], in_=ps[:, b, :].rearrange("p (c n) -> p c n", c=4)),
                    reads=[f"ps{b}"], writes=[dname])

        def kv_tokmajor(c0, xtn):
            bk = nb()
            for kc in range(8):
                P.op("pe", lambda e: e.matmul(ps[:, bk, 0:256], lhsT=xnT[:, kc, c0:c0 + 128],
                                              rhs=w_in[:, kc, 768:1024], start=(kc == 0), stop=(kc == 7)),
                     reads=[xtn, "w_in"], writes=[f"ps{bk}"], sig=(kc == 7))
            bv = nb()
            for kc in range(8):
                P.op("pe", lambda e: e.matmul(ps[:, bv, 0:512], lhsT=xnT[:, kc, c0:c0 + 128],
                                              rhs=w_in[:, kc, 1024:1536], start=(kc == 0), stop=(kc == 7)),
                     reads=[xtn, "w_in"], writes=[f"ps{bv}"], sig=(kc == 7))
            return bk, bv

        def softplus_neg(gc0, g1name, par, mask_col=None):
            bl = nb()
            P.op("pe", lambda e: e.matmul(ps[:, bl, 0:256], lhsT=g1[0:17, gc0:gc0 + 128], rhs=w_gu[:, :],
                                          start=True, stop=True), reads=[g1name, "w_gu"], writes=[f"ps{bl}"], sig=False)
            pe_fence(bl)
            P.op("act", lambda e: e.activation(out=sp[par][:], in_=ps[:, bl, 0:256], func=AF.Exp, scale=-1.0),
                 reads=[f"ps{bl}"], writes=[f"sp{par}"])
            P.op("act", lambda e: e.activation(out=sp[par][:], in_=sp[par][:], func=AF.Ln, bias=1.0),
                 reads=[f"sp{par}"], writes=[f"sp{par}"])
            if mask_col is not None:
                P.op("dve", lambda e: e.tensor_scalar(out=sp[par][:], in0=sp[par][:], scalar1=pmask[:, mask_col:mask_col + 1],
                                                      scalar2=None, op0=ALU.mult),
                     reads=[f"sp{par}", "pmask"], writes=[f"sp{par}"])

        def kend_and_v(bk, bv, par):
            be = nb()
            P.op("pe", lambda e: e.matmul(ps[:, be, 0:256], lhsT=TRI_STR, rhs=sp[par][:], start=True, stop=True),
                 reads=["consts", f"sp{par}"], writes=[f"ps{be}"], sig=False)
            pe_fence(be)
            P.op("act", lambda e: e.activation(out=eend[par][:], in_=ps[:, be, 0:256], func=AF.Exp),
                 reads=[f"ps{be}"], writes=[f"eend{par}"])
            P.op("dve", lambda e: e.tensor_tensor(out=kend[par][:], in0=ps[:, bk, 0:256], in1=eend[par][:], op=ALU.mult),
                 reads=[f"ps{bk}", f"eend{par}"], writes=[f"kend{par}"])
            P.op("dve", lambda e: e.tensor_copy(out=vtok[par][:], in_=ps[:, bv, 0:512]),
                 reads=[f"ps{bv}"], writes=[f"vtok{par}"])

        def cum_decay(par, want_neg):
            bb = nb()
            for j in range(2):
                P.op("pe", lambda e: e.matmul(ps[:, bb, j * 128:(j + 1) * 128], lhsT=sp[par][:, j * 128:(j + 1) * 128],
                                              rhs=TRI_INC, start=True, stop=True),
                     reads=[f"sp{par}", "consts"], writes=[f"ps{bb}"], sig=False)
            pe_fence(bb)
            P.op("act", lambda e: e.activation(out=eb[par][:], in_=ps[:, bb, 0:256].rearrange("p (j n) -> p j n", j=2),
                                               func=AF.Exp), reads=[f"ps{bb}"], writes=[f"eb{par}"])
            if want_neg:
                P.op("act", lambda e: e.activation(out=enb[par][:], in_=ps[:, bb, 0:256].rearrange("p (j n) -> p j n", j=2),
                                                   func=AF.Exp, scale=-1.0), reads=[f"ps{bb}"], writes=[f"enb{par}"])

        def state_update(par):
            bs = nb()
            for j in range(2):
                P.op("pe", lambda e: e.matmul(ps[:, bs, j * 256:(j + 1) * 256], lhsT=kend[par][:, j * 128:(j + 1) * 128],
                                              rhs=vtok[par][:, j * 256:(j + 1) * 256], start=True, stop=True),
                     reads=[f"kend{par}", f"vtok{par}"], writes=[f"ps{bs}"], sig=(j == 1))
            for j in range(2):
                for hh in range(2):
                    r0 = hh * 64
                    P.op("dve", lambda e: e.scalar_tensor_tensor(
                        out=S[r0:r0 + 64, j, :], in0=S[r0:r0 + 64, j, :], scalar=eb[par][r0:r0 + 64, j, 127:128],
                        in1=ps[r0:r0 + 64, bs, j * 256 + hh * 128:j * 256 + hh * 128 + 128],
                        op0=ALU.mult, op1=ALU.add), reads=["S", f"eb{par}", f"ps{bs}"], writes=["S"])

        def prefix_tile(i):
            if i % 2 == 0 and i // 2 < NE:
                convert_load(i // 2)
            if i % 2 == 1 and i // 2 < NE:
                convert_store(i // 2)
            slot = i % 4
            par = i % 2
            c0 = slot * 128
            P.op("sp", lambda e: e.dma_start(out=x[:, slot, :], in_=xp_d[i * 128:(i + 1) * 128, :]),
                 writes=[f"x0{slot}"], dma_sem=f"ldx0{slot}")
            rmsnorm(x[:, slot, :], f"x0{slot}", None, None, xn[par][:], f"xn{par}", slot)
            yield
            transpose_bf(xn[par], f"xn{par}", xnT[:, :, c0:c0 + 128], f"xnT{slot}")
            yield
            bk, bv = kv_tokmajor(c0, f"xnT{slot}")
            bg = nb()
            for kc in range(8):
                P.op("pe", lambda e: e.matmul(ps[0:16, bg, 0:128], lhsT=w_in[:, kc, 2048:2064],
                                              rhs=xnT[:, kc, c0:c0 + 128], start=(kc == 0), stop=(kc == 7)),
                     reads=[f"xnT{slot}", "w_in"], writes=[f"ps{bg}"], sig=(kc == 7))
            if i == NPRE - 1:
                bh = nb()
                for g in range(4):
                    for kc in range(8):
                        P.op("pe", lambda e: e.matmul(ps[:, bh, g * 16:(g + 1) * 16],
                                                      lhsT=w_in[:, kc, g * 128:(g + 1) * 128],
                                                      rhs=xnT[:, kc, c0 + 112:c0 + 128],
                                                      start=(kc == 0), stop=(kc == 7)),
                             reads=[f"xnT{slot}", "w_in"], writes=[f"ps{bh}"], sig=(g == 3 and kc == 7))
                P.op("act", lambda e: e.activation(out=pvT[:, :, 0:16],
                                                   in_=ps[:, bh, 0:64].rearrange("p (g n) -> p g n", g=4), func=AF.Copy),
                     reads=[f"ps{bh}"], writes=["pvT"])
            P.op("dve", lambda e: e.tensor_copy(out=ktok[i % 3][:], in_=ps[:, bk, 0:256]),
                 reads=[f"ps{bk}"], writes=[f"ktok{i % 3}"])
            P.op("dve", lambda e: e.tensor_copy(out=vtok[slot][:], in_=ps[:, bv, 0:512]),
                 reads=[f"ps{bv}"], writes=[f"vtok{slot}"])
            P.op("act", lambda e: e.activation(out=g1[0:16, c0:c0 + 128], in_=ps[0:16, bg, 0:128], func=AF.Copy),
                 reads=[f"ps{bg}"], writes=[f"g1_{slot}"])
            yield
            softplus_neg(c0, f"g1_{slot}", par, mask_col=i)
            yield
            be = nb()
            P.op("pe", lambda e: e.matmul(ps[:, be, 0:256], lhsT=TRI_STR, rhs=sp[par][:], start=True, stop=True),
                 reads=["consts", f"sp{par}"], writes=[f"ps{be}"], sig=False)
            pe_fence(be)
            bb = nb()
            for j in range(2):
                P.op("pe", lambda e: e.matmul(ps[:, bb, j * 128:(j + 1) * 128], lhsT=sp[par][:, j * 128:(j + 1) * 128],
                                              rhs=TRI_INC, start=True, stop=True),
                     reads=[f"sp{par}", "consts"], writes=[f"ps{bb}"], sig=False)
            pe_fence(bb)
            P.op("act", lambda e: e.activation(out=eend[par][:], in_=ps[:, be, 0:256], func=AF.Exp),
                 reads=[f"ps{be}"], writes=[f"eend{par}"])
            P.op("act", lambda e: e.activation(out=eb[par][:], in_=ps[:, bb, 0:256].rearrange("p (j n) -> p j n", j=2),
                                               func=AF.Exp), reads=[f"ps{bb}"], writes=[f"eb{par}"])
            P.op("dve", lambda e: e.tensor_tensor(out=kend[par][:], in0=ktok[i % 3][:], in1=eend[par][:], op=ALU.mult),
                 reads=[f"ktok{i % 3}", f"eend{par}"], writes=[f"kend{par}"])
            yield
            bs = nb()
            for j in range(2):
                P.op("pe", lambda e: e.matmul(ps[:, bs, j * 256:(j + 1) * 256], lhsT=kend[par][:, j * 128:(j + 1) * 128],
                                              rhs=vtok[slot][:, j * 256:(j + 1) * 256], start=True, stop=True),
                     reads=[f"kend{par}", f"vtok{slot}"], writes=[f"ps{bs}"], sig=(j == 1))
            for j in range(2):
                for hh in range(2):
                    r0 = hh * 64
                    P.op("dve", lambda e: e.scalar_tensor_tensor(
                        out=S[r0:r0 + 64, j, :], in0=S[r0:r0 + 64, j, :], scalar=eb[par][r0:r0 + 64, j, 127:128],
                        in1=ps[r0:r0 + 64, bs, j * 256 + hh * 128:j * 256 + hh * 128 + 128],
                        op0=ALU.mult, op1=ALU.add), reads=["S", f"eb{par}", f"ps{bs}"], writes=["S"])

        run_streams([prefix_tile(i) for i in range(NPRE)], PRE_WIDTH, 1)
        for e_ in range(NE):
            if e_ == NPRE // 2 and NPRE % 2 == 1:
                convert_store(e_)
            elif e_ >= (NPRE + 1) // 2:
                convert_load(e_)
                convert_store(e_)
        P.op("act", lambda e: e.activation(out=Sb[0][:], in_=S[:], func=AF.Copy), reads=["S"], writes=["Sb0"])

        XT = [f"xnT{t}" for t in range(4)]
        G1ALL = [f"g1_{t}" for t in range(4)]
        out_toks = []

        def load_expert(ge):
            e_ = ge % NE
            s_ = ge % NSLOT
            P.op("sp", lambda e: e.dma_start(out=wgu[s_][:], in_=wgu_s[e_]), reads=[f"wgus{e_}"],
                 writes=[f"wgu{s_}a", f"wgu{s_}b"], dma_sem=f"wlg{s_}")
            P.op("sp", lambda e: e.dma_start(out=wd[s_][:], in_=wd_s[e_]), reads=[f"wds{e_}"],
                 writes=[f"wd{s_}"], dma_sem=f"wld{s_}")

        NGE = NBLK * NE
        load_expert(0)

        def front_tile(blk, t):
            r0 = blk * 512 + t * 128
            par = t % 2
            pb_ = blk % 2
            x = xb[pb_]
            P.op("sp", lambda e: e.dma_start(out=x[:, t, :], in_=xm_d[r0:r0 + 128, :]),
                 writes=[f"x{pb_}{t}"], dma_sem=f"ldx{pb_}{t}")
            rmsnorm(x[:, t, :], f"x{pb_}{t}", None, None, xn[par][:], f"xn{par}", t)
            yield
            transpose_bf(xn[par], f"xn{par}", xnT[:, :, t * 128:(t + 1) * 128], f"xnT{t}")

        def proj_chunk(col0, m):
            b = nb()
            for kc in range(8):
                P.op("pe", lambda e: e.matmul(ps[0:m, b, :], lhsT=w_in[:, kc, col0:col0 + m], rhs=xnT[:, kc, :],
                                              start=(kc == 0), stop=(kc == 7)),
                     reads=XT + ["w_in"], writes=[f"ps{b}"], sig=(kc == 7))
            return b

        def block_front(blk):
            fts = [front_tile(blk, t) for t in range(4)]
            next(fts[0])
            yield
            for t in range(4):
                if t + 1 < 4:
                    next(fts[t + 1])
                    yield
                for _ in fts[t]:
                    pass
                yield
            for g in range(4):
                b = proj_chunk(g * 128, 128)
                P.op("act", lambda e: e.activation(out=pvT[:, g, 16:528], in_=ps[:, b, :], func=AF.Copy),
                     reads=[f"ps{b}"], writes=["pvT"])
                yield
            for j in range(2):
                b = proj_chunk(512 + j * 128, 128)
                P.op("act", lambda e: e.activation(out=qT[:, j, :], in_=ps[:, b, :], func=AF.Copy),
                     reads=[f"ps{b}"], writes=["qT"])
                yield
            for j in range(2):
                b = proj_chunk(768 + j * 128, 128)
                P.op("act", lambda e: e.activation(out=kT[:, j, :], in_=ps[:, b, :], func=AF.Copy),
                     reads=[f"ps{b}"], writes=["kT"])
                yield
            b = proj_chunk(2048, 16)
            P.op("act", lambda e: e.activation(out=g1[0:16, :], in_=ps[0:16, b, :], func=AF.Copy),
                 reads=[f"ps{b}"], writes=G1ALL)
            for h in range(4):
                b = proj_chunk(1536 + h * 128, 128)
                P.op("act", lambda e: e.activation(out=srT[:, h, :], in_=ps[:, b, :], func=AF.Silu),
                     reads=[f"ps{b}"], writes=["srT"])
                yield
            for g in range(4):
                p_ = pvT[:, g, :]
                w_ = 2 << g
                P.op("pool", lambda e: e.tensor_tensor(out=pa[:, 1:528], in0=p_[:, 1:528], in1=p_[:, 0:527], op=ALU.add),
                     reads=["pvT"], writes=["pa"])
                cur, curname = pa, "pa"
                if g >= 1:
                    P.op("pool", lambda e: e.tensor_tensor(out=pb[:, 3:528], in0=pa[:, 3:528], in1=pa[:, 1:526], op=ALU.add),
                         reads=["pa"], writes=["pb"])
                    cur, curname = pb, "pb"
                if g >= 2:
                    P.op("pool", lambda e: e.tensor_tensor(out=pa[:, 7:528], in0=pb[:, 7:528], in1=pb[:, 3:524], op=ALU.add),
                         reads=["pb"], writes=["pa"])
                    cur, curname = pa, "pa"
                if g >= 3:
                    P.op("pool", lambda e: e.tensor_tensor(out=pb[:, 15:528], in0=pa[:, 15:528], in1=pa[:, 7:520], op=ALU.add),
                         reads=["pa"], writes=["pb"])
                    cur, curname = pb, "pb"
                P.op("dve", lambda e: e.scalar_tensor_tensor(
                    out=pooledT[:, g, :], in0=cur[:, 16:528], scalar=1.0 / w_, in1=p_[:, 16:528],
                    op0=ALU.mult, op1=ALU.subtract), reads=[curname, "pvT"], writes=["pooledT"])
                yield
            P.op("pool", lambda e: e.tensor_copy(out=pvT[:, :, 0:16], in_=pvT[:, :, 512:528]), reads=["pvT"], writes=["pvT"])
            for g in range(4):
                b = nb()
                P.op("pe", lambda e: e.matmul(ps[:, b, :], lhsT=pool_w[:, g, :], rhs=pooledT[:, g, :], start=True, stop=True),
                     reads=["pool_w", "pooledT"], writes=[f"ps{b}"])
                P.op("act", lambda e: e.activation(out=ymT[:, g, :], in_=ps[:, b, :], func=AF.Copy, scale=pscale[:, g:g + 1]),
                     reads=[f"ps{b}", "pscale"], writes=["ymTp"])
            yield

        tile_ctr = [0]

        def main_tile(blk, t):
            c0 = t * 128
            par = tile_ctr[0] % 2
            tile_ctr[0] += 1
            stat4 = stat4b[par]
            st4n = f"stat4{par}"
            pb_ = blk % 2
            x = xb[pb_]
            xn_ = f"x{pb_}{t}"
            bk, bv = kv_tokmajor(c0, f"xnT{t}")
            softplus_neg(c0, f"g1_{t}", par)
            kend_and_v(bk, bv, par)
            cum_decay(par, True)
            for i_ in range(2):
                r0 = i_ * 64
                P.op("dve", lambda e: e.scalar_tensor_tensor(out=qtX[par][i_][r0:r0 + 64], in0=qT[r0:r0 + 64, :, c0:c0 + 128],
                                                             scalar=0.125, in1=eb[par][r0:r0 + 64], op0=ALU.mult, op1=ALU.mult),
                     reads=["qT", f"eb{par}"], writes=[f"qt{par}"])
                P.op("dve", lambda e: e.tensor_tensor(out=ktX[par][i_][r0:r0 + 64], in0=kT[r0:r0 + 64, :, c0:c0 + 128],
                                                      in1=enb[par][r0:r0 + 64], op=ALU.mult),
                     reads=["kT", f"enb{par}"], writes=[f"kt{par}"])
            yield
            bsc = nb()
            for h in range(4):
                j = h // 2
                P.op("pe", lambda e: e.matmul(ps[:, bsc, h * 128:(h + 1) * 128], lhsT=ktX[par][h % 2][:, j, :],
                                              rhs=qtX[par][h % 2][:, j, :], start=True, stop=True),
                     reads=[f"kt{par}", f"qt{par}"], writes=[f"ps{bsc}"], sig=(h == 3))
            P.op("dve", lambda e: e.tensor_tensor(out=scT[:], in0=ps[:, bsc, :].rearrange("p (h n) -> p h n", h=4),
                                                  in1=CAUS4, op=ALU.mult),
                 reads=[f"ps{bsc}", "caus4"], writes=["scT"])
            yield
            bo = nb()
            for h in range(4):
                j = h // 2
                P.op("pe", lambda e: e.matmul(ps[:, bo, h * 128:(h + 1) * 128], lhsT=scT[:, h, :],
                                              rhs=vtok[par][:, h * 128:(h + 1) * 128], start=True, stop=False),
                     reads=["scT", f"vtok{par}"], writes=[f"ps{bo}"], sig=False)
                P.op("pe", lambda e: e.matmul(ps[:, bo, h * 128:(h + 1) * 128], lhsT=qtX[par][h % 2][:, j, :],
                                              rhs=Sb[par][:, j, :], start=False, stop=True),
                     reads=[f"qt{par}", f"Sb{par}"], writes=[f"ps{bo}"], sig=(h == 3))
            state_update(par)
            P.op("act", lambda e: e.activation(out=Sb[1 - par][:], in_=S[:], func=AF.Copy),
                 reads=["S"], writes=[f"Sb{1 - par}"])
            P.op("dve", lambda e: e.memset(stat4[:, 0:4], 0.0), writes=[st4n])
            for h in range(4):
                P.op("act", lambda e: e.activation(out=junk[:, 0:128], in_=ps[:, bo, h * 128:(h + 1) * 128],
                                                   func=AF.Square, accum_out=stat4[:, h:h + 1]),
                     reads=[f"ps{bo}"], writes=["junk", st4n])
            P.op("act", lambda e: e.activation(out=stat4[:, 4:8], in_=stat4[:, 0:4], func=AF.Ln, scale=1.0 / 128,
                                               bias=EPS), reads=[st4n], writes=[st4n])
            P.op("act", lambda e: e.activation(out=stat4[:, 8:12], in_=stat4[:, 4:8], func=AF.Exp, scale=-0.5),
                 reads=[st4n], writes=[st4n])
            for h in range(4):
                P.op("act", lambda e: e.activation(out=on[:, h * 128:(h + 1) * 128], in_=ps[:, bo, h * 128:(h + 1) * 128],
                                                   func=AF.Copy, scale=stat4[:, 8 + h:9 + h]),
                     reads=[f"ps{bo}", st4n], writes=["on"])
            yield
            bt = nb()
            for h in range(4):
                P.op("pe", lambda e: e.matmul(ps[:, bt, h * 128:(h + 1) * 128], lhsT=on[:, h * 128:(h + 1) * 128],
                                              rhs=idb[:], start=True, stop=True),
                     reads=["on", "idb"], writes=[f"ps{bt}"], sig=(h == 3))
            P.op("dve", lambda e: e.scalar_tensor_tensor(out=ymT[:, 4:8, c0:c0 + 128],
                                                         in0=ps[:, bt, :].rearrange("p (h n) -> p h n", h=4),
                                                         scalar=gnw[:, 0:1], in1=srT[:, :, c0:c0 + 128],
                                                         op0=ALU.mult, op1=ALU.mult),
                 reads=[f"ps{bt}", "gnw", "srT"], writes=[f"ymTg{t}"])
            yield
            for half in range(2):
                b = nb()
                for kc in range(8):
                    P.op("pe", lambda e: e.matmul(
                        ps[:, b, :], lhsT=ymT[:, kc, c0:c0 + 128], rhs=w_out[:, kc, half * 512:(half + 1) * 512],
                        start=(kc == 0), stop=(kc == 7)), reads=["ymTp", f"ymTg{t}", "w_out"], writes=[f"ps{b}"],
                        sig=(kc == 7))
                P.op("dve", lambda e: e.tensor_tensor(
                    out=x[:, t, half * 512:(half + 1) * 512], in0=ps[:, b, :], in1=x[:, t, half * 512:(half + 1) * 512],
                    op=ALU.add), reads=[f"ps{b}", xn_], writes=[xn_])
            if not OVERLAP:
                yield
                for _ in main_tile_b(blk, t):
                    yield

        def main_tile_b(blk, t):
            c0 = t * 128
            par = t % 2
            rt = rtb[par]
            rtn = f"rt{par}"
            pb_ = blk % 2
            x = xb[pb_]
            rmsnorm(x[:, t, :], f"x{pb_}{t}", None, None, xn2f[:], "xn2f", t)
            yield
            for half in range(2):
                b = nb()
                for c in range(4):
                    kc = half * 4 + c
                    P.op("pe", lambda e: e.matmul(ps[:, b, c * 128:(c + 1) * 128],
                                                  lhsT=xn2f[:, kc * 128:(kc + 1) * 128], rhs=IDF, start=True, stop=True),
                         reads=["xn2f", "consts"], writes=[f"ps{b}"], sig=(c == 3))
                P.op("dve", lambda e: e.tensor_copy(
                    out=xn2Tf[:, half * 4:(half + 1) * 4, :], in_=ps[:, b, :].rearrange("p (c n) -> p c n", c=4)),
                    reads=[f"ps{b}"], writes=["xn2Tf"])
                P.op("act", lambda e: e.activation(
                    out=xn2T[:, half * 4:(half + 1) * 4, c0:c0 + 128], in_=xn2Tf[:, half * 4:(half + 1) * 4, :],
                    func=AF.Copy), reads=["xn2Tf"], writes=[f"xn2T{t}"])
            yield
            br = nb()
            for kc in range(8):
                P.op("pe", lambda e: e.matmul(ps[:, br, 0:20], lhsT=xn2Tf[:, kc, :], rhs=w_r[:, kc, :],
                                              start=(kc == 0), stop=(kc == 7)),
                     reads=["xn2Tf", "w_r"], writes=[f"ps{br}"], sig=False)
            pe_fence(br)
            V = lambda fn: P.op("dve", fn, reads=[rtn], writes=[rtn])
            P.op("dve", lambda e: e.tensor_tensor(out=rt[:, 0:20], in0=ps[:, br, 0:20], in1=b_r[:], op=ALU.add),
                 reads=[f"ps{br}", "b_r"], writes=[rtn])
            V(lambda e: e.tensor_reduce(out=rt[:, 20:21], in_=rt[:, 0:4], axis=AX.X, op=ALU.max))
            V(lambda e: e.tensor_scalar(out=rt[:, 24:28], in0=rt[:, 0:4], scalar1=rt[:, 20:21], scalar2=None, op0=ALU.is_equal))
            V(lambda e: e.tensor_scalar(out=rt[:, 21:22], in0=rt[:, 20:21], scalar1=-1.0, scalar2=None, op0=ALU.mult))
            P.op("dve", lambda e: e.memset(rt[:, 22:23], 0.0), reads=[rtn], writes=[rtn])
            P.op("act", lambda e: e.activation(out=rt[:, 28:32], in_=rt[:, 0:4], func=AF.Exp, bias=rt[:, 21:22],
                                               accum_out=rt[:, 22:23]), reads=[rtn], writes=[rtn])
            V(lambda e: e.reciprocal(out=rt[:, 23:24], in_=rt[:, 22:23]))
            V(lambda e: e.tensor_scalar(out=rt[:, 32:36], in0=rt[:, 24:28], scalar1=BIG, scalar2=-BIG, op0=ALU.mult, op1=ALU.add))
            for g in range(4):
                V(lambda e: e.tensor_scalar(out=rt[:, 40 + 4 * g:44 + 4 * g], in0=rt[:, 4 + 4 * g:8 + 4 * g],
                                            scalar1=rt[:, 32 + g:33 + g], scalar2=None, op0=ALU.add))
            V(lambda e: e.tensor_reduce(out=rt[:, 36:37], in_=rt[:, 40:56], axis=AX.X, op=ALU.max))
            V(lambda e: e.tensor_scalar(out=rt[:, 56:72], in0=rt[:, 40:56], scalar1=rt[:, 36:37], scalar2=None, op0=ALU.is_equal))
            V(lambda e: e.scalar_tensor_tensor(out=rt[:, 72:88], in0=rt[:, 56:72], scalar=-BIG, in1=rt[:, 40:56],
                                               op0=ALU.mult, op1=ALU.add))
            V(lambda e: e.tensor_reduce(out=rt[:, 37:38], in_=rt[:, 72:88], axis=AX.X, op=ALU.max))
            V(lambda e: e.tensor_scalar(out=rt[:, 88:104], in0=rt[:, 72:88], scalar1=rt[:, 37:38], scalar2=None, op0=ALU.is_equal))
            V(lambda e: e.tensor_tensor(out=rt[:, 38:39], in0=rt[:, 37:38], in1=rt[:, 36:37], op=ALU.subtract))
            P.op("act", lambda e: e.activation(out=rt[:, 39:40], in_=rt[:, 38:39], func=AF.Exp), reads=[rtn], writes=[rtn])
            V(lambda e: e.tensor_scalar(out=rt[:, 104:105], in0=rt[:, 39:40], scalar1=1.0, scalar2=None, op0=ALU.add))
            V(lambda e: e.reciprocal(out=rt[:, 105:106], in_=rt[:, 104:105]))
            V(lambda e: e.tensor_tensor(out=rt[:, 106:107], in0=rt[:, 105:106], in1=rt[:, 23:24], op=ALU.mult))
            V(lambda e: e.tensor_tensor(out=rt[:, 107:108], in0=rt[:, 23:24], in1=rt[:, 106:107], op=ALU.subtract))
            V(lambda e: e.tensor_scalar(out=rt[:, 108:124], in0=rt[:, 56:72], scalar1=rt[:, 106:107], scalar2=None, op0=ALU.mult))
            P.op("dve", lambda e: e.scalar_tensor_tensor(out=comb[:, t, :], in0=rt[:, 88:104], scalar=rt[:, 107:108],
                                                         in1=rt[:, 108:124], op0=ALU.mult, op1=ALU.add),
                 reads=[rtn], writes=[f"comb{t}"])

        X2T = [f"xn2T{t}" for t in range(4)]

        def experts(blk):
            pb_ = blk % 2
            x = xb[pb_]
            for e_ in range(NE):
                ge = blk * NE + e_
                s_ = ge % NSLOT
                if ge + 1 < NGE:
                    load_expert(ge + 1)
                hp = ge % 2
                for hc in range(2):
                    bb2 = []
                    for gu in range(2):
                        b = nb()
                        bb2.append(b)
                        col = gu * 256 + hc * 128
                        for kc in range(8):
                            P.op("pe", lambda e: e.matmul(
                                ps[:, b, :], lhsT=wgu[s_][:, kc, col:col + 128], rhs=xn2T[:, kc, :],
                                start=(kc == 0), stop=(kc == 7)), reads=X2T + [f"wgu{s_}" + "ab"[gu]], writes=[f"ps{b}"],
                                sig=(kc == 7))
                    bg_, bu_ = bb2
                    P.op("act", lambda e: e.activation(out=sg[:, hc, :], in_=ps[:, bg_, :], func=AF.Silu),
                         reads=[f"ps{bg_}"], writes=[f"sg{hc}"])
                    P.op("dve", lambda e: e.tensor_tensor(out=hT[hp][:, hc, :], in0=ps[:, bu_, :], in1=sg[:, hc, :], op=ALU.mult),
                         reads=[f"ps{bu_}", f"sg{hc}"], writes=[f"hT{hp}"])
                for t in range(4):
                    for half in range(2):
                        b = nb()
                        for hc in range(2):
                            P.op("pe", lambda e: e.matmul(
                                ps[:, b, :], lhsT=hT[hp][:, hc, t * 128:(t + 1) * 128],
                                rhs=wd[s_][:, hc, half * 512:(half + 1) * 512], start=(hc == 0), stop=(hc == 1)),
                                reads=[f"hT{hp}", f"wd{s_}"], writes=[f"ps{b}"], sig=(hc == 1))
                        P.op("dve", lambda e: e.scalar_tensor_tensor(
                            out=x[:, t, half * 512:(half + 1) * 512], in0=ps[:, b, :], scalar=comb[:, t, e_:e_ + 1],
                            in1=x[:, t, half * 512:(half + 1) * 512], op0=ALU.mult, op1=ALU.add),
                            reads=[f"ps{b}", f"comb{t}", f"x{pb_}{t}"], writes=[f"x{pb_}{t}"])
                yield

        def final(blk):
            pb_ = blk % 2
            x = xb[pb_]
            for t in range(4):
                r0 = blk * 512 + t * 128
                rmsnorm(x[:, t, :], f"x{pb_}{t}", fnw, "fnw", x[:, t, :], f"x{pb_}{t}", t)
                out_toks.append(P.op("sp", lambda e: e.dma_start(out=out_d[r0:r0 + 128, :], in_=x[:, t, :]),
                                     reads=[f"x{pb_}{t}"], dma_sem=f"st{pb_}{t}"))

        def mixer1(blk):
            for _ in block_front(blk):
                yield
            gens = [main_tile(blk, t) for t in range(4)]
            active, nxt, since = [], 0, MIX_STAGGER
            while active or nxt < 4:
                if nxt < 4 and len(active) < MIX_WIDTH and since >= MIX_STAGGER:
                    active.append(gens[nxt])
                    nxt += 1
                    since = 0
                since += 1
                for g in list(reversed(active)):
                    try:
                        next(g)
                    except StopIteration:
                        active.remove(g)
                yield

        def part2(blk):
            if OVERLAP:
                run_streams([main_tile_b(blk, t) for t in range(4)], 2, 2)

        if OVERLAP:
            for _ in mixer1(0):
                pass
            part2(0)
            for blk in range(NBLK):
                mg = mixer1(blk + 1) if blk + 1 < NBLK else None
                for _ in experts(blk):
                    if mg is not None:
                        for _k in range(4):
                            if next(mg, "done") == "done":
                                mg = None
                                break
                final(blk)
                if mg is not None:
                    for _ in mg:
                        pass
                if blk + 1 < NBLK:
                    part2(blk + 1)
        else:
            for blk in range(NBLK):
                for _ in mixer1(blk):
                    pass
                part2(blk)
                for _ in experts(blk):
                    pass
                final(blk)
        P.finish("sp", out_toks)
        P.emit(sems)
    return nc


def make_consts():
    c = np.zeros((128, 5, 128), np.float32)
    s = np.arange(128)[:, None]
    cc = np.arange(128)[None, :]
    c[:, 0] = np.eye(128, dtype=np.float32)
    c[:, 1] = np.where(s <= cc, -1.0 / 16, 0.0)
    c[:, 2] = np.where(s > cc, -1.0 / 16, 0.0)
    c[:, 3] = -1.0 / 16
    c[:, 4] = (s <= cc).astype(np.float32)
    return c


def make_in_maps(inp, seq):
    B = inp["x"].shape[0]
    half_len = seq // 2
    NPRE = half_len // 128 + 1
    f = lambda a: np.ascontiguousarray(np.asarray(a, dtype=np.float32))
    x = f(inp["x"])
    meta = f(inp["meta_tokens"])
    shared = {
        "w_in": f(inp["w_in"][0]),
        "w_gu_b": f(np.concatenate([inp["w_gate_up"][0], inp["b_gate"][0][None, :]], axis=0)),
        "gnw": f(inp["gla_norm_w"][0].reshape(128, 1)),
        "pool_w": f(inp["pool_w"][0]),
        "pscale": f(inp["pool_scale"][0].reshape(4, 128).T),
        "w_out": f(inp["w_out"][0]),
        "nmw_pk": f(inp["norm_mix_w"][0].reshape(8, 128).T),
        "nfw_pk": f(inp["norm_ffn_w"][0].reshape(8, 128).T),
        "fnw": f(inp["final_norm_w"]),
        "w_r": f(np.concatenate([inp["w_router_group"][0], inp["w_router_expert"][0]], axis=1)),
        "b_r": f(np.concatenate([inp["b_router_group"][0], inp["b_router_expert"][0]], axis=0)),
        "w_eg": f(inp["w_expert_gate"][0]),
        "w_eu": f(inp["w_expert_up"][0]),
        "w_ed": f(inp["w_expert_down"][0]),
        "consts": make_consts(),
    }
    maps = []
    for b in range(B):
        for half in range(2):
            xp = np.zeros((NPRE * 128, D), np.float32)
            mask = np.zeros((NPRE * 128,), np.float32)
            if half == 0:
                xp[-16:] = meta
                mask[-16:] = 1.0
            else:
                xp[112:128] = meta
                xp[128:] = x[b, :half_len]
                mask[112:] = 1.0
            m = dict(shared)
            m["xp"] = xp
            m["xm"] = np.ascontiguousarray(x[b, half * half_len:(half + 1) * half_len])
            m["pmask"] = np.ascontiguousarray(mask.reshape(NPRE, 128).T)
            maps.append(m)
    return maps, NPRE, half_len // 512


def kernel(**inputs):
    x = np.asarray(inputs["x"])
    B, seq, _ = x.shape
    maps, NPRE, NBLK = make_in_maps(inputs, seq)
    nc = build(NPRE, NBLK)
    res = run_bass_kernel_spmd(nc, maps, core_ids=list(range(len(maps))))
    half_len = seq // 2
    out = np.empty((B, seq, D), np.float32)
    for b in range(B):
        for half in range(2):
            out[b, half * half_len:(half + 1) * half_len] = res.results[2 * b + half]["out"]
    return out
```

```python
import numpy as np
from contextlib import ExitStack
import concourse.bass as bass
import concourse.mybir as mybir
from concourse.bass_utils import run_bass_kernel_spmd

F32 = mybir.dt.float32
BF16 = mybir.dt.bfloat16
AF = mybir.ActivationFunctionType
ALU = mybir.AluOpType
AX = mybir.AxisListType

D = 1024
NIN = 2064
NE = 16
EH = 256
EPS = 1e-6
BIG = 1.0e4
ENGS = ("pe", "act", "dve", "pool", "sp")
OVERLAP = False
MIX_WIDTH = 5
MIX_STAGGER = 2
PRE_WIDTH = 6


class _Rec:
    def __init__(self):
        self.call = None

    def __getattr__(self, name):
        def f(*a, **k):
            self.call = (name, a, k)
            return self
        return f


class Prog:
    def __init__(self, nc, dma_sems=()):
        self.nc = nc
        self.ops = {e: [] for e in ENGS}
        self.cnt = {e: 0 for e in ENGS}
        for s in dma_sems:
            self.cnt[s] = 0
        self.seen = {e: {} for e in ENGS}
        self.lastw = {}
        self.readers = {}
        self.stopped = False
        import os
        self.debug = bool(os.environ.get("TRKDEBUG"))

    def op(self, eng, fn, reads=(), writes=(), sig=True, dma_sem=None):
        if self.stopped:
            return None
        waits = {}

        def need(tok):
            if tok is None:
                return
            s, v = tok
            if s == eng and eng == "pe":
                return
            if self.seen[eng].get(s, 0) >= v:
                return
            if waits.get(s, 0) < v:
                waits[s] = v

        for b in reads:
            need(self.lastw.get(b))
        for b in writes:
            need(self.lastw.get(b))
            for t in self.readers.get(b, ()):
                need(t)
        for s, v in waits.items():
            self.seen[eng][s] = v
        if dma_sem is not None:
            self.cnt[dma_sem] += 16
            tok = (dma_sem, self.cnt[dma_sem])
            inc = (dma_sem, 16)
        elif sig:
            self.cnt[eng] += 1
            tok = (eng, self.cnt[eng])
            inc = (eng, 1)
        else:
            tok = (eng, self.cnt[eng] + 1)
            inc = None
        for b in reads:
            self.readers.setdefault(b, []).append(tok)
        for b in writes:
            self.lastw[b] = tok
            self.readers[b] = []
        rec = _Rec()
        fn(rec)
        call = rec.call
        self.ops[eng].append((sorted(waits.items()), call, inc))
        if self.debug:
            import sys
            ln = sys._getframe(1).f_lineno
            print(f"OP {eng:4s} L{ln} waits={sorted(waits.items())} tok={tok} r={list(reads)} w={list(writes)}")
        return tok

    def finish(self, eng, toks):
        w = {}
        for tk in toks:
            if tk is None:
                continue
            s, v = tk
            w[s] = max(w.get(s, 0), v)
        for s in self.cnt:
            if self.cnt[s] > 0:
                w[s] = max(w.get(s, 0), self.cnt[s])
        self.ops[eng].append((sorted(w.items()), None, None))

    def emit(self, sems):
        nc = self.nc
        engobj = {"pe": "tensor", "act": "scalar", "dve": "vector", "pool": "gpsimd", "sp": "sync"}
        with nc.Block() as block:
            for e in ENGS:
                ops = self.ops[e]

                def body(eng, ops=ops):
                    for waits, fn, inc in ops:
                        for s, v in waits:
                            eng.wait_ge(sems[s], v)
                        if fn is None:
                            continue
                        ins = getattr(eng, fn[0])(*fn[1], **fn[2])
                        if inc is not None:
                            ins.then_inc(sems[inc[0]], inc[1])

                getattr(block, engobj[e])(body)


def build(NPRE, NBLK, STAGE=99):
    nc = bass.Bass("TRN2", target_bir_lowering=False)
    TP, TM = NPRE * 128, NBLK * 512
    dt_in = lambda name, shape: nc.dram_tensor(name, list(shape), F32, kind="ExternalInput").ap()
    xp_d = dt_in("xp", (TP, D))
    xm_d = dt_in("xm", (TM, D))
    pmask_d = dt_in("pmask", (128, NPRE))
    w_in_d = dt_in("w_in", (D, NIN))
    w_gu_d = dt_in("w_gu_b", (17, 256))
    gnw_d = dt_in("gnw", (128, 1))
    pool_w_d = dt_in("pool_w", (4, 128, 128))
    pscale_d = dt_in("pscale", (128, 4))
    w_out_d = dt_in("w_out", (D, D))
    nmw_d = dt_in("nmw_pk", (128, 8))
    nfw_d = dt_in("nfw_pk", (128, 8))
    fnw_d = dt_in("fnw", (D,))
    w_r_d = dt_in("w_r", (D, 20))
    b_r_d = dt_in("b_r", (20,))
    w_eg_d = dt_in("w_eg", (NE, D, EH))
    w_eu_d = dt_in("w_eu", (NE, D, EH))
    w_ed_d = dt_in("w_ed", (NE, EH, D))
    consts_d = dt_in("consts", (128, 5, 128))
    out_d = nc.dram_tensor("out", [TM, D], F32, kind="ExternalOutput").ap()
    wgu_s = nc.dram_tensor("wgu_scratch", [NE, 128, 8, 512], BF16, kind="Internal").ap()
    wd_s = nc.dram_tensor("wd_scratch", [NE, 128, 2, D], BF16, kind="Internal").ap()

    with ExitStack() as st:
        sb = lambda name, shape, dt: st.enter_context(nc.sbuf_tensor(name, list(shape), dt))
        w_in = sb("w_in_sb", (128, 8, NIN), BF16)
        w_out = sb("w_out_sb", (128, 8, D), BF16)
        pool_w = sb("pool_w_sb", (128, 4, 128), BF16)
        w_r = sb("w_r_sb", (128, 8, 20), F32)
        b_r = sb("b_r_sb", (128, 20), F32)
        w_gu = sb("w_gu_sb", (17, 256), F32)
        gnw = sb("gnw_sb", (128, 1), F32)
        pscale = sb("pscale_sb", (128, 4), F32)
        pmask = sb("pmask_sb", (128, NPRE), F32)
        fnw = sb("fnw_b", (128, D), F32)
        consts = sb("consts_sb", (128, 5, 128), F32)
        caus4 = sb("caus4", (128, 4, 128), BF16)
        idb = sb("idb", (128, 128), BF16)
        NSLOT = 2
        wgu = [sb(f"wgu{i}", (128, 8, 512), BF16) for i in range(NSLOT)]
        wd = [sb(f"wd{i}", (128, 2, D), BF16) for i in range(NSLOT)]
        xb = [sb(f"x{i}", (128, 4, D), F32) for i in range(2)]
        x = xb[0]
        xn2T = sb("xn2T", (128, 8, 512), BF16)
        nmw_pk = sb("nmw_pk_sb", (128, 8), F32)
        nfw_pk = sb("nfw_pk_sb", (128, 8), F32)
        junk = sb("junk", (128, D), BF16)
        xn = [sb(f"xn{i}", (128, D), BF16) for i in range(2)]
        xnT = sb("xnT", (128, 8, 512), BF16)
        xn2f = sb("xn2f", (128, D), F32)
        xn2Tf = sb("xn2Tf", (128, 8, 128), F32)
        pvT = sb("pvT", (128, 4, 528), F32)
        pa = sb("pa", (128, 528), F32)
        pb = sb("pb", (128, 528), F32)
        pooledT = sb("pooledT", (128, 4, 512), BF16)
        ymT = sb("ymT", (128, 8, 512), BF16)
        qT = sb("qT", (128, 2, 512), BF16)
        kT = sb("kT", (128, 2, 512), BF16)
        srT = sb("srT", (128, 4, 512), BF16)
        g1 = sb("g1", (32, 512), F32)
        vtok = [sb(f"vtok{i}", (128, 512), BF16) for i in range(4)]
        ktok = [sb(f"ktok{i}", (128, 256), BF16) for i in range(3)]
        sp = [sb(f"sp{i}", (128, 256), F32) for i in range(2)]
        eb = [sb(f"eb{i}", (128, 2, 128), F32) for i in range(2)]
        enb = [sb(f"enb{i}", (128, 2, 128), BF16) for i in range(2)]
        eend = [sb(f"eend{i}", (128, 256), BF16) for i in range(2)]
        qtX = [[sb(f"qt{c}{i}", (128, 2, 128), BF16) for c in "AB"] for i in range(2)]
        ktX = [[sb(f"kt{c}{i}", (128, 2, 128), BF16) for c in "AB"] for i in range(2)]
        kend = [sb(f"kend{i}", (128, 256), BF16) for i in range(2)]
        scT = sb("scT", (128, 4, 128), BF16)
        on = sb("on", (128, 512), BF16)
        S = sb("S", (128, 2, 128), F32)
        Sb = [sb(f"Sb{i}", (128, 2, 128), BF16) for i in range(2)]
        stat = sb("stat", (128, 4, 16), F32)
        stat4b = [sb(f"stat4{i}", (128, 16), F32) for i in range(2)]
        sg = sb("sg", (128, 2, 512), BF16)
        hT = [sb(f"hT{i}", (128, 2, 512), BF16) for i in range(2)]
        comb = sb("comb", (128, 4, 16), F32)
        rtb = [sb(f"rt{i}", (128, 128), F32) for i in range(2)]
        ps = st.enter_context(nc.psum_tensor("ps", [128, 7, 512], F32))

        dma_sems = ["ldc", "ldp"] + [f"ldx{p}{i}" for p in range(2) for i in range(4)] + [f"st{p}{i}" for p in range(2) for i in range(4)]
        for i in range(2):
            dma_sems += [f"wlg{i}", f"wld{i}", f"cvg{i}", f"cvu{i}", f"cvd{i}", f"csg{i}", f"csd{i}"]
        sems = {}
        for s in list(ENGS) + dma_sems:
            sems[s] = st.enter_context(nc.semaphore(s))
        P = Prog(nc, dma_sems=dma_sems)
        bankctr = [0]

        hard = [False]

        def stage(n):
            P.stopped = (STAGE < n) or hard[0]

        def nb():
            b = bankctr[0] % 7
            bankctr[0] += 1
            return b

        def pe_fence(b):
            P.op("pe", lambda e: e.matmul(ps[:, b, 508:512], lhsT=idb[0:1, :], rhs=idb[0:1, 0:4], start=True, stop=True),
                 reads=["idb"], writes=[f"ps{b}"])

        IDF = consts[:, 0, :]
        TRI_INC = consts[:, 1, :]
        TRI_STR = consts[:, 2, :]
        NEG16 = consts[:, 3, 0:1]
        CAUS4 = caus4[:]

        def ld(eng, out, in_, name, sem="ldc"):
            P.op(eng, lambda e: e.dma_start(out=out, in_=in_), writes=[name], dma_sem=sem)

        ld("sp", consts[:], consts_d, "consts")
        ld("sp", pmask[:], pmask_d, "pmask")
        ld("sp", w_gu[:], w_gu_d, "w_gu")
        ld("sp", gnw[:], gnw_d, "gnw")
        ld("sp", pscale[:], pscale_d, "pscale")
        ld("sp", nmw_pk[:], nmw_d, "nmw")
        ld("sp", nfw_pk[:], nfw_d, "nfw")
        ld("sp", fnw[:], fnw_d.partition_broadcast(128), "fnw")
        ld("sp", b_r[:], b_r_d.partition_broadcast(128), "b_r")
        ld("sp", w_r[:], w_r_d.rearrange("(kc p) n -> p kc n", p=128), "w_r")
        for kc in range(8):
            ld("pool", w_in[:, kc, :], w_in_d[kc * 128:(kc + 1) * 128, :], "w_in", "ldp")
        ld("pool", pool_w[:], pool_w_d.rearrange("g c d -> c g d"), "pool_w", "ldp")
        for kc in range(8):
            ld("pool", w_out[:, kc, :], w_out_d[kc * 128:(kc + 1) * 128, :], "w_out", "ldp")
        for name in ("consts", "pmask", "w_gu", "gnw", "pscale", "nmw", "nfw", "fnw", "b_r", "w_r"):
            P.lastw[name] = ("ldc", P.cnt["ldc"])
        for name in ("w_in", "pool_w", "w_out"):
            P.lastw[name] = ("ldp", P.cnt["ldp"])
        for kc in range(8):
            P.op("dve", lambda e: e.tensor_scalar(out=w_in[:, kc, :], in0=w_in[:, kc, :], scalar1=nmw_pk[:, kc:kc + 1],
                                                   scalar2=None, op0=ALU.mult), reads=["w_in", "nmw"], writes=["w_in"])
            P.op("dve", lambda e: e.tensor_scalar(out=w_r[:, kc, :], in0=w_r[:, kc, :], scalar1=nfw_pk[:, kc:kc + 1],
                                                  scalar2=None, op0=ALU.mult), reads=["w_r", "nfw"], writes=["w_r"])
        P.op("dve", lambda e: e.tensor_copy(out=idb[:], in_=IDF), reads=["consts"], writes=["idb"])
        for h_ in range(4):
            P.op("dve", lambda e: e.tensor_copy(out=caus4[:, h_, :], in_=consts[:, 4, :]), reads=["consts"], writes=["caus4"])
        P.op("dve", lambda e: e.memset(S[:], 0.0), writes=["S"])
        P.op("dve", lambda e: e.memset(g1[:], 1.0), writes=[f"g1_{i}" for i in range(4)])
        for p_ in range(2):
            for i_ in range(2):
                P.op("pool", lambda e: e.memset(qtX[p_][i_][:], 0.0), writes=[f"qt{p_}"])
                P.op("pool", lambda e: e.memset(ktX[p_][i_][:], 0.0), writes=[f"kt{p_}"])
        P.op("dve", lambda e: e.memset(pvT[:], 0.0), writes=["pvT"])

        cvt_tok = {}

        def convert_load(e_):
            s_ = e_ % NSLOT
            P.op("pool", lambda e: e.dma_start(out=wgu[s_][:, :, 0:256],
                                               in_=w_eg_d[e_].rearrange("(kc p) n -> p kc n", p=128)),
                 writes=[f"wgu{s_}a"], dma_sem=f"cvg{s_}")
            P.op("pool", lambda e: e.dma_start(out=wgu[s_][:, :, 256:512],
                                               in_=w_eu_d[e_].rearrange("(kc p) n -> p kc n", p=128)),
                 writes=[f"wgu{s_}b"], dma_sem=f"cvu{s_}")
            P.op("pool", lambda e: e.dma_start(out=wd[s_][:], in_=w_ed_d[e_].rearrange("(hc p) n -> p hc n", p=128)),
                 writes=[f"wd{s_}"], dma_sem=f"cvd{s_}")

        def convert_store(e_):
            s_ = e_ % NSLOT
            for kc in range(8):
                P.op("dve", lambda e: e.tensor_scalar(out=wgu[s_][:, kc, :], in0=wgu[s_][:, kc, :],
                                                      scalar1=nfw_pk[:, kc:kc + 1], scalar2=None, op0=ALU.mult),
                     reads=[f"wgu{s_}a", f"wgu{s_}b", "nfw"], writes=[f"wgu{s_}a", f"wgu{s_}b"])
            P.op("pool", lambda e: e.dma_start(out=wgu_s[e_], in_=wgu[s_][:]), reads=[f"wgu{s_}a", f"wgu{s_}b"],
                 writes=[f"wgus{e_}"], dma_sem=f"csg{s_}")
            P.op("pool", lambda e: e.dma_start(out=wd_s[e_], in_=wd[s_][:]), reads=[f"wd{s_}"],
                 writes=[f"wds{e_}"], dma_sem=f"csd{s_}")

        def run_streams(gens, width, stagger=1):
            it = iter(gens)
            active = []
            since = stagger
            done = False
            while True:
                if not done and len(active) < width and since >= stagger:
                    g = next(it, None)
                    if g is None:
                        done = True
                    else:
                        active.append(g)
                        since = 0
                if not active:
                    if done:
                        break
                    since = stagger
                    continue
                since += 1
                for g in list(reversed(active)):
                    try:
                        next(g)
                    except StopIteration:
                        active.remove(g)

        def rmsnorm(x_ap, xname, wb, wname, out_ap, oname, sidx):
            stn = f"stat{sidx}"
            P.op("dve", lambda e: e.memset(stat[:, sidx, 0:1], 0.0), writes=[stn])
            P.op("act", lambda e: e.activation(out=junk[:], in_=x_ap, func=AF.Square, accum_out=stat[:, sidx, 0:1]),
                 reads=[xname], writes=["junk", stn])
            P.op("act", lambda e: e.activation(out=stat[:, sidx, 1:2], in_=stat[:, sidx, 0:1], func=AF.Ln,
                                               scale=1.0 / D, bias=EPS), reads=[stn], writes=[stn])
            P.op("act", lambda e: e.activation(out=stat[:, sidx, 2:3], in_=stat[:, sidx, 1:2], func=AF.Exp, scale=-0.5),
                 reads=[stn], writes=[stn])
            if wb is None:
                P.op("dve", lambda e: e.tensor_scalar(out=out_ap, in0=x_ap, scalar1=stat[:, sidx, 2:3], scalar2=None,
                                                      op0=ALU.mult), reads=[xname, stn], writes=[oname])
            else:
                P.op("dve", lambda e: e.scalar_tensor_tensor(out=out_ap, in0=x_ap, scalar=stat[:, sidx, 2:3], in1=wb[:],
                                                             op0=ALU.mult, op1=ALU.mult),
                     reads=[xname, stn, wname], writes=[oname])

        def transpose_bf(src, sname, dst3, dname):
            for half in range(2):
                b = nb()
                for c in range(4):
                    kc = half * 4 + c
                    P.op("pe", lambda e: e.matmul(ps[:, b, c * 128:(c + 1) * 128],
                                                  lhsT=src[:, kc * 128:(kc + 1) * 128], rhs=idb[:], start=True, stop=True),
                         reads=[sname, "idb"], writes=[f"ps{b}"], sig=(c == 3))
                P.op("dve", lambda e: e.tensor_copy(
                    out=dst3[:, half * 4:(half + 1) * 4, :], in_=ps[:, b, :].rearrange("p (c n) -> p c n", c=4)),
                    reads=[f"ps{b}"], writes=[dname])

        def kv_tokmajor(c0, xtn):
            bk = nb()
            for kc in range(8):
                P.op("pe", lambda e: e.matmul(ps[:, bk, 0:256], lhsT=xnT[:, kc, c0:c0 + 128],
                                              rhs=w_in[:, kc, 768:1024], start=(kc == 0), stop=(kc == 7)),
                     reads=[xtn, "w_in"], writes=[f"ps{bk}"], sig=(kc == 7))
            bv = nb()
            for kc in range(8):
                P.op("pe", lambda e: e.matmul(ps[:, bv, 0:512], lhsT=xnT[:, kc, c0:c0 + 128],
                                              rhs=w_in[:, kc, 1024:1536], start=(kc == 0), stop=(kc == 7)),
                     reads=[xtn, "w_in"], writes=[f"ps{bv}"], sig=(kc == 7))
            return bk, bv

        def softplus_neg(gc0, g1name, par, mask_col=None):
            bl = nb()
            P.op("pe", lambda e: e.matmul(ps[:, bl, 0:256], lhsT=g1[0:17, gc0:gc0 + 128], rhs=w_gu[:, :],
                                          start=True, stop=True), reads=[g1name, "w_gu"], writes=[f"ps{bl}"], sig=False)
            pe_fence(bl)
            P.op("act", lambda e: e.activation(out=sp[par][:], in_=ps[:, bl, 0:256], func=AF.Exp, scale=-1.0),
                 reads=[f"ps{bl}"], writes=[f"sp{par}"])
            P.op("act", lambda e: e.activation(out=sp[par][:], in_=sp[par][:], func=AF.Ln, bias=1.0),
                 reads=[f"sp{par}"], writes=[f"sp{par}"])
            if mask_col is not None:
                P.op("dve", lambda e: e.tensor_scalar(out=sp[par][:], in0=sp[par][:], scalar1=pmask[:, mask_col:mask_col + 1],
                                                      scalar2=None, op0=ALU.mult),
                     reads=[f"sp{par}", "pmask"], writes=[f"sp{par}"])

        def kend_and_v(bk, bv, par):
            be = nb()
            P.op("pe", lambda e: e.matmul(ps[:, be, 0:256], lhsT=TRI_STR, rhs=sp[par][:], start=True, stop=True),
                 reads=["consts", f"sp{par}"], writes=[f"ps{be}"], sig=False)
            pe_fence(be)
            P.op("act", lambda e: e.activation(out=eend[par][:], in_=ps[:, be, 0:256], func=AF.Exp),
                 reads=[f"ps{be}"], writes=[f"eend{par}"])
            P.op("dve", lambda e: e.tensor_tensor(out=kend[par][:], in0=ps[:, bk, 0:256], in1=eend[par][:], op=ALU.mult),
                 reads=[f"ps{bk}", f"eend{par}"], writes=[f"kend{par}"])
            P.op("dve", lambda e: e.tensor_copy(out=vtok[par][:], in_=ps[:, bv, 0:512]),
                 reads=[f"ps{bv}"], writes=[f"vtok{par}"])

        def cum_decay(par, want_neg):
            bb = nb()
            for j in range(2):
                P.op("pe", lambda e: e.matmul(ps[:, bb, j * 128:(j + 1) * 128], lhsT=sp[par][:, j * 128:(j + 1) * 128],
                                              rhs=TRI_INC, start=True, stop=True),
                     reads=[f"sp{par}", "consts"], writes=[f"ps{bb}"], sig=False)
            pe_fence(bb)
            P.op("act", lambda e: e.activation(out=eb[par][:], in_=ps[:, bb, 0:256].rearrange("p (j n) -> p j n", j=2),
                                               func=AF.Exp), reads=[f"ps{bb}"], writes=[f"eb{par}"])
            if want_neg:
                P.op("act", lambda e: e.activation(out=enb[par][:], in_=ps[:, bb, 0:256].rearrange("p (j n) -> p j n", j=2),
                                                   func=AF.Exp, scale=-1.0), reads=[f"ps{bb}"], writes=[f"enb{par}"])

        def state_update(par, vt_, vtn):
            bs = nb()
            for j in range(2):
                P.op("pe", lambda e: e.matmul(ps[:, bs, j * 256:(j + 1) * 256], lhsT=kend[par][:, j * 128:(j + 1) * 128],
                                              rhs=vt_[:, j * 256:(j + 1) * 256], start=True, stop=True),
                     reads=[f"kend{par}", vtn], writes=[f"ps{bs}"], sig=(j == 1))
            for j in range(2):
                for hh in range(2):
                    r0 = hh * 64
                    P.op("dve", lambda e: e.scalar_tensor_tensor(
                        out=S[r0:r0 + 64, j, :], in0=S[r0:r0 + 64, j, :], scalar=eb[par][r0:r0 + 64, j, 127:128],
                        in1=ps[r0:r0 + 64, bs, j * 256 + hh * 128:j * 256 + hh * 128 + 128],
                        op0=ALU.mult, op1=ALU.add), reads=["S", f"eb{par}", f"ps{bs}"], writes=["S"])

        def prefix_tile(i):
            if i % 2 == 0 and i // 2 < NE:
                convert_load(i // 2)
            if i % 2 == 1 and i // 2 < NE:
                convert_store(i // 2)
            slot = i % 4
            par = i % 2
            c0 = slot * 128
            P.op("sp", lambda e: e.dma_start(out=x[:, slot, :], in_=xp_d[i * 128:(i + 1) * 128, :]),
                 writes=[f"x0{slot}"], dma_sem=f"ldx0{slot}")
            rmsnorm(x[:, slot, :], f"x0{slot}", None, None, xn[par][:], f"xn{par}", slot)
            yield
            transpose_bf(xn[par], f"xn{par}", xnT[:, :, c0:c0 + 128], f"xnT{slot}")
            yield
            bk, bv = kv_tokmajor(c0, f"xnT{slot}")
            bg = nb()
            for kc in range(8):
                P.op("pe", lambda e: e.matmul(ps[0:16, bg, 0:128], lhsT=w_in[:, kc, 2048:2064],
                                              rhs=xnT[:, kc, c0:c0 + 128], start=(kc == 0), stop=(kc == 7)),
                     reads=[f"xnT{slot}", "w_in"], writes=[f"ps{bg}"], sig=(kc == 7))
            if i == NPRE - 1:
                bh = nb()
                for g in range(4):
                    for kc in range(8):
                        P.op("pe", lambda e: e.matmul(ps[:, bh, g * 16:(g + 1) * 16],
                                                      lhsT=w_in[:, kc, g * 128:(g + 1) * 128],
                                                      rhs=xnT[:, kc, c0 + 112:c0 + 128],
                                                      start=(kc == 0), stop=(kc == 7)),
                             reads=[f"xnT{slot}", "w_in"], writes=[f"ps{bh}"], sig=(g == 3 and kc == 7))
                P.op("act", lambda e: e.activation(out=pvT[:, :, 0:16],
                                                   in_=ps[:, bh, 0:64].rearrange("p (g n) -> p g n", g=4), func=AF.Copy),
                     reads=[f"ps{bh}"], writes=["pvT"])
            P.op("dve", lambda e: e.tensor_copy(out=ktok[i % 3][:], in_=ps[:, bk, 0:256]),
                 reads=[f"ps{bk}"], writes=[f"ktok{i % 3}"])
            P.op("dve", lambda e: e.tensor_copy(out=vtok[slot][:], in_=ps[:, bv, 0:512]),
                 reads=[f"ps{bv}"], writes=[f"vtok{slot}"])
            P.op("act", lambda e: e.activation(out=g1[0:16, c0:c0 + 128], in_=ps[0:16, bg, 0:128], func=AF.Copy),
                 reads=[f"ps{bg}"], writes=[f"g1_{slot}"])
            yield
            softplus_neg(c0, f"g1_{slot}", par, mask_col=i)
            yield
            be = nb()
            P.op("pe", lambda e: e.matmul(ps[:, be, 0:256], lhsT=TRI_STR, rhs=sp[par][:], start=True, stop=True),
                 reads=["consts", f"sp{par}"], writes=[f"ps{be}"], sig=False)
            pe_fence(be)
            bb = nb()
            for j in range(2):
                P.op("pe", lambda e: e.matmul(ps[:, bb, j * 128:(j + 1) * 128], lhsT=sp[par][:, j * 128:(j + 1) * 128],
                                              rhs=TRI_INC, start=True, stop=True),
                     reads=[f"sp{par}", "consts"], writes=[f"ps{bb}"], sig=False)
            pe_fence(bb)
            P.op("act", lambda e: e.activation(out=eend[par][:], in_=ps[:, be, 0:256], func=AF.Exp),
                 reads=[f"ps{be}"], writes=[f"eend{par}"])
            P.op("act", lambda e: e.activation(out=eb[par][:], in_=ps[:, bb, 0:256].rearrange("p (j n) -> p j n", j=2),
                                               func=AF.Exp), reads=[f"ps{bb}"], writes=[f"eb{par}"])
            P.op("dve", lambda e: e.tensor_tensor(out=kend[par][:], in0=ktok[i % 3][:], in1=eend[par][:], op=ALU.mult),
                 reads=[f"ktok{i % 3}", f"eend{par}"], writes=[f"kend{par}"])
            yield
            bs = nb()
            for j in range(2):
                P.op("pe", lambda e: e.matmul(ps[:, bs, j * 256:(j + 1) * 256], lhsT=kend[par][:, j * 128:(j + 1) * 128],
                                              rhs=vtok[slot][:, j * 256:(j + 1) * 256], start=True, stop=True),
                     reads=[f"kend{par}", f"vtok{slot}"], writes=[f"ps{bs}"], sig=(j == 1))
            for j in range(2):
                for hh in range(2):
                    r0 = hh * 64
                    P.op("dve", lambda e: e.scalar_tensor_tensor(
                        out=S[r0:r0 + 64, j, :], in0=S[r0:r0 + 64, j, :], scalar=eb[par][r0:r0 + 64, j, 127:128],
                        in1=ps[r0:r0 + 64, bs, j * 256 + hh * 128:j * 256 + hh * 128 + 128],
                        op0=ALU.mult, op1=ALU.add), reads=["S", f"eb{par}", f"ps{bs}"], writes=["S"])

        run_streams([prefix_tile(i) for i in range(NPRE)], PRE_WIDTH, 1)
        for e_ in range(NE):
            if e_ == NPRE // 2 and NPRE % 2 == 1:
                convert_store(e_)
            elif e_ >= (NPRE + 1) // 2:
                convert_load(e_)
                convert_store(e_)
        P.op("act", lambda e: e.activation(out=Sb[0][:], in_=S[:], func=AF.Copy), reads=["S"], writes=["Sb0"])

        XT = [f"xnT{t}" for t in range(4)]
        G1ALL = [f"g1_{t}" for t in range(4)]
        out_toks = []

        def load_expert(ge):
            e_ = ge % NE
            s_ = ge % NSLOT
            P.op("sp", lambda e: e.dma_start(out=wgu[s_][:], in_=wgu_s[e_]), reads=[f"wgus{e_}"],
                 writes=[f"wgu{s_}a", f"wgu{s_}b"], dma_sem=f"wlg{s_}")
            P.op("sp", lambda e: e.dma_start(out=wd[s_][:], in_=wd_s[e_]), reads=[f"wds{e_}"],
                 writes=[f"wd{s_}"], dma_sem=f"wld{s_}")

        NGE = NBLK * NE
        load_expert(0)

        def front_tile(blk, t):
            r0 = blk * 512 + t * 128
            par = t % 2
            pb_ = blk % 2
            x = xb[pb_]
            P.op("sp", lambda e: e.dma_start(out=x[:, t, :], in_=xm_d[r0:r0 + 128, :]),
                 writes=[f"x{pb_}{t}"], dma_sem=f"ldx{pb_}{t}")
            rmsnorm(x[:, t, :], f"x{pb_}{t}", None, None, xn[par][:], f"xn{par}", t)
            yield
            transpose_bf(xn[par], f"xn{par}", xnT[:, :, t * 128:(t + 1) * 128], f"xnT{t}")

        def proj_chunk(col0, m):
            b = nb()
            for kc in range(8):
                P.op("pe", lambda e: e.matmul(ps[0:m, b, :], lhsT=w_in[:, kc, col0:col0 + m], rhs=xnT[:, kc, :],
                                              start=(kc == 0), stop=(kc == 7)),
                     reads=XT + ["w_in"], writes=[f"ps{b}"], sig=(kc == 7))
            return b

        def block_front(blk):
            fts = [front_tile(blk, t) for t in range(4)]
            next(fts[0])
            yield
            for t in range(4):
                if t + 1 < 4:
                    next(fts[t + 1])
                    yield
                for _ in fts[t]:
                    pass
                yield
            for g in range(4):
                b = proj_chunk(g * 128, 128)
                P.op("act", lambda e: e.activation(out=pvT[:, g, 16:528], in_=ps[:, b, :], func=AF.Copy),
                     reads=[f"ps{b}"], writes=["pvT"])
                yield
            for j in range(2):
                b = proj_chunk(512 + j * 128, 128)
                P.op("act", lambda e: e.activation(out=qT[:, j, :], in_=ps[:, b, :], func=AF.Copy),
                     reads=[f"ps{b}"], writes=["qT"])
                yield
            for j in range(2):
                b = proj_chunk(768 + j * 128, 128)
                P.op("act", lambda e: e.activation(out=kT[:, j, :], in_=ps[:, b, :], func=AF.Copy),
                     reads=[f"ps{b}"], writes=["kT"])
                yield
            b = proj_chunk(2048, 16)
            P.op("act", lambda e: e.activation(out=g1[0:16, :], in_=ps[0:16, b, :], func=AF.Copy),
                 reads=[f"ps{b}"], writes=G1ALL)
            for h in range(4):
                b = proj_chunk(1536 + h * 128, 128)
                P.op("act", lambda e: e.activation(out=srT[:, h, :], in_=ps[:, b, :], func=AF.Silu),
                     reads=[f"ps{b}"], writes=["srT"])
                yield
            for g in range(4):
                p_ = pvT[:, g, :]
                w_ = 2 << g
                P.op("pool", lambda e: e.tensor_tensor(out=pa[:, 1:528], in0=p_[:, 1:528], in1=p_[:, 0:527], op=ALU.add),
                     reads=["pvT"], writes=["pa"])
                cur, curname = pa, "pa"
                if g >= 1:
                    P.op("pool", lambda e: e.tensor_tensor(out=pb[:, 3:528], in0=pa[:, 3:528], in1=pa[:, 1:526], op=ALU.add),
                         reads=["pa"], writes=["pb"])
                    cur, curname = pb, "pb"
                if g >= 2:
                    P.op("pool", lambda e: e.tensor_tensor(out=pa[:, 7:528], in0=pb[:, 7:528], in1=pb[:, 3:524], op=ALU.add),
                         reads=["pb"], writes=["pa"])
                    cur, curname = pa, "pa"
                if g >= 3:
                    P.op("pool", lambda e: e.tensor_tensor(out=pb[:, 15:528], in0=pa[:, 15:528], in1=pa[:, 7:520], op=ALU.add),
                         reads=["pa"], writes=["pb"])
                    cur, curname = pb, "pb"
                P.op("dve", lambda e: e.scalar_tensor_tensor(
                    out=pooledT[:, g, :], in0=cur[:, 16:528], scalar=1.0 / w_, in1=p_[:, 16:528],
                    op0=ALU.mult, op1=ALU.subtract), reads=[curname, "pvT"], writes=["pooledT"])
                yield
            P.op("pool", lambda e: e.tensor_copy(out=pvT[:, :, 0:16], in_=pvT[:, :, 512:528]), reads=["pvT"], writes=["pvT"])
            for g in range(4):
                b = nb()
                P.op("pe", lambda e: e.matmul(ps[:, b, :], lhsT=pool_w[:, g, :], rhs=pooledT[:, g, :], start=True, stop=True),
                     reads=["pool_w", "pooledT"], writes=[f"ps{b}"])
                P.op("act", lambda e: e.activation(out=ymT[:, g, :], in_=ps[:, b, :], func=AF.Copy, scale=pscale[:, g:g + 1]),
                     reads=[f"ps{b}", "pscale"], writes=["ymTp"])
            yield

        tile_ctr = [0]

        def main_tile(blk, t):
            c0 = t * 128
            tc = tile_ctr[0]
            par = tc % 2
            tile_ctr[0] += 1
            kt_, vt_ = ktok[tc % 3], vtok[tc % 4]
            ktn, vtn = f"ktok{tc % 3}", f"vtok{tc % 4}"
            stat4 = stat4b[par]
            st4n = f"stat4{par}"
            pb_ = blk % 2
            x = xb[pb_]
            xn_ = f"x{pb_}{t}"
            bk, bv = kv_tokmajor(c0, f"xnT{t}")
            P.op("dve", lambda e: e.tensor_copy(out=kt_[:], in_=ps[:, bk, 0:256]), reads=[f"ps{bk}"], writes=[ktn])
            P.op("dve", lambda e: e.tensor_copy(out=vt_[:], in_=ps[:, bv, 0:512]), reads=[f"ps{bv}"], writes=[vtn])
            yield
            softplus_neg(c0, f"g1_{t}", par)
            yield
            be = nb()
            P.op("pe", lambda e: e.matmul(ps[:, be, 0:256], lhsT=TRI_STR, rhs=sp[par][:], start=True, stop=True),
                 reads=["consts", f"sp{par}"], writes=[f"ps{be}"], sig=False)
            pe_fence(be)
            cum_decay(par, True)
            P.op("act", lambda e: e.activation(out=eend[par][:], in_=ps[:, be, 0:256], func=AF.Exp),
                 reads=[f"ps{be}"], writes=[f"eend{par}"])
            P.op("dve", lambda e: e.tensor_tensor(out=kend[par][:], in0=kt_[:], in1=eend[par][:], op=ALU.mult),
                 reads=[ktn, f"eend{par}"], writes=[f"kend{par}"])
            for i_ in range(2):
                r0 = i_ * 64
                P.op("dve", lambda e: e.scalar_tensor_tensor(out=qtX[par][i_][r0:r0 + 64], in0=qT[r0:r0 + 64, :, c0:c0 + 128],
                                                             scalar=0.125, in1=eb[par][r0:r0 + 64], op0=ALU.mult, op1=ALU.mult),
                     reads=["qT", f"eb{par}"], writes=[f"qt{par}"])
                P.op("dve", lambda e: e.tensor_tensor(out=ktX[par][i_][r0:r0 + 64], in0=kT[r0:r0 + 64, :, c0:c0 + 128],
                                                      in1=enb[par][r0:r0 + 64], op=ALU.mult),
                     reads=["kT", f"enb{par}"], writes=[f"kt{par}"])
            yield
            bsc = nb()
            for h in range(4):
                j = h // 2
                P.op("pe", lambda e: e.matmul(ps[:, bsc, h * 128:(h + 1) * 128], lhsT=ktX[par][h % 2][:, j, :],
                                              rhs=qtX[par][h % 2][:, j, :], start=True, stop=True),
                     reads=[f"kt{par}", f"qt{par}"], writes=[f"ps{bsc}"], sig=(h == 3))
            P.op("dve", lambda e: e.tensor_tensor(out=scT[:], in0=ps[:, bsc, :].rearrange("p (h n) -> p h n", h=4),
                                                  in1=CAUS4, op=ALU.mult),
                 reads=[f"ps{bsc}", "caus4"], writes=["scT"])
            yield
            bo = nb()
            for h in range(4):
                j = h // 2
                P.op("pe", lambda e: e.matmul(ps[:, bo, h * 128:(h + 1) * 128], lhsT=scT[:, h, :],
                                              rhs=vt_[:, h * 128:(h + 1) * 128], start=True, stop=False),
                     reads=["scT", vtn], writes=[f"ps{bo}"], sig=False)
                P.op("pe", lambda e: e.matmul(ps[:, bo, h * 128:(h + 1) * 128], lhsT=qtX[par][h % 2][:, j, :],
                                              rhs=Sb[par][:, j, :], start=False, stop=True),
                     reads=[f"qt{par}", f"Sb{par}"], writes=[f"ps{bo}"], sig=(h == 3))
            state_update(par, vt_, vtn)
            P.op("act", lambda e: e.activation(out=Sb[1 - par][:], in_=S[:], func=AF.Copy),
                 reads=["S"], writes=[f"Sb{1 - par}"])
            P.op("dve", lambda e: e.memset(stat4[:, 0:4], 0.0), writes=[st4n])
            for h in range(4):
                P.op("act", lambda e: e.activation(out=junk[:, 0:128], in_=ps[:, bo, h * 128:(h + 1) * 128],
                                                   func=AF.Square, accum_out=stat4[:, h:h + 1]),
                     reads=[f"ps{bo}"], writes=["junk", st4n])
            P.op("act", lambda e: e.activation(out=stat4[:, 4:8], in_=stat4[:, 0:4], func=AF.Ln, scale=1.0 / 128,
                                               bias=EPS), reads=[st4n], writes=[st4n])
            P.op("act", lambda e: e.activation(out=stat4[:, 8:12], in_=stat4[:, 4:8], func=AF.Exp, scale=-0.5),
                 reads=[st4n], writes=[st4n])
            for h in range(4):
                P.op("act", lambda e: e.activation(out=on[:, h * 128:(h + 1) * 128], in_=ps[:, bo, h * 128:(h + 1) * 128],
                                                   func=AF.Copy, scale=stat4[:, 8 + h:9 + h]),
                     reads=[f"ps{bo}", st4n], writes=["on"])
            yield
            bt = nb()
            for h in range(4):
                P.op("pe", lambda e: e.matmul(ps[:, bt, h * 128:(h + 1) * 128], lhsT=on[:, h * 128:(h + 1) * 128],
                                              rhs=idb[:], start=True, stop=True),
                     reads=["on", "idb"], writes=[f"ps{bt}"], sig=(h == 3))
            P.op("dve", lambda e: e.scalar_tensor_tensor(out=ymT[:, 4:8, c0:c0 + 128],
                                                         in0=ps[:, bt, :].rearrange("p (h n) -> p h n", h=4),
                                                         scalar=gnw[:, 0:1], in1=srT[:, :, c0:c0 + 128],
                                                         op0=ALU.mult, op1=ALU.mult),
                 reads=[f"ps{bt}", "gnw", "srT"], writes=[f"ymTg{t}"])
            yield
            for half in range(2):
                b = nb()
                for kc in range(8):
                    P.op("pe", lambda e: e.matmul(
                        ps[:, b, :], lhsT=ymT[:, kc, c0:c0 + 128], rhs=w_out[:, kc, half * 512:(half + 1) * 512],
                        start=(kc == 0), stop=(kc == 7)), reads=["ymTp", f"ymTg{t}", "w_out"], writes=[f"ps{b}"],
                        sig=(kc == 7))
                P.op("dve", lambda e: e.tensor_tensor(
                    out=x[:, t, half * 512:(half + 1) * 512], in0=ps[:, b, :], in1=x[:, t, half * 512:(half + 1) * 512],
                    op=ALU.add), reads=[f"ps{b}", xn_], writes=[xn_])
            if not OVERLAP:
                yield
                for _ in main_tile_b(blk, t):
                    yield

        def main_tile_b(blk, t):
            c0 = t * 128
            par = t % 2
            rt = rtb[par]
            rtn = f"rt{par}"
            pb_ = blk % 2
            x = xb[pb_]
            rmsnorm(x[:, t, :], f"x{pb_}{t}", None, None, xn2f[:], "xn2f", t)
            yield
            for half in range(2):
                b = nb()
                for c in range(4):
                    kc = half * 4 + c
                    P.op("pe", lambda e: e.matmul(ps[:, b, c * 128:(c + 1) * 128],
                                                  lhsT=xn2f[:, kc * 128:(kc + 1) * 128], rhs=IDF, start=True, stop=True),
                         reads=["xn2f", "consts"], writes=[f"ps{b}"], sig=(c == 3))
                P.op("dve", lambda e: e.tensor_copy(
                    out=xn2Tf[:, half * 4:(half + 1) * 4, :], in_=ps[:, b, :].rearrange("p (c n) -> p c n", c=4)),
                    reads=[f"ps{b}"], writes=["xn2Tf"])
                P.op("act", lambda e: e.activation(
                    out=xn2T[:, half * 4:(half + 1) * 4, c0:c0 + 128], in_=xn2Tf[:, half * 4:(half + 1) * 4, :],
                    func=AF.Copy), reads=["xn2Tf"], writes=[f"xn2T{t}"])
            yield
            br = nb()
            for kc in range(8):
                P.op("pe", lambda e: e.matmul(ps[:, br, 0:20], lhsT=xn2Tf[:, kc, :], rhs=w_r[:, kc, :],
                                              start=(kc == 0), stop=(kc == 7)),
                     reads=["xn2Tf", "w_r"], writes=[f"ps{br}"], sig=False)
            pe_fence(br)
            V = lambda fn: P.op("dve", fn, reads=[rtn], writes=[rtn])
            P.op("dve", lambda e: e.tensor_tensor(out=rt[:, 0:20], in0=ps[:, br, 0:20], in1=b_r[:], op=ALU.add),
                 reads=[f"ps{br}", "b_r"], writes=[rtn])
            V(lambda e: e.tensor_reduce(out=rt[:, 20:21], in_=rt[:, 0:4], axis=AX.X, op=ALU.max))
            V(lambda e: e.tensor_scalar(out=rt[:, 24:28], in0=rt[:, 0:4], scalar1=rt[:, 20:21], scalar2=None, op0=ALU.is_equal))
            V(lambda e: e.tensor_scalar(out=rt[:, 21:22], in0=rt[:, 20:21], scalar1=-1.0, scalar2=None, op0=ALU.mult))
            P.op("dve", lambda e: e.memset(rt[:, 22:23], 0.0), reads=[rtn], writes=[rtn])
            P.op("act", lambda e: e.activation(out=rt[:, 28:32], in_=rt[:, 0:4], func=AF.Exp, bias=rt[:, 21:22],
                                               accum_out=rt[:, 22:23]), reads=[rtn], writes=[rtn])
            V(lambda e: e.reciprocal(out=rt[:, 23:24], in_=rt[:, 22:23]))
            V(lambda e: e.tensor_scalar(out=rt[:, 32:36], in0=rt[:, 24:28], scalar1=BIG, scalar2=-BIG, op0=ALU.mult, op1=ALU.add))
            for g in range(4):
                V(lambda e: e.tensor_scalar(out=rt[:, 40 + 4 * g:44 + 4 * g], in0=rt[:, 4 + 4 * g:8 + 4 * g],
                                            scalar1=rt[:, 32 + g:33 + g], scalar2=None, op0=ALU.add))
            V(lambda e: e.tensor_reduce(out=rt[:, 36:37], in_=rt[:, 40:56], axis=AX.X, op=ALU.max))
            V(lambda e: e.tensor_scalar(out=rt[:, 56:72], in0=rt[:, 40:56], scalar1=rt[:, 36:37], scalar2=None, op0=ALU.is_equal))
            V(lambda e: e.scalar_tensor_tensor(out=rt[:, 72:88], in0=rt[:, 56:72], scalar=-BIG, in1=rt[:, 40:56],
                                               op0=ALU.mult, op1=ALU.add))
            V(lambda e: e.tensor_reduce(out=rt[:, 37:38], in_=rt[:, 72:88], axis=AX.X, op=ALU.max))
            V(lambda e: e.tensor_scalar(out=rt[:, 88:104], in0=rt[:, 72:88], scalar1=rt[:, 37:38], scalar2=None, op0=ALU.is_equal))
            V(lambda e: e.tensor_tensor(out=rt[:, 38:39], in0=rt[:, 37:38], in1=rt[:, 36:37], op=ALU.subtract))
            P.op("act", lambda e: e.activation(out=rt[:, 39:40], in_=rt[:, 38:39], func=AF.Exp), reads=[rtn], writes=[rtn])
            V(lambda e: e.tensor_scalar(out=rt[:, 104:105], in0=rt[:, 39:40], scalar1=1.0, scalar2=None, op0=ALU.add))
            V(lambda e: e.reciprocal(out=rt[:, 105:106], in_=rt[:, 104:105]))
            V(lambda e: e.tensor_tensor(out=rt[:, 106:107], in0=rt[:, 105:106], in1=rt[:, 23:24], op=ALU.mult))
            V(lambda e: e.tensor_tensor(out=rt[:, 107:108], in0=rt[:, 23:24], in1=rt[:, 106:107], op=ALU.subtract))
            V(lambda e: e.tensor_scalar(out=rt[:, 108:124], in0=rt[:, 56:72], scalar1=rt[:, 106:107], scalar2=None, op0=ALU.mult))
            P.op("dve", lambda e: e.scalar_tensor_tensor(out=comb[:, t, :], in0=rt[:, 88:104], scalar=rt[:, 107:108],
                                                         in1=rt[:, 108:124], op0=ALU.mult, op1=ALU.add),
                 reads=[rtn], writes=[f"comb{t}"])

        X2T = [f"xn2T{t}" for t in range(4)]

        def experts(blk):
            pb_ = blk % 2
            x = xb[pb_]
            for e_ in range(NE):
                ge = blk * NE + e_
                s_ = ge % NSLOT
                if ge + 1 < NGE:
                    load_expert(ge + 1)
                hp = ge % 2
                for hc in range(2):
                    bb2 = []
                    for gu in range(2):
                        b = nb()
                        bb2.append(b)
                        col = gu * 256 + hc * 128
                        for kc in range(8):
                            P.op("pe", lambda e: e.matmul(
                                ps[:, b, :], lhsT=wgu[s_][:, kc, col:col + 128], rhs=xn2T[:, kc, :],
                                start=(kc == 0), stop=(kc == 7)), reads=X2T + [f"wgu{s_}" + "ab"[gu]], writes=[f"ps{b}"],
                                sig=(kc == 7))
                    bg_, bu_ = bb2
                    P.op("act", lambda e: e.activation(out=sg[:, hc, :], in_=ps[:, bg_, :], func=AF.Silu),
                         reads=[f"ps{bg_}"], writes=[f"sg{hc}"])
                    P.op("dve", lambda e: e.tensor_tensor(out=hT[hp][:, hc, :], in0=ps[:, bu_, :], in1=sg[:, hc, :], op=ALU.mult),
                         reads=[f"ps{bu_}", f"sg{hc}"], writes=[f"hT{hp}"])
                for t in range(4):
                    for half in range(2):
                        b = nb()
                        for hc in range(2):
                            P.op("pe", lambda e: e.matmul(
                                ps[:, b, :], lhsT=hT[hp][:, hc, t * 128:(t + 1) * 128],
                                rhs=wd[s_][:, hc, half * 512:(half + 1) * 512], start=(hc == 0), stop=(hc == 1)),
                                reads=[f"hT{hp}", f"wd{s_}"], writes=[f"ps{b}"], sig=(hc == 1))
                        P.op("dve", lambda e: e.scalar_tensor_tensor(
                            out=x[:, t, half * 512:(half + 1) * 512], in0=ps[:, b, :], scalar=comb[:, t, e_:e_ + 1],
                            in1=x[:, t, half * 512:(half + 1) * 512], op0=ALU.mult, op1=ALU.add),
                            reads=[f"ps{b}", f"comb{t}", f"x{pb_}{t}"], writes=[f"x{pb_}{t}"])
                yield

        def final(blk):
            pb_ = blk % 2
            x = xb[pb_]
            for t in range(4):
                r0 = blk * 512 + t * 128
                rmsnorm(x[:, t, :], f"x{pb_}{t}", fnw, "fnw", x[:, t, :], f"x{pb_}{t}", t)
                out_toks.append(P.op("sp", lambda e: e.dma_start(out=out_d[r0:r0 + 128, :], in_=x[:, t, :]),
                                     reads=[f"x{pb_}{t}"], dma_sem=f"st{pb_}{t}"))

        def mixer1(blk):
            for _ in block_front(blk):
                yield
            gens = [main_tile(blk, t) for t in range(4)]
            active, nxt, since = [], 0, MIX_STAGGER
            while active or nxt < 4:
                if nxt < 4 and len(active) < MIX_WIDTH and since >= MIX_STAGGER:
                    active.append(gens[nxt])
                    nxt += 1
                    since = 0
                since += 1
                for g in list(reversed(active)):
                    try:
                        next(g)
                    except StopIteration:
                        active.remove(g)
                yield

        def part2(blk):
            if OVERLAP:
                run_streams([main_tile_b(blk, t) for t in range(4)], 2, 2)

        if OVERLAP:
            for _ in mixer1(0):
                pass
            part2(0)
            for blk in range(NBLK):
                mg = mixer1(blk + 1) if blk + 1 < NBLK else None
                for _ in experts(blk):
                    if mg is not None:
                        for _k in range(4):
                            if next(mg, "done") == "done":
                                mg = None
                                break
                final(blk)
                if mg is not None:
                    for _ in mg:
                        pass
                if blk + 1 < NBLK:
                    part2(blk + 1)
        else:
            for blk in range(NBLK):
                for _ in mixer1(blk):
                    pass
                part2(blk)
                for _ in experts(blk):
                    pass
                final(blk)
        P.finish("sp", out_toks)
        P.emit(sems)
    return nc


def make_consts():
    c = np.zeros((128, 5, 128), np.float32)
    s = np.arange(128)[:, None]
    cc = np.arange(128)[None, :]
    c[:, 0] = np.eye(128, dtype=np.float32)
    c[:, 1] = np.where(s <= cc, -1.0 / 16, 0.0)
    c[:, 2] = np.where(s > cc, -1.0 / 16, 0.0)
    c[:, 3] = -1.0 / 16
    c[:, 4] = (s <= cc).astype(np.float32)
    return c


def make_in_maps(inp, seq):
    B = inp["x"].shape[0]
    half_len = seq // 2
    NPRE = half_len // 128 + 1
    f = lambda a: np.ascontiguousarray(np.asarray(a, dtype=np.float32))
    x = f(inp["x"])
    meta = f(inp["meta_tokens"])
    shared = {
        "w_in": f(inp["w_in"][0]),
        "w_gu_b": f(np.concatenate([inp["w_gate_up"][0], inp["b_gate"][0][None, :]], axis=0)),
        "gnw": f(inp["gla_norm_w"][0].reshape(128, 1)),
        "pool_w": f(inp["pool_w"][0]),
        "pscale": f(inp["pool_scale"][0].reshape(4, 128).T),
        "w_out": f(inp["w_out"][0]),
        "nmw_pk": f(inp["norm_mix_w"][0].reshape(8, 128).T),
        "nfw_pk": f(inp["norm_ffn_w"][0].reshape(8, 128).T),
        "fnw": f(inp["final_norm_w"]),
        "w_r": f(np.concatenate([inp["w_router_group"][0], inp["w_router_expert"][0]], axis=1)),
        "b_r": f(np.concatenate([inp["b_router_group"][0], inp["b_router_expert"][0]], axis=0)),
        "w_eg": f(inp["w_expert_gate"][0]),
        "w_eu": f(inp["w_expert_up"][0]),
        "w_ed": f(inp["w_expert_down"][0]),
        "consts": make_consts(),
    }
    maps = []
    for b in range(B):
        for half in range(2):
            xp = np.zeros((NPRE * 128, D), np.float32)
            mask = np.zeros((NPRE * 128,), np.float32)
            if half == 0:
                xp[-16:] = meta
                mask[-16:] = 1.0
            else:
                xp[112:128] = meta
                xp[128:] = x[b, :half_len]
                mask[112:] = 1.0
            m = dict(shared)
            m["xp"] = xp
            m["xm"] = np.ascontiguousarray(x[b, half * half_len:(half + 1) * half_len])
            m["pmask"] = np.ascontiguousarray(mask.reshape(NPRE, 128).T)
            maps.append(m)
    return maps, NPRE, half_len // 512


def kernel(**inputs):
    x = np.asarray(inputs["x"])
    B, seq, _ = x.shape
    maps, NPRE, NBLK = make_in_maps(inputs, seq)
    nc = build(NPRE, NBLK)
    res = run_bass_kernel_spmd(nc, maps, core_ids=list(range(len(maps))))
    half_len = seq // 2
    out = np.empty((B, seq, D), np.float32)
    for b in range(B):
        for half in range(2):
            out[b, half * half_len:(half + 1) * half_len] = res.results[2 * b + half]["out"]
    return out
```

```python
import numpy as np
from contextlib import ExitStack
import concourse.bass as bass
import concourse.mybir as mybir
from concourse.bass_utils import run_bass_kernel_spmd

F32 = mybir.dt.float32
BF16 = mybir.dt.bfloat16
AF = mybir.ActivationFunctionType
ALU = mybir.AluOpType
AX = mybir.AxisListType

D = 1024
NIN = 2064
NE = 16
EH = 256
EPS = 1e-6
BIG = 1.0e4
ENGS = ("pe", "act", "dve", "pool", "sp")
OVERLAP = False
MIX_WIDTH = 5
MIX_STAGGER = 2
PRE_WIDTH = 6


class _Rec:
    def __init__(self):
        self.call = None

    def __getattr__(self, name):
        def f(*a, **k):
            self.call = (name, a, k)
            return self
        return f


class Prog:
    def __init__(self, nc, dma_sems=()):
        self.nc = nc
        self.ops = {e: [] for e in ENGS}
        self.cnt = {e: 0 for e in ENGS}
        for s in dma_sems:
            self.cnt[s] = 0
        self.seen = {e: {} for e in ENGS}
        self.lastw = {}
        self.readers = {}
        self.stopped = False
        import os
        self.debug = bool(os.environ.get("TRKDEBUG"))

    def op(self, eng, fn, reads=(), writes=(), sig=True, dma_sem=None):
        if self.stopped:
            return None
        waits = {}

        def need(tok):
            if tok is None:
                return
            s, v = tok
            if s == eng and eng == "pe":
                return
            if self.seen[eng].get(s, 0) >= v:
                return
            if waits.get(s, 0) < v:
                waits[s] = v

        for b in reads:
            need(self.lastw.get(b))
        for b in writes:
            need(self.lastw.get(b))
            for t in self.readers.get(b, ()):
                need(t)
        for s, v in waits.items():
            self.seen[eng][s] = v
        if dma_sem is not None:
            self.cnt[dma_sem] += 16
            tok = (dma_sem, self.cnt[dma_sem])
            inc = (dma_sem, 16)
        elif sig:
            self.cnt[eng] += 1
            tok = (eng, self.cnt[eng])
            inc = (eng, 1)
        else:
            tok = (eng, self.cnt[eng] + 1)
            inc = None
        for b in reads:
            self.readers.setdefault(b, []).append(tok)
        for b in writes:
            self.lastw[b] = tok
            self.readers[b] = []
        rec = _Rec()
        fn(rec)
        call = rec.call
        self.ops[eng].append((sorted(waits.items()), call, inc))
        if self.debug:
            import sys
            ln = sys._getframe(1).f_lineno
            print(f"OP {eng:4s} L{ln} waits={sorted(waits.items())} tok={tok} r={list(reads)} w={list(writes)}")
        return tok

    def finish(self, eng, toks):
        w = {}
        for tk in toks:
            if tk is None:
                continue
            s, v = tk
            w[s] = max(w.get(s, 0), v)
        for s in self.cnt:
            if self.cnt[s] > 0:
                w[s] = max(w.get(s, 0), self.cnt[s])
        self.ops[eng].append((sorted(w.items()), None, None))

    def emit(self, sems):
        nc = self.nc
        engobj = {"pe": "tensor", "act": "scalar", "dve": "vector", "pool": "gpsimd", "sp": "sync"}
        with nc.Block() as block:
            for e in ENGS:
                ops = self.ops[e]

                def body(eng, ops=ops):
                    for waits, fn, inc in ops:
                        for s, v in waits:
                            eng.wait_ge(sems[s], v)
                        if fn is None:
                            continue
                        ins = getattr(eng, fn[0])(*fn[1], **fn[2])
                        if inc is not None:
                            ins.then_inc(sems[inc[0]], inc[1])

                getattr(block, engobj[e])(body)


def build(NPRE, NBLK, STAGE=99):
    nc = bass.Bass("TRN2", target_bir_lowering=False)
    TP, TM = NPRE * 128, NBLK * 512
    dt_in = lambda name, shape: nc.dram_tensor(name, list(shape), F32, kind="ExternalInput").ap()
    xp_d = dt_in("xp", (TP, D))
    xm_d = dt_in("xm", (TM, D))
    pmask_d = dt_in("pmask", (128, NPRE))
    w_in_d = dt_in("w_in", (D, NIN))
    w_gu_d = dt_in("w_gu_b", (17, 256))
    gnw_d = dt_in("gnw", (128, 1))
    pool_w_d = dt_in("pool_w", (4, 128, 128))
    pscale_d = dt_in("pscale", (128, 4))
    w_out_d = dt_in("w_out", (D, D))
    nmw_d = dt_in("nmw_pk", (128, 8))
    nfw_d = dt_in("nfw_pk", (128, 8))
    fnw_d = dt_in("fnw", (D,))
    w_r_d = dt_in("w_r", (D, 20))
    b_r_d = dt_in("b_r", (20,))
    w_eg_d = dt_in("w_eg", (NE, D, EH))
    w_eu_d = dt_in("w_eu", (NE, D, EH))
    w_ed_d = dt_in("w_ed", (NE, EH, D))
    consts_d = dt_in("consts", (128, 5, 128))
    out_d = nc.dram_tensor("out", [TM, D], F32, kind="ExternalOutput").ap()
    wgu_s = nc.dram_tensor("wgu_scratch", [NE, 128, 8, 512], BF16, kind="Internal").ap()
    wd_s = nc.dram_tensor("wd_scratch", [NE, 128, 2, D], BF16, kind="Internal").ap()

    with ExitStack() as st:
        sb = lambda name, shape, dt: st.enter_context(nc.sbuf_tensor(name, list(shape), dt))
        w_in = sb("w_in_sb", (128, 8, NIN), BF16)
        w_out = sb("w_out_sb", (128, 8, D), BF16)
        pool_w = sb("pool_w_sb", (128, 4, 128), BF16)
        w_r = sb("w_r_sb", (128, 8, 20), F32)
        b_r = sb("b_r_sb", (128, 20), F32)
        w_gu = sb("w_gu_sb", (17, 256), F32)
        gnw = sb("gnw_sb", (128, 1), F32)
        pscale = sb("pscale_sb", (128, 4), F32)
        pmask = sb("pmask_sb", (128, NPRE), F32)
        fnw = sb("fnw_b", (128, D), F32)
        consts = sb("consts_sb", (128, 5, 128), F32)
        caus4 = sb("caus4", (128, 4, 128), BF16)
        idb = sb("idb", (128, 128), BF16)
        NSLOT = 2
        wgu = [sb(f"wgu{i}", (128, 8, 512), BF16) for i in range(NSLOT)]
        wd = [sb(f"wd{i}", (128, 2, D), BF16) for i in range(NSLOT)]
        xb = [sb(f"x{i}", (128, 4, D), F32) for i in range(2)]
        x = xb[0]
        xn2T = sb("xn2T", (128, 8, 512), BF16)
        nmw_pk = sb("nmw_pk_sb", (128, 8), F32)
        nfw_pk = sb("nfw_pk_sb", (128, 8), F32)
        junk = sb("junk", (128, D), BF16)
        xn = [sb(f"xn{i}", (128, D), BF16) for i in range(2)]
        xnT = sb("xnT", (128, 8, 512), BF16)
        xn2f = sb("xn2f", (128, D), F32)
        xn2Tf = sb("xn2Tf", (128, 8, 128), F32)
        pvT = sb("pvT", (128, 4, 528), F32)
        pa = sb("pa", (128, 528), F32)
        pb = sb("pb", (128, 528), F32)
        pooledT = sb("pooledT", (128, 4, 512), BF16)
        ymT = sb("ymT", (128, 8, 512), BF16)
        qT = sb("qT", (128, 2, 512), BF16)
        kT = sb("kT", (128, 2, 512), BF16)
        srT = sb("srT", (128, 4, 512), BF16)
        g1 = sb("g1", (32, 512), F32)
        vtok = [sb(f"vtok{i}", (128, 512), BF16) for i in range(4)]
        ktok = [sb(f"ktok{i}", (128, 256), BF16) for i in range(3)]
        sp = [sb(f"sp{i}", (128, 256), F32) for i in range(2)]
        eb = [sb(f"eb{i}", (128, 2, 128), F32) for i in range(2)]
        enb = [sb(f"enb{i}", (128, 2, 128), BF16) for i in range(2)]
        eend = [sb(f"eend{i}", (128, 256), BF16) for i in range(2)]
        qtX = [[sb(f"qt{c}{i}", (128, 2, 128), BF16) for c in "AB"] for i in range(2)]
        ktX = [[sb(f"kt{c}{i}", (128, 2, 128), BF16) for c in "AB"] for i in range(2)]
        kend = [sb(f"kend{i}", (128, 256), BF16) for i in range(2)]
        scT = sb("scT", (128, 4, 128), BF16)
        on = sb("on", (128, 512), BF16)
        S = sb("S", (128, 2, 128), F32)
        Sb = [sb(f"Sb{i}", (128, 2, 128), BF16) for i in range(2)]
        stat = sb("stat", (128, 4, 16), F32)
        stat4b = [sb(f"stat4{i}", (128, 16), F32) for i in range(2)]
        sg = sb("sg", (128, 2, 512), BF16)
        hT = [sb(f"hT{i}", (128, 2, 512), BF16) for i in range(2)]
        comb = sb("comb", (128, 4, 16), F32)
        rtb = [sb(f"rt{i}", (128, 128), F32) for i in range(2)]
        ps = st.enter_context(nc.psum_tensor("ps", [128, 7, 512], F32))

        dma_sems = ["ldc", "ldp"] + [f"ldx{p}{i}" for p in range(2) for i in range(4)] + [f"st{p}{i}" for p in range(2) for i in range(4)]
        for i in range(2):
            dma_sems += [f"wlg{i}", f"wld{i}", f"cvg{i}", f"cvu{i}", f"cvd{i}", f"csg{i}", f"csd{i}"]
        sems = {}
        for s in list(ENGS) + dma_sems:
            sems[s] = st.enter_context(nc.semaphore(s))
        P = Prog(nc, dma_sems=dma_sems)
        bankctr = [0]

        hard = [False]

        def stage(n):
            P.stopped = (STAGE < n) or hard[0]

        def nb():
            b = bankctr[0] % 7
            bankctr[0] += 1
            return b

        def pe_fence(b):
            P.op("pe", lambda e: e.matmul(ps[:, b, 508:512], lhsT=idb[0:1, :], rhs=idb[0:1, 0:4], start=True, stop=True),
                 reads=["idb"], writes=[f"ps{b}"])

        IDF = consts[:, 0, :]
        TRI_INC = consts[:, 1, :]
        TRI_STR = consts[:, 2, :]
        NEG16 = consts[:, 3, 0:1]
        CAUS4 = caus4[:]

        def ld(eng, out, in_, name, sem="ldc"):
            P.op(eng, lambda e: e.dma_start(out=out, in_=in_), writes=[name], dma_sem=sem)

        ld("sp", consts[:], consts_d, "consts")
        ld("sp", pmask[:], pmask_d, "pmask")
        ld("sp", w_gu[:], w_gu_d, "w_gu")
        ld("sp", gnw[:], gnw_d, "gnw")
        ld("sp", pscale[:], pscale_d, "pscale")
        ld("sp", nmw_pk[:], nmw_d, "nmw")
        ld("sp", nfw_pk[:], nfw_d, "nfw")
        ld("sp", fnw[:], fnw_d.partition_broadcast(128), "fnw")
        ld("sp", b_r[:], b_r_d.partition_broadcast(128), "b_r")
        ld("sp", w_r[:], w_r_d.rearrange("(kc p) n -> p kc n", p=128), "w_r")
        for kc in range(8):
            ld("pool", w_in[:, kc, :], w_in_d[kc * 128:(kc + 1) * 128, :], "w_in", "ldp")
        ld("pool", pool_w[:], pool_w_d.rearrange("g c d -> c g d"), "pool_w", "ldp")
        for kc in range(8):
            ld("pool", w_out[:, kc, :], w_out_d[kc * 128:(kc + 1) * 128, :], "w_out", "ldp")
        for name in ("consts", "pmask", "w_gu", "gnw", "pscale", "nmw", "nfw", "fnw", "b_r", "w_r"):
            P.lastw[name] = ("ldc", P.cnt["ldc"])
        for name in ("w_in", "pool_w", "w_out"):
            P.lastw[name] = ("ldp", P.cnt["ldp"])
        for kc in range(8):
            P.op("dve", lambda e: e.tensor_scalar(out=w_in[:, kc, :], in0=w_in[:, kc, :], scalar1=nmw_pk[:, kc:kc + 1],
                                                   scalar2=None, op0=ALU.mult), reads=["w_in", "nmw"], writes=["w_in"])
            P.op("dve", lambda e: e.tensor_scalar(out=w_r[:, kc, :], in0=w_r[:, kc, :], scalar1=nfw_pk[:, kc:kc + 1],
                                                  scalar2=None, op0=ALU.mult), reads=["w_r", "nfw"], writes=["w_r"])
        P.op("dve", lambda e: e.tensor_copy(out=idb[:], in_=IDF), reads=["consts"], writes=["idb"])
        for h_ in range(4):
            P.op("dve", lambda e: e.tensor_copy(out=caus4[:, h_, :], in_=consts[:, 4, :]), reads=["consts"], writes=["caus4"])
        P.op("dve", lambda e: e.memset(S[:], 0.0), writes=["S"])
        P.op("dve", lambda e: e.memset(g1[:], 1.0), writes=[f"g1_{i}" for i in range(4)])
        for p_ in range(2):
            for i_ in range(2):
                P.op("pool", lambda e: e.memset(qtX[p_][i_][:], 0.0), writes=[f"qt{p_}"])
                P.op("pool", lambda e: e.memset(ktX[p_][i_][:], 0.0), writes=[f"kt{p_}"])
        P.op("dve", lambda e: e.memset(pvT[:], 0.0), writes=["pvT"])

        cvt_tok = {}

        def convert_load(e_):
            s_ = e_ % NSLOT
            P.op("pool", lambda e: e.dma_start(out=wgu[s_][:, :, 0:256],
                                               in_=w_eg_d[e_].rearrange("(kc p) n -> p kc n", p=128)),
                 writes=[f"wgu{s_}a"], dma_sem=f"cvg{s_}")
            P.op("pool", lambda e: e.dma_start(out=wgu[s_][:, :, 256:512],
                                               in_=w_eu_d[e_].rearrange("(kc p) n -> p kc n", p=128)),
                 writes=[f"wgu{s_}b"], dma_sem=f"cvu{s_}")
            P.op("pool", lambda e: e.dma_start(out=wd[s_][:], in_=w_ed_d[e_].rearrange("(hc p) n -> p hc n", p=128)),
                 writes=[f"wd{s_}"], dma_sem=f"cvd{s_}")

        def convert_store(e_):
            s_ = e_ % NSLOT
            for kc in range(8):
                P.op("dve", lambda e: e.tensor_scalar(out=wgu[s_][:, kc, :], in0=wgu[s_][:, kc, :],
                                                      scalar1=nfw_pk[:, kc:kc + 1], scalar2=None, op0=ALU.mult),
                     reads=[f"wgu{s_}a", f"wgu{s_}b", "nfw"], writes=[f"wgu{s_}a", f"wgu{s_}b"])
            P.op("pool", lambda e: e.dma_start(out=wgu_s[e_], in_=wgu[s_][:]), reads=[f"wgu{s_}a", f"wgu{s_}b"],
                 writes=[f"wgus{e_}"], dma_sem=f"csg{s_}")
            P.op("pool", lambda e: e.dma_start(out=wd_s[e_], in_=wd[s_][:]), reads=[f"wd{s_}"],
                 writes=[f"wds{e_}"], dma_sem=f"csd{s_}")

        def run_streams(gens, width, stagger=1):
            it = iter(gens)
            active = []
            since = stagger
            done = False
            while True:
                if not done and len(active) < width and since >= stagger:
                    g = next(it, None)
                    if g is None:
                        done = True
                    else:
                        active.append(g)
                        since = 0
                if not active:
                    if done:
                        break
                    since = stagger
                    continue
                since += 1
                for g in list(reversed(active)):
                    try:
                        next(g)
                    except StopIteration:
                        active.remove(g)

        def rmsnorm(x_ap, xname, wb, wname, out_ap, oname, sidx):
            stn = f"stat{sidx}"
            P.op("dve", lambda e: e.memset(stat[:, sidx, 0:1], 0.0), writes=[stn])
            P.op("act", lambda e: e.activation(out=junk[:], in_=x_ap, func=AF.Square, accum_out=stat[:, sidx, 0:1]),
                 reads=[xname], writes=["junk", stn])
            P.op("act", lambda e: e.activation(out=stat[:, sidx, 1:2], in_=stat[:, sidx, 0:1], func=AF.Ln,
                                               scale=1.0 / D, bias=EPS), reads=[stn], writes=[stn])
            P.op("act", lambda e: e.activation(out=stat[:, sidx, 2:3], in_=stat[:, sidx, 1:2], func=AF.Exp, scale=-0.5),
                 reads=[stn], writes=[stn])
            if wb is None:
                P.op("dve", lambda e: e.tensor_scalar(out=out_ap, in0=x_ap, scalar1=stat[:, sidx, 2:3], scalar2=None,
                                                      op0=ALU.mult), reads=[xname, stn], writes=[oname])
            else:
                P.op("dve", lambda e: e.scalar_tensor_tensor(out=out_ap, in0=x_ap, scalar=stat[:, sidx, 2:3], in1=wb[:],
                                                             op0=ALU.mult, op1=ALU.mult),
                     reads=[xname, stn, wname], writes=[oname])

        def transpose_bf(src, sname, dst3, dname):
            for half in range(2):
                b = nb()
                for c in range(4):
                    kc = half * 4 + c
                    P.op("pe", lambda e: e.matmul(ps[:, b, c * 128:(c + 1) * 128],
                                                  lhsT=src[:, kc * 128:(kc + 1) * 128], rhs=idb[:], start=True, stop=True),
                         reads=[sname, "idb"], writes=[f"ps{b}"], sig=(c == 3))
                P.op("dve", lambda e: e.tensor_copy(
                    out=dst3[:, half * 4:(half + 1) * 4, :], in_=ps[:, b, :].rearrange("p (c n) -> p c n", c=4)),
                    reads=[f"ps{b}"], writes=[dname])

        def kv_tokmajor(c0, xtn):
            bk = nb()
            for kc in range(8):
                P.op("pe", lambda e: e.matmul(ps[:, bk, 0:256], lhsT=xnT[:, kc, c0:c0 + 128],
                                              rhs=w_in[:, kc, 768:1024], start=(kc == 0), stop=(kc == 7)),
                     reads=[xtn, "w_in"], writes=[f"ps{bk}"], sig=(kc == 7))
            bv = nb()
            for kc in range(8):
                P.op("pe", lambda e: e.matmul(ps[:, bv, 0:512], lhsT=xnT[:, kc, c0:c0 + 128],
                                              rhs=w_in[:, kc, 1024:1536], start=(kc == 0), stop=(kc == 7)),
                     reads=[xtn, "w_in"], writes=[f"ps{bv}"], sig=(kc == 7))
            return bk, bv

        def softplus_neg(gc0, g1name, par, mask_col=None):
            bl = nb()
            P.op("pe", lambda e: e.matmul(ps[:, bl, 0:256], lhsT=g1[0:17, gc0:gc0 + 128], rhs=w_gu[:, :],
                                          start=True, stop=True), reads=[g1name, "w_gu"], writes=[f"ps{bl}"], sig=False)
            pe_fence(bl)
            P.op("act", lambda e: e.activation(out=sp[par][:], in_=ps[:, bl, 0:256], func=AF.Exp, scale=-1.0),
                 reads=[f"ps{bl}"], writes=[f"sp{par}"])
            P.op("act", lambda e: e.activation(out=sp[par][:], in_=sp[par][:], func=AF.Ln, bias=1.0),
                 reads=[f"sp{par}"], writes=[f"sp{par}"])
            if mask_col is not None:
                P.op("dve", lambda e: e.tensor_scalar(out=sp[par][:], in0=sp[par][:], scalar1=pmask[:, mask_col:mask_col + 1],
                                                      scalar2=None, op0=ALU.mult),
                     reads=[f"sp{par}", "pmask"], writes=[f"sp{par}"])

        def kend_and_v(bk, bv, par):
            be = nb()
            P.op("pe", lambda e: e.matmul(ps[:, be, 0:256], lhsT=TRI_STR, rhs=sp[par][:], start=True, stop=True),
                 reads=["consts", f"sp{par}"], writes=[f"ps{be}"], sig=False)
            pe_fence(be)
            P.op("act", lambda e: e.activation(out=eend[par][:], in_=ps[:, be, 0:256], func=AF.Exp),
                 reads=[f"ps{be}"], writes=[f"eend{par}"])
            P.op("dve", lambda e: e.tensor_tensor(out=kend[par][:], in0=ps[:, bk, 0:256], in1=eend[par][:], op=ALU.mult),
                 reads=[f"ps{bk}", f"eend{par}"], writes=[f"kend{par}"])
            P.op("dve", lambda e: e.tensor_copy(out=vtok[par][:], in_=ps[:, bv, 0:512]),
                 reads=[f"ps{bv}"], writes=[f"vtok{par}"])

        def cum_decay(par, want_neg):
            bb = nb()
            for j in range(2):
                P.op("pe", lambda e: e.matmul(ps[:, bb, j * 128:(j + 1) * 128], lhsT=sp[par][:, j * 128:(j + 1) * 128],
                                              rhs=TRI_INC, start=True, stop=True),
                     reads=[f"sp{par}", "consts"], writes=[f"ps{bb}"], sig=False)
            pe_fence(bb)
            P.op("act", lambda e: e.activation(out=eb[par][:], in_=ps[:, bb, 0:256].rearrange("p (j n) -> p j n", j=2),
                                               func=AF.Exp), reads=[f"ps{bb}"], writes=[f"eb{par}"])
            if want_neg:
                P.op("act", lambda e: e.activation(out=enb[par][:], in_=ps[:, bb, 0:256].rearrange("p (j n) -> p j n", j=2),
                                                   func=AF.Exp, scale=-1.0), reads=[f"ps{bb}"], writes=[f"enb{par}"])

        def state_update(par, vt_, vtn):
            bs = nb()
            for j in range(2):
                P.op("pe", lambda e: e.matmul(ps[:, bs, j * 256:(j + 1) * 256], lhsT=kend[par][:, j * 128:(j + 1) * 128],
                                              rhs=vt_[:, j * 256:(j + 1) * 256], start=True, stop=True),
                     reads=[f"kend{par}", vtn], writes=[f"ps{bs}"], sig=(j == 1))
            for j in range(2):
                for hh in range(2):
                    r0 = hh * 64
                    P.op("dve", lambda e: e.scalar_tensor_tensor(
                        out=S[r0:r0 + 64, j, :], in0=S[r0:r0 + 64, j, :], scalar=eb[par][r0:r0 + 64, j, 127:128],
                        in1=ps[r0:r0 + 64, bs, j * 256 + hh * 128:j * 256 + hh * 128 + 128],
                        op0=ALU.mult, op1=ALU.add), reads=["S", f"eb{par}", f"ps{bs}"], writes=["S"])

        def prefix_tile(i):
            if i % 2 == 0 and i // 2 < NE:
                convert_load(i // 2)
            if i % 2 == 1 and i // 2 < NE:
                convert_store(i // 2)
            slot = i % 4
            par = i % 2
            c0 = slot * 128
            P.op("sp", lambda e: e.dma_start(out=x[:, slot, :], in_=xp_d[i * 128:(i + 1) * 128, :]),
                 writes=[f"x0{slot}"], dma_sem=f"ldx0{slot}")
            rmsnorm(x[:, slot, :], f"x0{slot}", None, None, xn[par][:], f"xn{par}", slot)
            yield
            transpose_bf(xn[par], f"xn{par}", xnT[:, :, c0:c0 + 128], f"xnT{slot}")
            yield
            bk, bv = kv_tokmajor(c0, f"xnT{slot}")
            bg = nb()
            for kc in range(8):
                P.op("pe", lambda e: e.matmul(ps[0:16, bg, 0:128], lhsT=w_in[:, kc, 2048:2064],
                                              rhs=xnT[:, kc, c0:c0 + 128], start=(kc == 0), stop=(kc == 7)),
                     reads=[f"xnT{slot}", "w_in"], writes=[f"ps{bg}"], sig=(kc == 7))
            if i == NPRE - 1:
                bh = nb()
                for g in range(4):
                    for kc in range(8):
                        P.op("pe", lambda e: e.matmul(ps[:, bh, g * 16:(g + 1) * 16],
                                                      lhsT=w_in[:, kc, g * 128:(g + 1) * 128],
                                                      rhs=xnT[:, kc, c0 + 112:c0 + 128],
                                                      start=(kc == 0), stop=(kc == 7)),
                             reads=[f"xnT{slot}", "w_in"], writes=[f"ps{bh}"], sig=(g == 3 and kc == 7))
                P.op("act", lambda e: e.activation(out=pvT[:, :, 0:16],
                                                   in_=ps[:, bh, 0:64].rearrange("p (g n) -> p g n", g=4), func=AF.Copy),
                     reads=[f"ps{bh}"], writes=["pvT"])
            P.op("dve", lambda e: e.tensor_copy(out=ktok[i % 3][:], in_=ps[:, bk, 0:256]),
                 reads=[f"ps{bk}"], writes=[f"ktok{i % 3}"])
            P.op("dve", lambda e: e.tensor_copy(out=vtok[slot][:], in_=ps[:, bv, 0:512]),
                 reads=[f"ps{bv}"], writes=[f"vtok{slot}"])
            P.op("act", lambda e: e.activation(out=g1[0:16, c0:c0 + 128], in_=ps[0:16, bg, 0:128], func=AF.Copy),
                 reads=[f"ps{bg}"], writes=[f"g1_{slot}"])
            yield
            softplus_neg(c0, f"g1_{slot}", par, mask_col=i)
            yield
            be = nb()
            P.op("pe", lambda e: e.matmul(ps[:, be, 0:256], lhsT=TRI_STR, rhs=sp[par][:], start=True, stop=True),
                 reads=["consts", f"sp{par}"], writes=[f"ps{be}"], sig=False)
            pe_fence(be)
            bb = nb()
            for j in range(2):
                P.op("pe", lambda e: e.matmul(ps[:, bb, j * 128:(j + 1) * 128], lhsT=sp[par][:, j * 128:(j + 1) * 128],
                                              rhs=TRI_INC, start=True, stop=True),
                     reads=[f"sp{par}", "consts"], writes=[f"ps{bb}"], sig=False)
            pe_fence(bb)
            P.op("act", lambda e: e.activation(out=eend[par][:], in_=ps[:, be, 0:256], func=AF.Exp),
                 reads=[f"ps{be}"], writes=[f"eend{par}"])
            P.op("act", lambda e: e.activation(out=eb[par][:], in_=ps[:, bb, 0:256].rearrange("p (j n) -> p j n", j=2),
                                               func=AF.Exp), reads=[f"ps{bb}"], writes=[f"eb{par}"])
            P.op("dve", lambda e: e.tensor_tensor(out=kend[par][:], in0=ktok[i % 3][:], in1=eend[par][:], op=ALU.mult),
                 reads=[f"ktok{i % 3}", f"eend{par}"], writes=[f"kend{par}"])
            yield
            bs = nb()
            for j in range(2):
                P.op("pe", lambda e: e.matmul(ps[:, bs, j * 256:(j + 1) * 256], lhsT=kend[par][:, j * 128:(j + 1) * 128],
                                              rhs=vtok[slot][:, j * 256:(j + 1) * 256], start=True, stop=True),
                     reads=[f"kend{par}", f"vtok{slot}"], writes=[f"ps{bs}"], sig=(j == 1))
            for j in range(2):
                for hh in range(2):
                    r0 = hh * 64
                    P.op("dve", lambda e: e.scalar_tensor_tensor(
                        out=S[r0:r0 + 64, j, :], in0=S[r0:r0 + 64, j, :], scalar=eb[par][r0:r0 + 64, j, 127:128],
                        in1=ps[r0:r0 + 64, bs, j * 256 + hh * 128:j * 256 + hh * 128 + 128],
                        op0=ALU.mult, op1=ALU.add), reads=["S", f"eb{par}", f"ps{bs}"], writes=["S"])

        run_streams([prefix_tile(i) for i in range(NPRE)], PRE_WIDTH, 1)
        for e_ in range(NE):
            if e_ == NPRE // 2 and NPRE % 2 == 1:
                convert_store(e_)
            elif e_ >= (NPRE + 1) // 2:
                convert_load(e_)
                convert_store(e_)
        P.op("act", lambda e: e.activation(out=Sb[0][:], in_=S[:], func=AF.Copy), reads=["S"], writes=["Sb0"])

        XT = [f"xnT{t}" for t in range(4)]
        G1ALL = [f"g1_{t}" for t in range(4)]
        out_toks = []

        def load_expert(ge):
            e_ = ge % NE
            s_ = ge % NSLOT
            P.op("sp", lambda e: e.dma_start(out=wgu[s_][:], in_=wgu_s[e_]), reads=[f"wgus{e_}"],
                 writes=[f"wgu{s_}a", f"wgu{s_}b"], dma_sem=f"wlg{s_}")
            P.op("sp", lambda e: e.dma_start(out=wd[s_][:], in_=wd_s[e_]), reads=[f"wds{e_}"],
                 writes=[f"wd{s_}"], dma_sem=f"wld{s_}")

        NGE = NBLK * NE
        load_expert(0)

        def front_tile(blk, t):
            r0 = blk * 512 + t * 128
            par = t % 2
            pb_ = blk % 2
            x = xb[pb_]
            P.op("sp", lambda e: e.dma_start(out=x[:, t, :], in_=xm_d[r0:r0 + 128, :]),
                 writes=[f"x{pb_}{t}"], dma_sem=f"ldx{pb_}{t}")
            rmsnorm(x[:, t, :], f"x{pb_}{t}", None, None, xn[par][:], f"xn{par}", t)
            yield
            transpose_bf(xn[par], f"xn{par}", xnT[:, :, t * 128:(t + 1) * 128], f"xnT{t}")

        def proj_chunk(col0, m):
            b = nb()
            for kc in range(8):
                P.op("pe", lambda e: e.matmul(ps[0:m, b, :], lhsT=w_in[:, kc, col0:col0 + m], rhs=xnT[:, kc, :],
                                              start=(kc == 0), stop=(kc == 7)),
                     reads=XT + ["w_in"], writes=[f"ps{b}"], sig=(kc == 7))
            return b

        def block_front(blk):
            fts = [front_tile(blk, t) for t in range(4)]
            next(fts[0])
            yield
            for t in range(4):
                if t + 1 < 4:
                    next(fts[t + 1])
                    yield
                for _ in fts[t]:
                    pass
                yield
            for g in range(4):
                b = proj_chunk(g * 128, 128)
                P.op("act", lambda e: e.activation(out=pvT[:, g, 16:528], in_=ps[:, b, :], func=AF.Copy),
                     reads=[f"ps{b}"], writes=["pvT"])
                yield
            for j in range(2):
                b = proj_chunk(512 + j * 128, 128)
                P.op("act", lambda e: e.activation(out=qT[:, j, :], in_=ps[:, b, :], func=AF.Copy),
                     reads=[f"ps{b}"], writes=["qT"])
                yield
            for j in range(2):
                b = proj_chunk(768 + j * 128, 128)
                P.op("act", lambda e: e.activation(out=kT[:, j, :], in_=ps[:, b, :], func=AF.Copy),
                     reads=[f"ps{b}"], writes=["kT"])
                yield
            b = proj_chunk(2048, 16)
            P.op("act", lambda e: e.activation(out=g1[0:16, :], in_=ps[0:16, b, :], func=AF.Copy),
                 reads=[f"ps{b}"], writes=G1ALL)
            for h in range(4):
                b = proj_chunk(1536 + h * 128, 128)
                P.op("act", lambda e: e.activation(out=srT[:, h, :], in_=ps[:, b, :], func=AF.Silu),
                     reads=[f"ps{b}"], writes=["srT"])
                yield
            for g in range(4):
                p_ = pvT[:, g, :]
                w_ = 2 << g
                P.op("pool", lambda e: e.tensor_tensor(out=pa[:, 1:528], in0=p_[:, 1:528], in1=p_[:, 0:527], op=ALU.add),
                     reads=["pvT"], writes=["pa"])
                cur, curname = pa, "pa"
                if g >= 1:
                    P.op("pool", lambda e: e.tensor_tensor(out=pb[:, 3:528], in0=pa[:, 3:528], in1=pa[:, 1:526], op=ALU.add),
                         reads=["pa"], writes=["pb"])
                    cur, curname = pb, "pb"
                if g >= 2:
                    P.op("pool", lambda e: e.tensor_tensor(out=pa[:, 7:528], in0=pb[:, 7:528], in1=pb[:, 3:524], op=ALU.add),
                         reads=["pb"], writes=["pa"])
                    cur, curname = pa, "pa"
                if g >= 3:
                    P.op("pool", lambda e: e.tensor_tensor(out=pb[:, 15:528], in0=pa[:, 15:528], in1=pa[:, 7:520], op=ALU.add),
                         reads=["pa"], writes=["pb"])
                    cur, curname = pb, "pb"
                P.op("dve", lambda e: e.scalar_tensor_tensor(
                    out=pooledT[:, g, :], in0=cur[:, 16:528], scalar=1.0 / w_, in1=p_[:, 16:528],
                    op0=ALU.mult, op1=ALU.subtract), reads=[curname, "pvT"], writes=["pooledT"])
                yield
            P.op("pool", lambda e: e.tensor_copy(out=pvT[:, :, 0:16], in_=pvT[:, :, 512:528]), reads=["pvT"], writes=["pvT"])
            for g in range(4):
                b = nb()
                P.op("pe", lambda e: e.matmul(ps[:, b, :], lhsT=pool_w[:, g, :], rhs=pooledT[:, g, :], start=True, stop=True),
                     reads=["pool_w", "pooledT"], writes=[f"ps{b}"])
                P.op("act", lambda e: e.activation(out=ymT[:, g, :], in_=ps[:, b, :], func=AF.Copy, scale=pscale[:, g:g + 1]),
                     reads=[f"ps{b}", "pscale"], writes=["ymTp"])
            yield

        tile_ctr = [0]

        def main_tile(blk, t):
            c0 = t * 128
            tc = tile_ctr[0]
            par = tc % 2
            tile_ctr[0] += 1
            kt_, vt_ = ktok[tc % 3], vtok[tc % 4]
            ktn, vtn = f"ktok{tc % 3}", f"vtok{tc % 4}"
            stat4 = stat4b[par]
            st4n = f"stat4{par}"
            pb_ = blk % 2
            x = xb[pb_]
            xn_ = f"x{pb_}{t}"
            bk, bv = kv_tokmajor(c0, f"xnT{t}")
            P.op("dve", lambda e: e.tensor_copy(out=kt_[:], in_=ps[:, bk, 0:256]), reads=[f"ps{bk}"], writes=[ktn])
            P.op("dve", lambda e: e.tensor_copy(out=vt_[:], in_=ps[:, bv, 0:512]), reads=[f"ps{bv}"], writes=[vtn])
            yield
            softplus_neg(c0, f"g1_{t}", par)
            yield
            be = nb()
            P.op("pe", lambda e: e.matmul(ps[:, be, 0:256], lhsT=TRI_STR, rhs=sp[par][:], start=True, stop=True),
                 reads=["consts", f"sp{par}"], writes=[f"ps{be}"], sig=False)
            pe_fence(be)
            cum_decay(par, True)
            P.op("act", lambda e: e.activation(out=eend[par][:], in_=ps[:, be, 0:256], func=AF.Exp),
                 reads=[f"ps{be}"], writes=[f"eend{par}"])
            P.op("dve", lambda e: e.tensor_tensor(out=kend[par][:], in0=kt_[:], in1=eend[par][:], op=ALU.mult),
                 reads=[ktn, f"eend{par}"], writes=[f"kend{par}"])
            for i_ in range(2):
                r0 = i_ * 64
                P.op("dve", lambda e: e.scalar_tensor_tensor(out=qtX[par][i_][r0:r0 + 64], in0=qT[r0:r0 + 64, :, c0:c0 + 128],
                                                             scalar=0.125, in1=eb[par][r0:r0 + 64], op0=ALU.mult, op1=ALU.mult),
                     reads=["qT", f"eb{par}"], writes=[f"qt{par}"])
                P.op("dve", lambda e: e.tensor_tensor(out=ktX[par][i_][r0:r0 + 64], in0=kT[r0:r0 + 64, :, c0:c0 + 128],
                                                      in1=enb[par][r0:r0 + 64], op=ALU.mult),
                     reads=["kT", f"enb{par}"], writes=[f"kt{par}"])
            yield
            bsc = nb()
            for h in range(4):
                j = h // 2
                P.op("pe", lambda e: e.matmul(ps[:, bsc, h * 128:(h + 1) * 128], lhsT=ktX[par][h % 2][:, j, :],
                                              rhs=qtX[par][h % 2][:, j, :], start=True, stop=True),
                     reads=[f"kt{par}", f"qt{par}"], writes=[f"ps{bsc}"], sig=(h == 3))
            P.op("dve", lambda e: e.tensor_tensor(out=scT[:], in0=ps[:, bsc, :].rearrange("p (h n) -> p h n", h=4),
                                                  in1=CAUS4, op=ALU.mult),
                 reads=[f"ps{bsc}", "caus4"], writes=["scT"])
            yield
            bo = nb()
            for h in range(4):
                j = h // 2
                P.op("pe", lambda e: e.matmul(ps[:, bo, h * 128:(h + 1) * 128], lhsT=scT[:, h, :],
                                              rhs=vt_[:, h * 128:(h + 1) * 128], start=True, stop=False),
                     reads=["scT", vtn], writes=[f"ps{bo}"], sig=False)
                P.op("pe", lambda e: e.matmul(ps[:, bo, h * 128:(h + 1) * 128], lhsT=qtX[par][h % 2][:, j, :],
                                              rhs=Sb[par][:, j, :], start=False, stop=True),
                     reads=[f"qt{par}", f"Sb{par}"], writes=[f"ps{bo}"], sig=(h == 3))
            state_update(par, vt_, vtn)
            P.op("act", lambda e: e.activation(out=Sb[1 - par][:], in_=S[:], func=AF.Copy),
                 reads=["S"], writes=[f"Sb{1 - par}"])
            P.op("dve", lambda e: e.memset(stat4[:, 0:4], 0.0), writes=[st4n])
            for h in range(4):
                P.op("act", lambda e: e.activation(out=junk[:, 0:128], in_=ps[:, bo, h * 128:(h + 1) * 128],
                                                   func=AF.Square, accum_out=stat4[:, h:h + 1]),
                     reads=[f"ps{bo}"], writes=["junk", st4n])
            P.op("act", lambda e: e.activation(out=stat4[:, 4:8], in_=stat4[:, 0:4], func=AF.Ln, scale=1.0 / 128,
                                               bias=EPS), reads=[st4n], writes=[st4n])
            P.op("act", lambda e: e.activation(out=stat4[:, 8:12], in_=stat4[:, 4:8], func=AF.Exp, scale=-0.5),
                 reads=[st4n], writes=[st4n])
            for h in range(4):
                P.op("act", lambda e: e.activation(out=on[:, h * 128:(h + 1) * 128], in_=ps[:, bo, h * 128:(h + 1) * 128],
                                                   func=AF.Copy, scale=stat4[:, 8 + h:9 + h]),
                     reads=[f"ps{bo}", st4n], writes=["on"])
            yield
            bt = nb()
            for h in range(4):
                P.op("pe", lambda e: e.matmul(ps[:, bt, h * 128:(h + 1) * 128], lhsT=on[:, h * 128:(h + 1) * 128],
                                              rhs=idb[:], start=True, stop=True),
                     reads=["on", "idb"], writes=[f"ps{bt}"], sig=(h == 3))
            P.op("dve", lambda e: e.scalar_tensor_tensor(out=ymT[:, 4:8, c0:c0 + 128],
                                                         in0=ps[:, bt, :].rearrange("p (h n) -> p h n", h=4),
                                                         scalar=gnw[:, 0:1], in1=srT[:, :, c0:c0 + 128],
                                                         op0=ALU.mult, op1=ALU.mult),
                 reads=[f"ps{bt}", "gnw", "srT"], writes=[f"ymTg{t}"])
            yield
            for half in range(2):
                b = nb()
                for kc in range(8):
                    P.op("pe", lambda e: e.matmul(
                        ps[:, b, :], lhsT=ymT[:, kc, c0:c0 + 128], rhs=w_out[:, kc, half * 512:(half + 1) * 512],
                        start=(kc == 0), stop=(kc == 7)), reads=["ymTp", f"ymTg{t}", "w_out"], writes=[f"ps{b}"],
                        sig=(kc == 7))
                P.op("dve", lambda e: e.tensor_tensor(
                    out=x[:, t, half * 512:(half + 1) * 512], in0=ps[:, b, :], in1=x[:, t, half * 512:(half + 1) * 512],
                    op=ALU.add), reads=[f"ps{b}", xn_], writes=[xn_])
            if not OVERLAP:
                yield
                for _ in main_tile_b(blk, t):
                    yield

        def main_tile_b(blk, t):
            c0 = t * 128
            par = t % 2
            rt = rtb[par]
            rtn = f"rt{par}"
            pb_ = blk % 2
            x = xb[pb_]
            rmsnorm(x[:, t, :], f"x{pb_}{t}", None, None, xn2f[:], "xn2f", t)
            yield
            for half in range(2):
                b = nb()
                for c in range(4):
                    kc = half * 4 + c
                    P.op("pe", lambda e: e.matmul(ps[:, b, c * 128:(c + 1) * 128],
                                                  lhsT=xn2f[:, kc * 128:(kc + 1) * 128], rhs=IDF, start=True, stop=True),
                         reads=["xn2f", "consts"], writes=[f"ps{b}"], sig=(c == 3))
                P.op("dve", lambda e: e.tensor_copy(
                    out=xn2Tf[:, half * 4:(half + 1) * 4, :], in_=ps[:, b, :].rearrange("p (c n) -> p c n", c=4)),
                    reads=[f"ps{b}"], writes=["xn2Tf"])
                P.op("act", lambda e: e.activation(
                    out=xn2T[:, half * 4:(half + 1) * 4, c0:c0 + 128], in_=xn2Tf[:, half * 4:(half + 1) * 4, :],
                    func=AF.Copy), reads=["xn2Tf"], writes=[f"xn2T{t}"])
            yield
            br = nb()
            for kc in range(8):
                P.op("pe", lambda e: e.matmul(ps[:, br, 0:20], lhsT=xn2Tf[:, kc, :], rhs=w_r[:, kc, :],
                                              start=(kc == 0), stop=(kc == 7)),
                     reads=["xn2Tf", "w_r"], writes=[f"ps{br}"], sig=False)
            pe_fence(br)
            V = lambda fn: P.op("dve", fn, reads=[rtn], writes=[rtn])
            P.op("dve", lambda e: e.tensor_tensor(out=rt[:, 0:20], in0=ps[:, br, 0:20], in1=b_r[:], op=ALU.add),
                 reads=[f"ps{br}", "b_r"], writes=[rtn])
            V(lambda e: e.tensor_reduce(out=rt[:, 20:21], in_=rt[:, 0:4], axis=AX.X, op=ALU.max))
            V(lambda e: e.tensor_scalar(out=rt[:, 24:28], in0=rt[:, 0:4], scalar1=rt[:, 20:21], scalar2=None, op0=ALU.is_equal))
            V(lambda e: e.tensor_scalar(out=rt[:, 21:22], in0=rt[:, 20:21], scalar1=-1.0, scalar2=None, op0=ALU.mult))
            P.op("dve", lambda e: e.memset(rt[:, 22:23], 0.0), reads=[rtn], writes=[rtn])
            P.op("act", lambda e: e.activation(out=rt[:, 28:32], in_=rt[:, 0:4], func=AF.Exp, bias=rt[:, 21:22],
                                               accum_out=rt[:, 22:23]), reads=[rtn], writes=[rtn])
            V(lambda e: e.reciprocal(out=rt[:, 23:24], in_=rt[:, 22:23]))
            V(lambda e: e.tensor_scalar(out=rt[:, 32:36], in0=rt[:, 24:28], scalar1=BIG, scalar2=-BIG, op0=ALU.mult, op1=ALU.add))
            for g in range(4):
                V(lambda e: e.tensor_scalar(out=rt[:, 40 + 4 * g:44 + 4 * g], in0=rt[:, 4 + 4 * g:8 + 4 * g],
                                            scalar1=rt[:, 32 + g:33 + g], scalar2=None, op0=ALU.add))
            V(lambda e: e.tensor_reduce(out=rt[:, 36:37], in_=rt[:, 40:56], axis=AX.X, op=ALU.max))
            V(lambda e: e.tensor_scalar(out=rt[:, 56:72], in0=rt[:, 40:56], scalar1=rt[:, 36:37], scalar2=None, op0=ALU.is_equal))
            V(lambda e: e.scalar_tensor_tensor(out=rt[:, 72:88], in0=rt[:, 56:72], scalar=-BIG, in1=rt[:, 40:56],
                                               op0=ALU.mult, op1=ALU.add))
            V(lambda e: e.tensor_reduce(out=rt[:, 37:38], in_=rt[:, 72:88], axis=AX.X, op=ALU.max))
            V(lambda e: e.tensor_scalar(out=rt[:, 88:104], in0=rt[:, 72:88], scalar1=rt[:, 37:38], scalar2=None, op0=ALU.is_equal))
            V(lambda e: e.tensor_tensor(out=rt[:, 38:39], in0=rt[:, 37:38], in1=rt[:, 36:37], op=ALU.subtract))
            P.op("act", lambda e: e.activation(out=rt[:, 39:40], in_=rt[:, 38:39], func=AF.Exp), reads=[rtn], writes=[rtn])
            V(lambda e: e.tensor_scalar(out=rt[:, 104:105], in0=rt[:, 39:40], scalar1=1.0, scalar2=None, op0=ALU.add))
            V(lambda e: e.reciprocal(out=rt[:, 105:106], in_=rt[:, 104:105]))
            V(lambda e: e.tensor_tensor(out=rt[:, 106:107], in0=rt[:, 105:106], in1=rt[:, 23:24], op=ALU.mult))
            V(lambda e: e.tensor_tensor(out=rt[:, 107:108], in0=rt[:, 23:24], in1=rt[:, 106:107], op=ALU.subtract))
            V(lambda e: e.tensor_scalar(out=rt[:, 108:124], in0=rt[:, 56:72], scalar1=rt[:, 106:107], scalar2=None, op0=ALU.mult))
            P.op("dve", lambda e: e.scalar_tensor_tensor(out=comb[:, t, :], in0=rt[:, 88:104], scalar=rt[:, 107:108],
                                                         in1=rt[:, 108:124], op0=ALU.mult, op1=ALU.add),
                 reads=[rtn], writes=[f"comb{t}"])

        X2T = [f"xn2T{t}" for t in range(4)]

        def experts(blk):
            pb_ = blk % 2
            x = xb[pb_]
            for e_ in range(NE):
                ge = blk * NE + e_
                s_ = ge % NSLOT
                if ge + 1 < NGE:
                    load_expert(ge + 1)
                hp = ge % 2
                for hc in range(2):
                    bb2 = []
                    for gu in range(2):
                        b = nb()
                        bb2.append(b)
                        col = gu * 256 + hc * 128
                        for kc in range(8):
                            P.op("pe", lambda e: e.matmul(
                                ps[:, b, :], lhsT=wgu[s_][:, kc, col:col + 128], rhs=xn2T[:, kc, :],
                                start=(kc == 0), stop=(kc == 7)), reads=X2T + [f"wgu{s_}" + "ab"[gu]], writes=[f"ps{b}"],
                                sig=(kc == 7))
                    bg_, bu_ = bb2
                    P.op("act", lambda e: e.activation(out=sg[:, hc, :], in_=ps[:, bg_, :], func=AF.Silu),
                         reads=[f"ps{bg_}"], writes=[f"sg{hc}"])
                    P.op("dve", lambda e: e.tensor_tensor(out=hT[hp][:, hc, :], in0=ps[:, bu_, :], in1=sg[:, hc, :], op=ALU.mult),
                         reads=[f"ps{bu_}", f"sg{hc}"], writes=[f"hT{hp}"])
                for t in range(4):
                    for half in range(2):
                        b = nb()
                        for hc in range(2):
                            P.op("pe", lambda e: e.matmul(
                                ps[:, b, :], lhsT=hT[hp][:, hc, t * 128:(t + 1) * 128],
                                rhs=wd[s_][:, hc, half * 512:(half + 1) * 512], start=(hc == 0), stop=(hc == 1)),
                                reads=[f"hT{hp}", f"wd{s_}"], writes=[f"ps{b}"], sig=(hc == 1))
                        P.op("dve", lambda e: e.scalar_tensor_tensor(
                            out=x[:, t, half * 512:(half + 1) * 512], in0=ps[:, b, :], scalar=comb[:, t, e_:e_ + 1],
                            in1=x[:, t, half * 512:(half + 1) * 512], op0=ALU.mult, op1=ALU.add),
                            reads=[f"ps{b}", f"comb{t}", f"x{pb_}{t}"], writes=[f"x{pb_}{t}"])
                yield

        def final_tile(blk, t):
            pb_ = blk % 2
            x = xb[pb_]
            r0 = blk * 512 + t * 128
            rmsnorm(x[:, t, :], f"x{pb_}{t}", fnw, "fnw", x[:, t, :], f"x{pb_}{t}", t)
            out_toks.append(P.op("sp", lambda e: e.dma_start(out=out_d[r0:r0 + 128, :], in_=x[:, t, :]),
                                 reads=[f"x{pb_}{t}"], dma_sem=f"st{pb_}{t}"))

        def final(blk):
            for t in range(4):
                final_tile(blk, t)

        def tiles_gen(blk):
            gens = [main_tile(blk, t) for t in range(4)]
            active, nxt, since = [], 0, MIX_STAGGER
            while active or nxt < 4:
                if nxt < 4 and len(active) < MIX_WIDTH and since >= MIX_STAGGER:
                    active.append(gens[nxt])
                    nxt += 1
                    since = 0
                since += 1
                for g in list(reversed(active)):
                    try:
                        next(g)
                    except StopIteration:
                        active.remove(g)
                yield

        def mixer1(blk):
            for _ in block_front(blk):
                yield
            for _ in tiles_gen(blk):
                yield

        def part2(blk):
            if OVERLAP:
                run_streams([main_tile_b(blk, t) for t in range(4)], 2, 2)

        if OVERLAP:
            for _ in mixer1(0):
                pass
            part2(0)
            for blk in range(NBLK):
                mg = mixer1(blk + 1) if blk + 1 < NBLK else None
                for _ in experts(blk):
                    if mg is not None:
                        for _k in range(4):
                            if next(mg, "done") == "done":
                                mg = None
                                break
                final(blk)
                if mg is not None:
                    for _ in mg:
                        pass
                if blk + 1 < NBLK:
                    part2(blk + 1)
        else:
            for _ in block_front(0):
                pass
            tg = tiles_gen(0)
            for blk in range(NBLK):
                for _ in tg:
                    pass
                fg = block_front(blk + 1) if blk + 1 < NBLK else None
                for _ in experts(blk):
                    if fg is not None:
                        for _k in range(2):
                            if next(fg, "done") == "done":
                                fg = None
                                break
                if fg is not None:
                    for _ in fg:
                        pass
                tg = tiles_gen(blk + 1) if blk + 1 < NBLK else iter(())
                for t in range(4):
                    final_tile(blk, t)
                    next(tg, None)
        P.finish("sp", out_toks)
        P.emit(sems)
    return nc


def make_consts():
    c = np.zeros((128, 5, 128), np.float32)
    s = np.arange(128)[:, None]
    cc = np.arange(128)[None, :]
    c[:, 0] = np.eye(128, dtype=np.float32)
    c[:, 1] = np.where(s <= cc, -1.0 / 16, 0.0)
    c[:, 2] = np.where(s > cc, -1.0 / 16, 0.0)
    c[:, 3] = -1.0 / 16
    c[:, 4] = (s <= cc).astype(np.float32)
    return c


def make_in_maps(inp, seq):
    B = inp["x"].shape[0]
    half_len = seq // 2
    NPRE = half_len // 128 + 1
    f = lambda a: np.ascontiguousarray(np.asarray(a, dtype=np.float32))
    x = f(inp["x"])
    meta = f(inp["meta_tokens"])
    shared = {
        "w_in": f(inp["w_in"][0]),
        "w_gu_b": f(np.concatenate([inp["w_gate_up"][0], inp["b_gate"][0][None, :]], axis=0)),
        "gnw": f(inp["gla_norm_w"][0].reshape(128, 1)),
        "pool_w": f(inp["pool_w"][0]),
        "pscale": f(inp["pool_scale"][0].reshape(4, 128).T),
        "w_out": f(inp["w_out"][0]),
        "nmw_pk": f(inp["norm_mix_w"][0].reshape(8, 128).T),
        "nfw_pk": f(inp["norm_ffn_w"][0].reshape(8, 128).T),
        "fnw": f(inp["final_norm_w"]),
        "w_r": f(np.concatenate([inp["w_router_group"][0], inp["w_router_expert"][0]], axis=1)),
        "b_r": f(np.concatenate([inp["b_router_group"][0], inp["b_router_expert"][0]], axis=0)),
        "w_eg": f(inp["w_expert_gate"][0]),
        "w_eu": f(inp["w_expert_up"][0]),
        "w_ed": f(inp["w_expert_down"][0]),
        "consts": make_consts(),
    }
    maps = []
    for b in range(B):
        for half in range(2):
            xp = np.zeros((NPRE * 128, D), np.float32)
            mask = np.zeros((NPRE * 128,), np.float32)
            if half == 0:
                xp[-16:] = meta
                mask[-16:] = 1.0
            else:
                xp[112:128] = meta
                xp[128:] = x[b, :half_len]
                mask[112:] = 1.0
            m = dict(shared)
            m["xp"] = xp
            m["xm"] = np.ascontiguousarray(x[b, half * half_len:(half + 1) * half_len])
            m["pmask"] = np.ascontiguousarray(mask.reshape(NPRE, 128).T)
            maps.append(m)
    return maps, NPRE, half_len // 512


def kernel(**inputs):
    x = np.asarray(inputs["x"])
    B, seq, _ = x.shape
    maps, NPRE, NBLK = make_in_maps(inputs, seq)
    nc = build(NPRE, NBLK)
    res = run_bass_kernel_spmd(nc, maps, core_ids=list(range(len(maps))))
    half_len = seq // 2
    out = np.empty((B, seq, D), np.float32)
    for b in range(B):
        for half in range(2):
            out[b, half * half_len:(half + 1) * half_len] = res.results[2 * b + half]["out"]
    return out
```
